# Optimizing a Trainium2 kernel written in Bass

```python
import jax
import jax.numpy as jnp
from jax import lax
import numpy as np

D_MODEL = 1024
BATCH = 8
SEQ = 2048
DEPTH = 4

N_MEM = 256
EPS = 1e-6
NEG_INF = -1e30

HEAD_DIM = 64
ROPE_DIM = HEAD_DIM // 4
ROPE_THETA = 500000.0

A_HEADS = 8
A_WIDTH = A_HEADS * HEAD_DIM
MOBA_BLOCK = 256
MOBA_TOPK = 3
MOBA_QCHUNK = 32

B_GROUPS = 8
B_WIDTH = B_GROUPS * HEAD_DIM
GMLP_CHUNK = 128

GLA_HEADS = 4
GLA_DK = (D_MODEL // 2) // GLA_HEADS
GLA_DV = D_MODEL // GLA_HEADS
GLA_LOWRANK = 16
GLA_TAU = 16.0
GLA_CHUNK = 64

X_HEADS = 4
X_HEAD_DIM = D_MODEL // X_HEADS

D_FF = -(-8 * D_MODEL // (3 * 256)) * 256

EV_IN = 3 * A_WIDTH + 2 * B_WIDTH
OD_IN = 2 * GLA_HEADS * GLA_DK + 2 * GLA_HEADS * GLA_DV

kernel_name = 'hybrid_moba_gmlp_gla_trunk'


def rmsnorm(x, g):
    xf = x.astype(jnp.float32)
    y = xf * lax.rsqrt(jnp.mean(xf * xf, axis=-1, keepdims=True) + EPS)
    return (y * g.astype(jnp.float32)).astype(x.dtype)


def layernorm(x, g, b):
    xf = x.astype(jnp.float32)
    mu = jnp.mean(xf, axis=-1, keepdims=True)
    var = jnp.mean(jnp.square(xf - mu), axis=-1, keepdims=True)
    y = (xf - mu) * lax.rsqrt(var + EPS) * g.astype(jnp.float32) + b.astype(jnp.float32)
    return y.astype(x.dtype)


def partial_rope(x, positions):
    half = ROPE_DIM // 2
    inv_freq = ROPE_THETA ** (-jnp.arange(half, dtype=jnp.float32) * 2.0 / ROPE_DIM)
    ang = positions.astype(jnp.float32)[:, None, :, None] * inv_freq
    cos, sin = jnp.cos(ang), jnp.sin(ang)
    xf = x.astype(jnp.float32)
    x1, x2 = xf[..., :half], xf[..., half:ROPE_DIM]
    out = jnp.concatenate([x1 * cos - x2 * sin, x2 * cos + x1 * sin, xf[..., ROPE_DIM:]], axis=-1)
    return out.astype(x.dtype)


def moba_attention(q, k, v):
    Bn, H, S, Dh = q.shape
    S_pad = -(-S // MOBA_BLOCK) * MOBA_BLOCK
    pad = [(0, 0), (0, 0), (0, S_pad - S), (0, 0)]
    q, k, v = jnp.pad(q, pad), jnp.pad(k, pad), jnp.pad(v, pad)
    nb = S_pad // MOBA_BLOCK
    topk = min(MOBA_TOPK, nb)
    scale = Dh ** -0.5
    kb = k.reshape(Bn, H, nb, MOBA_BLOCK, Dh)
    vb = v.reshape(Bn, H, nb, MOBA_BLOCK, Dh)
    k_mean = jnp.mean(kb.astype(jnp.float32), axis=3)
    gate = jnp.einsum('bhsd,bhnd->bhsn', q.astype(jnp.float32), k_mean)
    own = jnp.arange(S_pad) // MOBA_BLOCK
    past = jnp.arange(nb)[None, :] < own[:, None]
    gate = jnp.where(past[None, None], gate, NEG_INF)
    _, sel = lax.top_k(gate, topk)
    valid = sel < own[None, None, :, None]

    nq = S_pad // MOBA_QCHUNK
    q_c = q.reshape(Bn, H, nq, MOBA_QCHUNK, Dh).transpose(2, 0, 1, 3, 4)
    sel_c = sel.reshape(Bn, H, nq, MOBA_QCHUNK, topk).transpose(2, 0, 1, 3, 4)
    valid_c = valid.reshape(Bn, H, nq, MOBA_QCHUNK, topk).transpose(2, 0, 1, 3, 4)
    chunk_ids = jnp.arange(nq, dtype=jnp.int32)
    bi = jnp.arange(Bn)[:, None, None, None]
    hi = jnp.arange(H)[None, :, None, None]
    key_idx = jnp.arange(MOBA_BLOCK)

    def one_chunk(args):
        qc, selc, validc, ci = args
        blk = (ci * MOBA_QCHUNK) // MOBA_BLOCK
        k_own = lax.dynamic_index_in_dim(kb, blk, axis=2, keepdims=False)
        v_own = lax.dynamic_index_in_dim(vb, blk, axis=2, keepdims=False)
        qpos = (ci * MOBA_QCHUNK) % MOBA_BLOCK + jnp.arange(MOBA_QCHUNK)
        causal = key_idx[None, :] <= qpos[:, None]
        s_own = jnp.einsum('bhqd,bhkd->bhqk', qc, k_own, preferred_element_type=jnp.float32) * scale
        s_own = jnp.where(causal[None, None], s_own, NEG_INF)
        k_sel = kb[bi, hi, selc]
        v_sel = vb[bi, hi, selc]
        s_sel = jnp.einsum('bhqd,bhqnkd->bhqnk', qc, k_sel, preferred_element_type=jnp.float32) * scale
        s_sel = jnp.where(validc[..., None], s_sel, NEG_INF)
        logits = jnp.concatenate([s_own, s_sel.reshape(Bn, H, MOBA_QCHUNK, topk * MOBA_BLOCK)], axis=-1)
        p = jax.nn.softmax(logits, axis=-1)
        p_own = p[..., :MOBA_BLOCK].astype(v.dtype)
        p_sel = p[..., MOBA_BLOCK:].reshape(Bn, H, MOBA_QCHUNK, topk, MOBA_BLOCK).astype(v.dtype)
        out = (jnp.einsum('bhqk,bhkd->bhqd', p_own, v_own, preferred_element_type=jnp.float32)
               + jnp.einsum('bhqnk,bhqnkd->bhqd', p_sel, v_sel, preferred_element_type=jnp.float32))
        return out.astype(q.dtype)

    out = lax.map(one_chunk, (q_c, sel_c, valid_c, chunk_ids))
    out = out.transpose(1, 2, 0, 3, 4).reshape(Bn, H, S_pad, Dh)
    return out[:, :, :S]


def chunked_spatial_gating(u, v, w_s, b_s, ln_g, ln_b):
    Bn, S, G, Dg = v.shape
    nc = S // GMLP_CHUNK
    vn = layernorm(v, ln_g, ln_b).reshape(Bn, nc, GMLP_CHUNK, G, Dg)
    mask = jnp.tril(jnp.ones((GMLP_CHUNK, GMLP_CHUNK), dtype=w_s.dtype))
    ws = w_s * mask[None]
    mixed = jnp.einsum('gij,bcjgd->bcigd', ws, vn) + b_s.T[None, None, :, :, None]
    return u * mixed.reshape(Bn, S, G, Dg).astype(u.dtype)


def even_mixer(h, positions, w_in, q_gain, k_gain, w_s, b_s, ln_g, ln_b, w_out):
    Bn, S, _ = h.shape
    proj = h @ w_in
    q, k, v, pu, pv = jnp.split(proj, [A_WIDTH, 2 * A_WIDTH, 3 * A_WIDTH, 3 * A_WIDTH + B_WIDTH], axis=-1)

    def heads(t):
        return t.reshape(Bn, S, A_HEADS, HEAD_DIM).transpose(0, 2, 1, 3)

    q = partial_rope(rmsnorm(heads(q), q_gain), positions)
    k = partial_rope(rmsnorm(heads(k), k_gain), positions)
    a = moba_attention(q, k, heads(v)).transpose(0, 2, 1, 3).reshape(Bn, S, A_WIDTH)
    u = jax.nn.gelu(pu, approximate=False).reshape(Bn, S, B_GROUPS, HEAD_DIM)
    vg = jax.nn.gelu(pv, approximate=False).reshape(Bn, S, B_GROUPS, HEAD_DIM)
    g = chunked_spatial_gating(u, vg, w_s, b_s, ln_g, ln_b).reshape(Bn, S, B_WIDTH)
    return jnp.concatenate([a, g], axis=-1) @ w_out


def gla_chunked(q, k, v, log_a):
    Bn, H, S, dk = q.shape
    dv = v.shape[-1]
    nc = S // GLA_CHUNK
    f32 = jnp.float32
    qc = q.astype(f32).reshape(Bn, H, nc, GLA_CHUNK, dk)
    kc = k.astype(f32).reshape(Bn, H, nc, GLA_CHUNK, dk)
    vc = v.astype(f32).reshape(Bn, H, nc, GLA_CHUNK, dv)
    b = jnp.cumsum(log_a.astype(f32).reshape(Bn, H, nc, GLA_CHUNK, dk), axis=3)
    b_last = b[:, :, :, -1:, :]
    q_in = qc * jnp.exp(b)
    k_in = kc * jnp.exp(-b)
    causal = jnp.tril(jnp.ones((GLA_CHUNK, GLA_CHUNK), dtype=bool))
    att = jnp.where(causal, jnp.einsum('bhnid,bhnjd->bhnij', q_in, k_in), 0.0)
    o_intra = jnp.einsum('bhnij,bhnjv->bhniv', att, vc)
    k_state = kc * jnp.exp(b_last - b)
    d_state = jnp.einsum('bhnjd,bhnjv->bhndv', k_state, vc)
    decay = jnp.exp(b_last[:, :, :, 0, :])

    def step(s_prev, xs):
        ds_n, dec_n = xs
        return dec_n[..., None] * s_prev + ds_n, s_prev

    _, s_before = lax.scan(step, jnp.zeros((Bn, H, dk, dv), f32),
                           (jnp.moveaxis(d_state, 2, 0), jnp.moveaxis(decay, 2, 0)))
    s_before = jnp.moveaxis(s_before, 0, 2)
    o_inter = jnp.einsum('bhnid,bhndv->bhniv', q_in, s_before)
    return (o_intra + o_inter).reshape(Bn, H, S, dv).astype(v.dtype)


def gla_mixer(h, w_in, w_g1, w_g2, b_g, o_gain, w_out):
    Bn, S, _ = h.shape
    qk_w = GLA_HEADS * GLA_DK
    v_w = GLA_HEADS * GLA_DV
    proj = h @ w_in
    q, k, v, r = jnp.split(proj, [qk_w, 2 * qk_w, 2 * qk_w + v_w], axis=-1)
    log_a = jax.nn.log_sigmoid(((h @ w_g1) @ w_g2 + b_g).astype(jnp.float32)) / GLA_TAU

    def heads(t, d):
        return t.reshape(Bn, S, GLA_HEADS, d).transpose(0, 2, 1, 3)

    o = gla_chunked(heads(q, GLA_DK) * (GLA_DK ** -0.5), heads(k, GLA_DK),
                    heads(v, GLA_DV), heads(log_a, GLA_DK))
    o = rmsnorm(o, o_gain[:, None, :])
    o = o.transpose(0, 2, 1, 3).reshape(Bn, S, v_w) * jax.nn.silu(r)
    return o @ w_out


def mem_cross_attention(h, mem_n, w_q, w_kv, q_gain, k_gain, w_out):
    Bn, S, _ = h.shape
    M = mem_n.shape[1]
    q = rmsnorm((h @ w_q).reshape(Bn, S, X_HEADS, X_HEAD_DIM), q_gain)
    k, v = jnp.split(mem_n @ w_kv, 2, axis=-1)
    k = rmsnorm(k.reshape(Bn, M, X_HEADS, X_HEAD_DIM), k_gain)
    v = v.reshape(Bn, M, X_HEADS, X_HEAD_DIM)
    s = jnp.einsum('bqhd,bkhd->bhqk', q, k, preferred_element_type=jnp.float32) * (X_HEAD_DIM ** -0.5)
    p = jax.nn.softmax(s, axis=-1).astype(v.dtype)
    o = jnp.einsum('bhqk,bkhd->bqhd', p, v).reshape(Bn, S, D_MODEL)
    return o @ w_out


def swiglu(h, w_gu, w_down):
    g, u = jnp.split(h @ w_gu, 2, axis=-1)
    return (jax.nn.silu(g) * u) @ w_down


def setup_inputs(seed: int = 0) -> dict:
    key = jax.random.key(seed)
    ks = iter(jax.random.split(key, 32))
    f32 = jnp.float32
    n_ev = (DEPTH + 1) // 2
    n_od = DEPTH // 2

    def nrm(shape, scale):
        return jax.random.normal(next(ks), shape, f32) * scale

    def gain(shape):
        return 1.0 + 0.05 * jax.random.normal(next(ks), shape, f32)

    x = nrm((BATCH, SEQ, D_MODEL), 1.0)
    mem = nrm((BATCH, N_MEM, D_MODEL), 1.0)
    positions = (jnp.arange(SEQ, dtype=jnp.int32)[None, :]
                 + jax.random.randint(next(ks), (BATCH, 1), 0, 4096, dtype=jnp.int32))
    return {
        'x': x,
        'mem': mem,
        'positions': positions,
        'norm_mix': gain((DEPTH, D_MODEL)),
        'norm_mem_q': gain((DEPTH, D_MODEL)),
        'norm_mem_kv': gain((DEPTH, D_MODEL)),
        'norm_ffn': gain((DEPTH, D_MODEL)),
        'ev_w_in': nrm((n_ev, D_MODEL, EV_IN), D_MODEL ** -0.5),
        'ev_q_gain': gain((n_ev, HEAD_DIM)),
        'ev_k_gain': gain((n_ev, HEAD_DIM)),
        'ev_w_s': nrm((n_ev, B_GROUPS, GMLP_CHUNK, GMLP_CHUNK), GMLP_CHUNK ** -0.5),
        'ev_b_s': gain((n_ev, B_GROUPS, GMLP_CHUNK)),
        'ev_ln_g': gain((n_ev, B_GROUPS, HEAD_DIM)),
        'ev_ln_b': nrm((n_ev, B_GROUPS, HEAD_DIM), 0.02),
        'ev_w_out': nrm((n_ev, A_WIDTH + B_WIDTH, D_MODEL), 0.5 * (A_WIDTH + B_WIDTH) ** -0.5),
        'od_w_in': nrm((n_od, D_MODEL, OD_IN), D_MODEL ** -0.5),
        'od_w_g1': nrm((n_od, D_MODEL, GLA_LOWRANK), D_MODEL ** -0.5),
        'od_w_g2': nrm((n_od, GLA_LOWRANK, GLA_HEADS * GLA_DK), GLA_LOWRANK ** -0.5),
        'od_b_g': nrm((n_od, GLA_HEADS * GLA_DK), 0.02),
        'od_o_gain': gain((n_od, GLA_HEADS, GLA_DV)),
        'od_w_out': nrm((n_od, GLA_HEADS * GLA_DV, D_MODEL), 0.5 * (GLA_HEADS * GLA_DV) ** -0.5),
        'xa_w_q': nrm((DEPTH, D_MODEL, D_MODEL), D_MODEL ** -0.5),
        'xa_w_kv': nrm((DEPTH, D_MODEL, 2 * D_MODEL), D_MODEL ** -0.5),
        'xa_q_gain': gain((DEPTH, X_HEAD_DIM)),
        'xa_k_gain': gain((DEPTH, X_HEAD_DIM)),
        'xa_w_out': nrm((DEPTH, D_MODEL, D_MODEL), 0.5 * D_MODEL ** -0.5),
        'ffn_w_gu': nrm((DEPTH, D_MODEL, 2 * D_FF), D_MODEL ** -0.5),
        'ffn_w_down': nrm((DEPTH, D_FF, D_MODEL), 0.5 * D_FF ** -0.5),
    }


def reference(x, mem, positions, norm_mix, norm_mem_q, norm_mem_kv, norm_ffn,
              ev_w_in, ev_q_gain, ev_k_gain, ev_w_s, ev_b_s, ev_ln_g, ev_ln_b, ev_w_out,
              od_w_in, od_w_g1, od_w_g2, od_b_g, od_o_gain, od_w_out,
              xa_w_q, xa_w_kv, xa_q_gain, xa_k_gain, xa_w_out,
              ffn_w_gu, ffn_w_down):
    for l in range(DEPTH):
        i = l // 2
        h = rmsnorm(x, norm_mix[l])
        if l % 2 == 0:
            x = x + even_mixer(h, positions, ev_w_in[i], ev_q_gain[i], ev_k_gain[i],
                               ev_w_s[i], ev_b_s[i], ev_ln_g[i], ev_ln_b[i], ev_w_out[i])
        else:
            x = x + gla_mixer(h, od_w_in[i], od_w_g1[i], od_w_g2[i], od_b_g[i],
                              od_o_gain[i], od_w_out[i])
        x = x + mem_cross_attention(rmsnorm(x, norm_mem_q[l]), rmsnorm(mem, norm_mem_kv[l]),
                                    xa_w_q[l], xa_w_kv[l], xa_q_gain[l], xa_k_gain[l], xa_w_out[l])
        x = x + swiglu(rmsnorm(x, norm_ffn[l]), ffn_w_gu[l], ffn_w_down[l])
    return x
```

```python
import numpy as np
from contextlib import ExitStack
import concourse.bass as bass
import concourse.mybir as mybir
from concourse.bass_utils import run_bass_kernel_spmd

F32 = mybir.dt.float32
BF16 = mybir.dt.bfloat16
I32 = mybir.dt.int32
AF = mybir.ActivationFunctionType
ALU = mybir.AluOpType
AX = mybir.AxisListType

S_LEN = 2048
D = 1024
NMEM = 256
DEPTH = 4
DFF = 2816
EPS = 1e-6
NEG = -30000.0
ENGS = ("pe", "act", "dve", "pool", "sp")


class _Op:
    __slots__ = ("eng", "fn", "deps", "is_dma", "slot", "ticket", "needs_inc", "waits")

    def __init__(self, eng, fn, is_dma, slot):
        self.eng = eng
        self.fn = fn
        self.deps = ()
        self.is_dma = is_dma
        self.slot = slot
        self.ticket = None
        self.needs_inc = False
        self.waits = None


class Sched:
    def __init__(self, nc):
        self.nc = nc
        self.ops = {e: [] for e in ENGS}
        self.last_w = {}
        self.readers = {}
        self.all_ops = []
        self.slot_last = {}

    def op(self, eng, fn, reads=(), writes=(), dma_slot=None):
        o = _Op(eng, fn, dma_slot is not None, dma_slot)
        if dma_slot is not None:
            self.slot_last[dma_slot] = o
        deps = set()
        for r in reads:
            w = self.last_w.get(r)
            if w is not None:
                deps.add(w)
        for r in writes:
            w = self.last_w.get(r)
            if w is not None:
                deps.add(w)
            rl = self.readers.get(r)
            if rl:
                deps.update(rl)
        deps = set((self.slot_last[d.slot] if d.is_dma else d) for d in deps)
        deps.discard(o)
        o.deps = tuple(deps)
        for r in reads:
            self.readers.setdefault(r, []).append(o)
        for r in writes:
            self.last_w[r] = o
            self.readers[r] = []
        self.all_ops.append(o)
        self.ops[eng].append(o)
        return o

    def barrier(self):
        lasts = []
        for e in ENGS:
            for o in reversed(self.ops[e]):
                if not o.is_dma and o.fn is not None:
                    lasts.append(o)
                    break
        slots = {}
        for o in self.all_ops:
            if o.is_dma:
                slots[o.slot] = o
        lasts.extend(slots.values())
        for e in ENGS:
            b = _Op(e, None, False, None)
            b.deps = tuple(lasts)
            self.all_ops.append(b)
            self.ops[e].append(b)
        self.last_w = {}
        self.readers = {}

    def final_wait(self, eng, ops):
        b = _Op(eng, None, False, None)
        b.deps = tuple(ops)
        self.all_ops.append(b)
        self.ops[eng].append(b)

    @staticmethod
    def _skip(d, o):
        return (not d.is_dma) and d.eng == o.eng and d.eng == "pe"

    def finalize(self, es):
        nc = self.nc
        for o in self.all_ops:
            for d in o.deps:
                if d.is_dma or self._skip(d, o):
                    continue
                d.needs_inc = True
        self.sems = {e: es.enter_context(nc.semaphore("prog_" + e)) for e in ENGS}
        cnt = {e: 0 for e in ENGS}
        slot_cnt = {}
        self.slot_sems = {}
        for o in self.all_ops:
            if o.is_dma:
                if o.slot not in self.slot_sems:
                    self.slot_sems[o.slot] = es.enter_context(nc.semaphore("dma_%d" % len(self.slot_sems)))
                    slot_cnt[o.slot] = 0
                slot_cnt[o.slot] += 16
                o.ticket = slot_cnt[o.slot]
            elif o.needs_inc:
                cnt[o.eng] += 1
                o.ticket = cnt[o.eng]
        waited = {e: {} for e in ENGS}
        for o in self.all_ops:
            need = {}
            for d in o.deps:
                if d.is_dma:
                    key = ("slot", d.slot)
                else:
                    if self._skip(d, o):
                        continue
                    key = ("eng", d.eng)
                if need.get(key, 0) < d.ticket:
                    need[key] = d.ticket
            w = []
            wd = waited[o.eng]
            for key, v in need.items():
                if wd.get(key, 0) >= v:
                    continue
                wd[key] = v
                w.append((key, v))
            o.waits = w

    def _sem(self, key):
        return self.sems[key[1]] if key[0] == "eng" else self.slot_sems[key[1]]

    def emit(self, block):
        def body(engname):
            def f(e):
                for o in self.ops[engname]:
                    for key, v in o.waits:
                        e.wait_ge(self._sem(key), v)
                    if o.fn is None:
                        continue
                    ins = o.fn(e)
                    if o.is_dma:
                        ins.then_inc(self.slot_sems[o.slot], 16)
                    elif o.needs_inc:
                        ins.then_inc(self.sems[engname], 1)
            return f
        block.tensor(body("pe"))
        block.scalar(body("act"))
        block.vector(body("dve"))
        block.gpsimd(body("pool"))
        block.sync(body("sp"))


C_ID, C_BO, C_PM, C_CN, C_C01, C_U01, C_ONE, C_E8, C_INVF, C_SGN, C_END = (
    0, 128, 256, 384, 512, 640, 768, 896, 1920, 1921, 1922)


def _consts():
    c = np.zeros((128, C_END), np.float32)
    p = np.arange(128)
    c[:, C_ID:C_ID + 128] = np.eye(128, dtype=np.float32)
    c[:, C_BO:C_BO + 128] = (p[:, None] // 64 == p[None, :] // 64).astype(np.float32)
    pm = np.zeros((128, 128), np.float32)
    for m in range(128):
        r = m % 64
        if r < 8:
            pm[m + 8, m] = 1.0
        elif r < 16:
            pm[m - 8, m] = 1.0
    c[:, C_PM:C_PM + 128] = pm
    c[:, C_CN:C_CN + 128] = np.where(p[:, None] <= p[None, :], 0.0, NEG)
    c[:, C_C01:C_C01 + 128] = (p[:, None] <= p[None, :]).astype(np.float32)
    c[:, C_U01:C_U01 + 128] = ((p[:, None] <= p[None, :]) & (p[:, None] // 64 == p[None, :] // 64)).astype(np.float32)
    c[:, C_ONE:C_ONE + 128] = 1.0
    e8 = np.zeros((128, 8, 128), np.float32)
    for n in range(8):
        e8[(p % 32) == n, n, :] = 1.0
    c[:, C_E8:C_E8 + 1024] = e8.reshape(128, 1024)
    half = 8
    inv_freq = (np.float32(500000.0) ** (-np.arange(half, dtype=np.float32) * np.float32(2.0) / np.float32(16))).astype(np.float32)
    r = p % 64
    c[:, C_INVF] = np.where(r < 16, inv_freq[r % 8], 0.0)
    c[:, C_SGN] = np.where(r < 8, -1.0, np.where(r < 16, 1.0, 0.0))
    return c


class Builder:
    def __init__(self, layers, subs=("mix", "xa", "ffn")):
        self.layers = list(layers)
        self.subs = subs
        self.nc = bass.Bass("TRN2", target_bir_lowering=False)
        self.dram = {}
        self.bank_ctr = 0
        self.reserved = set()
        self.conv_dve = False
        self.stop_at = 0
        self.stage_ctr = 0
        self.wb_ctr = 0
        self.uid = 0

    def din(self, name, shape, dt=F32):
        t = self.nc.dram_tensor(name, list(shape), dt, kind="ExternalInput").ap()
        self.dram[name] = t
        return t

    def bank(self):
        while True:
            b = self.bank_ctr % 8
            self.bank_ctr += 1
            if b not in self.reserved:
                return b

    def reserve(self, n):
        out = []
        for _ in range(n):
            b = self.bank()
            self.reserved.add(b)
            out.append(b)
        return out

    def release(self, banks):
        for b in banks:
            self.reserved.discard(b)

    def psf(self, b, lo=0, hi=512):
        return self.PS[:, 512 * b + lo:512 * b + hi]

    def psb(self, b):
        return self.PS[:, 512 * b:512 * b + 512].bitcast(BF16)

    def key(self, name):
        self.uid += 1
        return (name, self.uid)

    def mm(self, out, lhsT, rhs, start, stop, reads, writes, **kw):
        return self.S.op("pe", lambda e: e.matmul(out, lhsT, rhs, start=start, stop=stop, **kw), reads=reads, writes=writes)

    def tr(self, out, in_, ident, reads, writes):
        return self.S.op("pe", lambda e: e.transpose(out, in_, ident), reads=reads, writes=writes)

    def act(self, out, in_, func, reads, writes, **kw):
        return self.S.op("act", lambda e: e.activation(out=out, in_=in_, func=func, **kw), reads=reads, writes=writes)

    def tt(self, eng, out, in0, in1, op, reads, writes):
        return self.S.op(eng, lambda e: e.tensor_tensor(out, in0, in1, op), reads=reads, writes=writes)

    def ts(self, eng, out, in0, s1, s2, op0, op1, reads, writes):
        if op1 is None:
            return self.S.op(eng, lambda e: e.tensor_scalar(out, in0, s1, None, op0), reads=reads, writes=writes)
        return self.S.op(eng, lambda e: e.tensor_scalar(out, in0, s1, s2, op0, op1), reads=reads, writes=writes)

    def stt(self, eng, out, in0, scalar, in1, op0, op1, reads, writes):
        return self.S.op(eng, lambda e: e.scalar_tensor_tensor(out, in0, scalar, in1, op0, op1), reads=reads, writes=writes)

    def cp(self, eng, out, in_, reads, writes):
        if eng == "act":
            return self.S.op("act", lambda e: e.copy(out, in_), reads=reads, writes=writes)
        return self.S.op(eng, lambda e: e.tensor_copy(out, in_), reads=reads, writes=writes)

    def recip(self, out, in_, reads, writes):
        return self.S.op("dve", lambda e: e.reciprocal(out, in_), reads=reads, writes=writes)

    def memset(self, eng, ap, val, writes):
        return self.S.op(eng, lambda e: e.memset(ap, val), writes=writes)

    def dma(self, out, in_, slot, reads=(), writes=()):
        return self.S.op("sp", lambda e: e.dma_start(out=out, in_=in_), reads=reads, writes=writes, dma_slot=slot)

    def a_reset(self):
        self.a_off = 0

    def a_f32(self, n):
        ap = self.ARENA[:, self.a_off:self.a_off + n]
        self.a_off += n
        assert self.a_off <= self.A_WORDS, ("arena overflow", self.a_off, self.A_WORDS)
        return ap

    def a_bf(self, n):
        w = (n + 1) // 2
        return self.a_f32(w).bitcast(BF16)

    def _conv(self, i, dst, src, g, skey, wkey):
        e = ("pool", "act", "dve")[i % 3] if self.conv_dve else ("pool", "act")[i % 2]
        if g is None:
            self.cp(e, dst, src, reads=[skey], writes=[wkey])
        elif e == "act":
            self.S.op("act", lambda en: en.activation(out=dst, in_=src, func=AF.Copy, scale=g), reads=[skey], writes=[wkey])
        else:
            self.ts(e, dst, src, g, None, ALU.mult, None, reads=[skey], writes=[wkey])

    def load_cols(self, wsrc, ranges, dst, gain, wkey):
        for kh in range(2):
            s = self.stage_ctr % 2
            self.stage_ctr += 1
            st = self.STAGE[s].rearrange("p (k n) -> p k n", k=4)
            skey = ("stage", s)
            off = 0
            for (c0, w) in ranges:
                self.dma(st[:, :, off:off + w], wsrc[:, 4 * kh:4 * kh + 4, c0:c0 + w], slot=("stage", s), writes=[skey])
                off += w
            for kk in range(4):
                k = 4 * kh + kk
                g = None if gain is None else gain[:, k:k + 1]
                self._conv(k, dst[:, k, :], st[:, kk, :], g, skey, wkey)

    def load_rows(self, wsrc, dst, n, gain, wkey):
        for i in range(n):
            s = self.stage_ctr % 2
            self.stage_ctr += 1
            st = self.STAGE[s]
            skey = ("stage", s)
            self.dma(st, wsrc[:, i, :], slot=("stage", s), writes=[skey])
            g = None if gain is None else gain[:, i:i + 1]
            for q in range(2):
                self._conv(2 * i + q, dst[:, i, 512 * q:512 * q + 512], st[:, 512 * q:512 * q + 512], g, skey, wkey)

    def wslab(self):
        i = self.wb_ctr % 3
        self.wb_ctr += 1
        return self.WB[i], ("wb", i)

    def norm_T(self):
        S = self.S
        for t in range(4):
            sl = slice(512 * t, 512 * t + 512)
            b = self.bank()
            for k in range(8):
                sq = self.SQ[k % 2]
                self.act(sq, self.xT[:, k, sl], AF.Square, reads=[("xT", k, t)], writes=[("sq", k % 2)])
                self.mm(self.psf(b), self.ones_bf, sq, k == 0, k == 7, reads=[("sq", k % 2)], writes=[("ps", b)])
            self.act(self.SD[0], self.psf(b), AF.Sqrt, reads=[("ps", b)], writes=[("sd", 0)], bias=self.eps_t[:, 0:1], scale=1.0 / D)
            self.recip(self.SD[1], self.SD[0], reads=[("sd", 0)], writes=[("sd", 1)])
            for k in range(8):
                self.tt("dve" if k % 2 == 0 else "pool", self.hT[:, k, sl], self.xT[:, k, sl], self.SD[1], ALU.mult,
                        reads=[("xT", k, t), ("sd", 1)], writes=[("hT", k, t)])

    def resid_add(self, b, k, t):
        sl = slice(512 * t, 512 * t + 512)
        self.tt("dve", self.xT[:, k, sl], self.psf(b), self.xT[:, k, sl], ALU.add,
                reads=[("ps", b), ("xT", k, t)], writes=[("xT", k, t)])

    def ffn(self, l):
        self.S.barrier()
        self.a_reset()
        self.norm_T()
        gain = self.G[:, l, 24:32]
        wgu = self.dram["ffn_w_gu"][l].rearrange("(k p) n -> p k n", p=128)
        wdn = self.dram["ffn_w_down"][l].rearrange("(c p) n -> p c n", p=128)
        parts = [(0, 6), (6, 12), (12, 17), (17, 22)]
        actT = self.a_bf(6 * 2048).rearrange("p (c n) -> p c n", c=6)
        wd = self.a_bf(6 * 1024).rearrange("p (c n) -> p c n", c=6)
        sg = [self.a_f32(512), self.a_f32(512)]
        for (c0, c1) in parts:
            self.load_rows(wdn[:, c0:c1, :], wd, c1 - c0, None, "wd")
            for c in range(c0, c1):
                wb, wk = self.wslab()
                wv = wb.rearrange("p (k n) -> p k n", k=8)
                self.load_cols(wgu, [(c * 128, 128), (DFF + c * 128, 128)], wv, gain, wk)
                for t in range(4):
                    sl = slice(512 * t, 512 * t + 512)
                    bg, bu = self.bank(), self.bank()
                    for k in range(8):
                        self.mm(self.psf(bg), wv[:, k, 0:128], self.hT[:, k, sl], k == 0, k == 7, reads=[wk, ("hT", k, t)], writes=[("ps", bg)])
                    for k in range(8):
                        self.mm(self.psf(bu), wv[:, k, 128:256], self.hT[:, k, sl], k == 0, k == 7, reads=[wk, ("hT", k, t)], writes=[("ps", bu)])
                    si = (c * 4 + t) % 2
                    self.act(sg[si], self.psf(bg), AF.Silu, reads=[("ps", bg)], writes=[("sg", si)])
                    self.tt("dve", actT[:, c - c0, sl], sg[si], self.psf(bu), ALU.mult, reads=[("sg", si), ("ps", bu)], writes=[("actT", c - c0, t)])
            nchunk = c1 - c0
            for dk in range(8):
                for t in range(4):
                    sl = slice(512 * t, 512 * t + 512)
                    b = self.bank()
                    for j in range(nchunk):
                        self.mm(self.psf(b), wd[:, j, dk * 128:(dk + 1) * 128], actT[:, j, sl], j == 0, j == nchunk - 1,
                                reads=["wd", ("actT", j, t)], writes=[("ps", b)])
                    self.resid_add(b, dk, t)

    def xattn(self, l):
        S = self.S
        S.barrier()
        self.a_reset()
        self.norm_T()
        gq = self.G[:, l, 8:16]
        gkv = self.G[:, l, 16:24]
        wq = self.dram["xa_w_q"][l].rearrange("(k p) n -> p k n", p=128)
        wkv = self.dram["xa_w_kv"][l].rearrange("(k p) n -> p k n", p=128)
        wo = self.dram["xa_w_out"][l].rearrange("(k p) n -> p k n", p=128)
        qT = self.a_bf(8 * 2048).rearrange("p (c n) -> p c n", c=8)
        kT = self.a_bf(8 * 256).rearrange("p (c n) -> p c n", c=8)
        vt = self.a_bf(2 * 1024).rearrange("p (c n) -> p c n", c=2)
        sqb = [self.a_bf(512), self.a_bf(512)]
        sd = [self.a_f32(512), self.a_f32(512)]
        pT = [self.a_bf(512), self.a_bf(512)]
        rden = self.a_f32(512)
        qg = self.xqg[:, l, :]
        kg = self.xkg[:, l, :]
        for cg in range(8):
            wb, wk = self.wslab()
            wv = wb.rearrange("p (k n) -> p k n", k=8)
            self.load_cols(wkv, [(cg * 256, 256)], wv, gkv, wk)
            if cg < 4:
                for j in range(2):
                    fc = cg * 2 + j
                    b = self.bank()
                    for k in range(8):
                        self.mm(self.psf(b, 0, 256), wv[:, k, j * 128:(j + 1) * 128], self.memT[:, k, :], k == 0, k == 7,
                                reads=[wk, "memT"], writes=[("ps", b)])
                    self.cp("act", kT[:, fc, :], self.psf(b, 0, 256), reads=[("ps", b)], writes=[("kT", fc)])
                    self.act(sqb[j][:, 0:256], self.psf(b, 0, 256), AF.Square, reads=[("ps", b)], writes=[("sqb", j)])
                h = cg
                b2 = self.bank()
                for j in range(2):
                    self.mm(self.psf(b2, 0, 256), self.ones_bf, sqb[j][:, 0:256], j == 0, j == 1, reads=[("sqb", j)], writes=[("ps", b2)])
                self.act(sd[0][:, 0:256], self.psf(b2, 0, 256), AF.Sqrt, reads=[("ps", b2)], writes=[("sd", 0)], bias=self.eps_t[:, 0:1], scale=1.0 / 256)
                self.recip(sd[1][:, 0:256], sd[0][:, 0:256], reads=[("sd", 0)], writes=[("sd", 1)])
                for j in range(2):
                    fc = 2 * h + j
                    self.stt("dve", kT[:, fc, :], kT[:, fc, :], kg[:, j:j + 1], sd[1][:, 0:256], ALU.mult, ALU.mult,
                             reads=[("kT", fc), ("sd", 1)], writes=[("kT", fc)])
            else:
                for mt in range(2):
                    b = self.bank()
                    for k in range(8):
                        self.mm(self.psf(b, 0, 256), self.memT[:, k, mt * 128:(mt + 1) * 128], wv[:, k, :], k == 0, k == 7,
                                reads=[wk, "memT"], writes=[("ps", b)])
                    c0 = (cg - 4) * 256
                    self.cp("act", vt[:, mt, c0:c0 + 256], self.psf(b, 0, 256), reads=[("ps", b)], writes=[("vt", mt, cg)])
        if self.stop_at == 1:
            return
        for cg in range(4):
            h = cg
            wb, wk = self.wslab()
            wv = wb.rearrange("p (k n) -> p k n", k=8)
            self.load_cols(wq, [(cg * 256, 256)], wv, gq, wk)
            for t in range(4):
                sl = slice(512 * t, 512 * t + 512)
                for j in range(2):
                    fc = 2 * h + j
                    b = self.bank()
                    for k in range(8):
                        self.mm(self.psf(b), wv[:, k, j * 128:(j + 1) * 128], self.hT[:, k, sl], k == 0, k == 7,
                                reads=[wk, ("hT", k, t)], writes=[("ps", b)])
                    self.cp("act", qT[:, fc, sl], self.psf(b), reads=[("ps", b)], writes=[("qT", fc, t)])
                    self.act(sqb[j], self.psf(b), AF.Square, reads=[("ps", b)], writes=[("sqb", j)])
                b2 = self.bank()
                for j in range(2):
                    self.mm(self.psf(b2), self.ones_bf, sqb[j], j == 0, j == 1, reads=[("sqb", j)], writes=[("ps", b2)])
                self.act(sd[0], self.psf(b2), AF.Sqrt, reads=[("ps", b2)], writes=[("sd", 0)], bias=self.eps_t[:, 0:1], scale=1.0 / 256)
                self.recip(sd[1], sd[0], reads=[("sd", 0)], writes=[("sd", 1)])
                for j in range(2):
                    fc = 2 * h + j
                    self.stt("dve", qT[:, fc, sl], qT[:, fc, sl], qg[:, j:j + 1], sd[1], ALU.mult, ALU.mult,
                             reads=[("qT", fc, t), ("sd", 1)], writes=[("qT", fc, t)])
        if self.stop_at == 2:
            return
        S.barrier()
        oT = self.hT
        scale = float(256 ** -0.5)
        for h in range(4):
            for t in range(4):
                sl = slice(512 * t, 512 * t + 512)
                for kt in range(2):
                    b = self.bank()
                    for j in range(2):
                        fc = 2 * h + j
                        self.mm(self.psf(b), kT[:, fc, kt * 128:(kt + 1) * 128], qT[:, fc, sl], j == 0, j == 1,
                                reads=[("kT", fc), ("qT", fc, t)], writes=[("ps", b)])
                    self.act(pT[kt], self.psf(b), AF.Exp, reads=[("ps", b)], writes=[("pT", kt)], scale=scale)
                bd = self.bank()
                for kt in range(2):
                    self.mm(self.psf(bd), self.ones_bf, pT[kt], kt == 0, kt == 1, reads=[("pT", kt)], writes=[("ps", bd)])
                self.recip(rden, self.psf(bd), reads=[("ps", bd)], writes=["rden"])
                for j in range(2):
                    fc = 2 * h + j
                    b = self.bank()
                    for kt in range(2):
                        self.mm(self.psf(b), vt[:, kt, fc * 128:(fc + 1) * 128], pT[kt], kt == 0, kt == 1,
                                reads=[("vt", kt, 4 + fc // 2), ("pT", kt)], writes=[("ps", b)])
                    self.tt("dve", oT[:, fc, sl], self.psf(b), rden, ALU.mult, reads=[("ps", b), "rden"], writes=[("oT", fc, t)])
        if self.stop_at == 3:
            return
        for cg in range(4):
            wb, wk = self.wslab()
            wv = wb.rearrange("p (k n) -> p k n", k=8)
            self.load_cols(wo, [(cg * 256, 256)], wv, None, wk)
            for j in range(2):
                dk = cg * 2 + j
                for t in range(4):
                    sl = slice(512 * t, 512 * t + 512)
                    b = self.bank()
                    for k in range(8):
                        self.mm(self.psf(b), wv[:, k, j * 128:(j + 1) * 128], oT[:, k, sl], k == 0, k == 7,
                                reads=[wk, ("oT", k, t)], writes=[("ps", b)])
                    self.resid_add(b, dk, t)

    def gla(self, l):
        S = self.S
        i = l // 2
        S.barrier()
        self.a_reset()
        self.norm_T()
        gain = self.G[:, l, 0:8]
        win = self.dram["od_w_in"][i].rearrange("(k p) n -> p k n", p=128)
        wout = self.dram["od_w_out"][i].rearrange("(c p) n -> p c n", p=128)
        wg1 = self.dram["od_w_g1"][i].rearrange("(k p) n -> p k n", p=128)
        og = self.ogain[:, i, :]
        g1aug = self.a_bf(2048)
        wg2 = self.a_bf(512)
        wg1b = self.a_bf(128).rearrange("p (k n) -> p k n", k=8)
        qin = self.a_bf(2048)
        kin = self.a_bf(2048)
        q2 = self.a_bf(2048)
        kin_tok = self.a_bf(2048).rearrange("p (t n) -> p t n", t=16)
        l_tok = self.a_bf(2048).rearrange("p (t n) -> p t n", t=16)
        v_tok = self.a_bf(4096).rearrange("p (t n) -> p t n", t=16)
        rs = self.a_bf(4096).rearrange("p (j n) -> p j n", j=2)
        attT = l_tok
        ogt = self.a_bf(4096).rearrange("p (j n) -> p j n", j=2)
        sqb = self.SQ
        dec = self.a_f32(32)
        R = self.a_f32(256)
        Rbf = [self.a_bf(256), self.a_bf(256)]
        e1 = [self.a_f32(512)]
        eq = self.a_f32(512)
        ek = self.a_f32(512)
        sd = self.SD
        tmp = self.a_f32(512)
        wo_b = self.a_bf(2 * 1024).rearrange("p (j n) -> p j n", j=2)
        self.memset("pool", g1aug[0:32, :], 1.0, writes=["g1aug"])
        st = self.STAGE[self.stage_ctr % 2]
        skey = ("stage", self.stage_ctr % 2)
        sslot = ("stage", self.stage_ctr % 2)
        self.stage_ctr += 1
        self.dma(st[:, 0:128].rearrange("p (k n) -> p k n", k=8), wg1, slot=sslot, writes=[skey])
        self.dma(st[0:16, 128:640], self.dram["od_w_g2"][i], slot=sslot, writes=[skey])
        self.dma(st[16:17, 128:640], self.dram["od_b_g"][i:i + 1, :], slot=sslot, writes=[skey])
        for k in range(8):
            self.ts("dve", wg1b[:, k, :], st[:, 16 * k:16 * k + 16], gain[:, k:k + 1], None, ALU.mult, None, reads=[skey], writes=["wg1b"])
        self.cp("dve", wg2[0:17, :], st[0:17, 128:640], reads=[skey], writes=["wg2"])
        for t in range(4):
            sl = slice(512 * t, 512 * t + 512)
            b = self.bank()
            for k in range(8):
                self.mm(self.PS[0:16, 512 * b:512 * b + 512], wg1b[:, k, :], self.hT[:, k, sl], k == 0, k == 7,
                        reads=["wg1b", ("hT", k, t)], writes=[("ps", b)])
            self.cp("act", g1aug[0:16, sl], self.PS[0:16, 512 * b:512 * b + 512], reads=[("ps", b)], writes=["g1aug"])
        for h in range(4):
            wbqk, wkqk = self.wslab()
            wqk = wbqk.rearrange("p (k n) -> p k n", k=8)
            self.load_cols(win, [(128 * h, 128), (512 + 128 * h, 128)], wqk, gain, wkqk)
            wbv, wkv_ = self.wslab()
            wvv = wbv.rearrange("p (k n) -> p k n", k=8)
            self.load_cols(win, [(1024 + 256 * h, 256)], wvv, gain, wkv_)
            wbr, wkr = self.wslab()
            wrv = wbr.rearrange("p (k n) -> p k n", k=8)
            self.load_cols(win, [(2048 + 256 * h, 256)], wrv, gain, wkr)
            for t in range(4):
                b = self.bank()
                for tt in range(4):
                    tok = 4 * t + tt
                    self.mm(self.psf(b, 128 * tt, 128 * tt + 128), g1aug[0:17, tok * 128:(tok + 1) * 128], wg2[0:17, 128 * h:128 * h + 128],
                            True, True, reads=["g1aug", "wg2"], writes=[("ps", b)])
                self.act(e1[0], self.psf(b), AF.Exp, reads=[("ps", b)], writes=["e1"], scale=-1.0)
                self.act(l_tok[:, 4 * t:4 * t + 4, :], e1[0].rearrange("p (t n) -> p t n", t=4), AF.Ln,
                         reads=["e1"], writes=[("LA", t)], bias=self.one_t[:, 0:1], scale=1.0)
            for tk in range(0, 16, 2):
                b = self.bank()
                for u in range(2):
                    tok = tk + u
                    for k in range(8):
                        self.mm(self.psf(b, 256 * u, 256 * u + 256), self.hT[:, k, tok * 128:(tok + 1) * 128], wvv[:, k, :], k == 0, k == 7,
                                reads=[wkv_, ("hT", k, tok // 4)], writes=[("ps", b)])
                self.cp("act", v_tok[:, tk:tk + 2, :], self.psf(b).rearrange("p (u n) -> p u n", u=2), reads=[("ps", b)], writes=[("v_tok", tk // 2)])
            for j in range(2):
                for t in range(4):
                    sl = slice(512 * t, 512 * t + 512)
                    b = self.bank()
                    for k in range(8):
                        self.mm(self.psf(b), wrv[:, k, 128 * j:128 * j + 128], self.hT[:, k, sl], k == 0, k == 7,
                                reads=[wkr, ("hT", k, t)], writes=[("ps", b)])
                    self.act(rs[:, j, sl], self.psf(b), AF.Silu, reads=[("ps", b)], writes=[("rs", j, t)])
            for t in range(4):
                sl = slice(512 * t, 512 * t + 512)
                bb = self.bank()
                for tt in range(4):
                    tok = 4 * t + tt
                    self.mm(self.psf(bb, 128 * tt, 128 * tt + 128), l_tok[:, tok, :], self.U_bf, True, True,
                            reads=[("LA", t)], writes=[("ps", bb)])
                self.act(eq, self.psf(bb), AF.Exp, reads=[("ps", bb)], writes=["eq"], scale=-1.0 / 16)
                self.act(ek, self.psf(bb), AF.Exp, reads=[("ps", bb)], writes=["ek"], scale=1.0 / 16)
                self.act(dec[:, 8 * t:8 * t + 8], self.psf(bb).rearrange("p (n c) -> p n c", c=64)[:, :, 63], AF.Exp,
                         reads=[("ps", bb)], writes=["dec"], scale=-1.0 / 16)
                bq = self.bank()
                for k in range(8):
                    self.mm(self.psf(bq), wqk[:, k, 0:128], self.hT[:, k, sl], k == 0, k == 7, reads=[wkqk, ("hT", k, t)], writes=[("ps", bq)])
                self.stt("dve", qin[:, sl], self.psf(bq), float(128 ** -0.5), eq, ALU.mult, ALU.mult, reads=[("ps", bq), "eq"], writes=[("qin", t)])
                bk = self.bank()
                for k in range(8):
                    self.mm(self.psf(bk), wqk[:, k, 128:256], self.hT[:, k, sl], k == 0, k == 7, reads=[wkqk, ("hT", k, t)], writes=[("ps", bk)])
                self.tt("dve", kin[:, sl], self.psf(bk), ek, ALU.mult, reads=[("ps", bk), "ek"], writes=[("kin", t)])
            self.tt("dve", q2[:, 64:2048].rearrange("p (n c) -> p n c", c=64), qin[:, 64:2048].rearrange("p (n c) -> p n c", c=64),
                    dec[:, 0:31].unsqueeze(2).broadcast_to([128, 31, 64]), ALU.mult,
                    reads=[("qin", t) for t in range(4)] + ["dec"], writes=["q2"])
            for t in range(4):
                b = self.bank()
                pb = self.psb(b)
                for tt in range(4):
                    tok = 4 * t + tt
                    self.tr(pb[:, 128 * tt:128 * tt + 128], kin[:, tok * 128:(tok + 1) * 128], self.ident_bf, reads=[("kin", t)], writes=[("ps", b)])
                self.cp("act", kin_tok[:, 4 * t:4 * t + 4, :], pb[:, 0:512].rearrange("p (t n) -> p t n", t=4), reads=[("ps", b)], writes=[("kin_tok", t)])
            for t in range(4):
                b = self.bank()
                for tt in range(4):
                    tok = 4 * t + tt
                    tsl = slice(tok * 128, tok * 128 + 128)
                    self.mm(self.psf(b, 128 * tt, 128 * tt + 128), kin[:, tsl], qin[:, tsl], True, True, reads=[("kin", t), ("qin", t)], writes=[("ps", b)])
                self.tt("dve", attT[:, 4 * t:4 * t + 4, :], self.psf(b).rearrange("p (t n) -> p t n", t=4),
                        self.U01.unsqueeze(1).broadcast_to([128, 4, 128]), ALU.mult, reads=[("ps", b)], writes=[("LA", t)])
            for t in range(4):
                sl = slice(512 * t, 512 * t + 512)
                bo = self.reserve(2)
                for tt in range(4):
                    p = 4 * t + tt
                    for s2 in range(2):
                        n = 2 * p + s2
                        bs = self.bank()
                        self.mm(self.psf(bs, 0, 256), kin_tok[64 * s2:64 * s2 + 64, p, :], v_tok[64 * s2:64 * s2 + 64, p, :], True, True,
                                reads=[("kin_tok", t), ("v_tok", p // 2)], writes=[("ps", bs)])
                        if s2 == 0:
                            for j in range(2):
                                self.mm(self.psf(bo[j], 128 * tt, 128 * tt + 128), v_tok[:, p, 128 * j:128 * j + 128], attT[:, p, :], True, False,
                                        reads=[("v_tok", p // 2), ("LA", t)], writes=[("ps", bo[j])], skip_group_check=True)
                        if n >= 1:
                            for j in range(2):
                                c0 = 128 * tt + 64 * s2
                                self.mm(self.psf(bo[j], c0, c0 + 64), Rbf[(n - 1) % 2][:, 128 * j:128 * j + 128], q2[:, 64 * n:64 * n + 64], False, True,
                                        reads=[("Rbf", (n - 1) % 2), "q2"], writes=[("ps", bo[j])], skip_group_check=True)
                        if n == 0:
                            self.cp("dve", R, self.psf(bs, 0, 256), reads=[("ps", bs)], writes=["R"])
                        else:
                            self.stt("dve", R, R, dec[:, n - 1:n], self.psf(bs, 0, 256), ALU.mult, ALU.add, reads=["R", "dec", ("ps", bs)], writes=["R"])
                        if n < 31:
                            self.cp("act", Rbf[n % 2], R, reads=["R"], writes=[("Rbf", n % 2)])
                for j in range(2):
                    self.act(sqb[j], self.psf(bo[j]), AF.Square, reads=[("ps", bo[j])], writes=[("sq", j)])
                b2 = self.bank()
                for j in range(2):
                    self.mm(self.psf(b2), self.ones_bf, sqb[j], j == 0, j == 1, reads=[("sq", j)], writes=[("ps", b2)])
                self.act(sd[0], self.psf(b2), AF.Sqrt, reads=[("ps", b2)], writes=[("sd", 0)], bias=self.eps_t[:, 0:1], scale=1.0 / 256)
                self.recip(sd[1], sd[0], reads=[("sd", 0)], writes=[("sd", 1)])
                for j in range(2):
                    self.tt("dve", tmp, self.psf(bo[j]), sd[1], ALU.mult, reads=[("ps", bo[j]), ("sd", 1)], writes=["tmp"])
                    self.tt("dve", ogt[:, j, sl], tmp, rs[:, j, sl], ALU.mult, reads=["tmp", ("rs", j, t)], writes=[("ogt", j, t)])
                self.release(bo)
            self.load_rows(wout[:, 2 * h:2 * h + 2, :], wo_b, 2, og[:, 2 * h:2 * h + 2], "wo_b")
            for dk in range(8):
                for t in range(4):
                    sl = slice(512 * t, 512 * t + 512)
                    b = self.bank()
                    for j in range(2):
                        self.mm(self.psf(b), wo_b[:, j, dk * 128:(dk + 1) * 128], ogt[:, j, sl], j == 0, j == 1,
                                reads=["wo_b", ("ogt", j, t)], writes=[("ps", b)])
                    self.resid_add(b, dk, t)

    def even(self, l):
        S = self.S
        i = l // 2
        S.barrier()
        self.a_reset()
        self.norm_T()
        gain = self.G[:, l, 0:8]
        win = self.dram["ev_w_in"][i].rearrange("(k p) n -> p k n", p=128)
        wout = self.dram["ev_w_out"][i].rearrange("(k p) n -> p k n", p=128)
        qg = self.evq[:, i, 0:1]
        kg = self.evk[:, i, 0:1]
        lng = self.evlng[:, i, :]
        lnb = self.evlnb[:, i, :]
        gT = self.a_bf(4 * 2048).rearrange("p (c n) -> p c n", c=4)
        xhat = self.a_bf(16 * 512).rearrange("p (t n) -> p t n", t=16)
        qf = xhat.rearrange("p t n -> p (t n)").rearrange("p (c n) -> p c n", c=4)
        kf = self.a_bf(4 * 2048).rearrange("p (c n) -> p c n", c=4)
        wsT = self.a_bf(8 * 128).rearrange("p (g n) -> p g n", g=8)
        btab = self.a_f32(4 * 128).rearrange("p (c n) -> p c n", c=4)
        bsT = self.a_f32(4 * 128).rearrange("p (c n) -> p c n", c=4)
        kmbd = self.a_bf(4 * 16).rearrange("p (c n) -> p c n", c=4)
        kmean = self.a_f32(4 * 8).rearrange("p (c n) -> p c n", c=4)
        st1 = self.a_f32(8)
        st2 = self.a_f32(8)
        mean = self.a_f32(8)
        var = self.a_f32(8)
        rstd8 = self.a_f32(8)
        F = [self.a_f32(512), self.a_f32(512)]
        Bt = [self.a_bf(512), self.a_bf(512), self.a_bf(512)]
        gel = self.SD
        sqf, xc = F[0], F[1]
        sqb = self.SQ
        qgb = [Bt[0], Bt[1]]
        qn = Bt[2]
        sd = self.SD
        t1, t2 = F[0], F[1]
        tmpg = F[0]
        cmp_ = F[0]
        pT = Bt
        gsb = self.a_f32(64)
        cnt = self.a_f32(64)
        nm_tok = [self.a_bf(384), self.a_bf(384)]
        rec = self.a_f32(256)
        s = self.stage_ctr % 2
        self.stage_ctr += 1
        st = self.STAGE[s]
        skey = ("stage", s)
        sslot = ("stage", s)
        ws_d = self.dram["ev_w_s"][i]
        self.dma(st[:, 0:1024].rearrange("p (g n) -> p g n", g=8), ws_d.rearrange("g i j -> i g j"), slot=sslot, writes=[skey])
        bs_d = self.dram["ev_b_s"][i]
        for g in range(8):
            c, hf = g // 2, g % 2
            self.dma(bsT[64 * hf:64 * hf + 64, c, :], bs_d[g:g + 1, :].partition_broadcast(64), slot=("misc", 0), writes=["bsT"])
        for g in range(8):
            c, hf = g // 2, g % 2
            b = self.bank()
            self.tr(self.psf(b, 0, 128), st[:, 128 * g:128 * g + 128], self.ident_f, reads=[skey], writes=[("ps", b)])
            self.tt("dve", wsT[:, g, :], self.psf(b, 0, 128), self.C01, ALU.mult, reads=[("ps", b)], writes=[("wsT", g)])
            b2 = self.bank()
            self.mm(self.PS[64 * hf:64 * hf + 64, 512 * b2:512 * b2 + 128], self.ones_bf[:, 0:64], wsT[:, g, :], True, True,
                    reads=[("wsT", g)], writes=[("ps", b2)])
            self.stt("dve", btab[64 * hf:64 * hf + 64, c, :], self.PS[64 * hf:64 * hf + 64, 512 * b2:512 * b2 + 128],
                     lnb[64 * hf:64 * hf + 64, c:c + 1], bsT[64 * hf:64 * hf + 64, c, :], ALU.mult, ALU.add,
                     reads=[("ps", b2), "bsT"], writes=[("btab", g)])

        def slab(c0):
            wb, wk = self.wslab()
            wv = wb.rearrange("p (k n) -> p k n", k=8)
            self.load_cols(win, [(c0, 256)], wv, gain, wk)
            return wv, wk

        for sl2 in range(2):
            wv, wk = slab(2048 + 256 * sl2)
            for tok in range(16):
                b = self.bank()
                for k in range(8):
                    self.mm(self.psf(b, 0, 256), self.hT[:, k, tok * 128:(tok + 1) * 128], wv[:, k, :], k == 0, k == 7,
                            reads=[wk, ("hT", k, tok // 4)], writes=[("ps", b)])
                gi = tok % 2
                ge = gel[gi][:, 0:256]
                self.act(ge, self.psf(b, 0, 256), AF.Gelu, reads=[("ps", b)], writes=[("sd", gi)])
                ge3 = ge.rearrange("p (g d) -> p g d", g=4)
                self.S.op("dve", (lambda o_, i_: lambda e: e.tensor_reduce(o_, i_, AX.X, ALU.add))(st1[:, 0:4], ge3), reads=[("sd", gi)], writes=["st1"])
                self.act(sqf[:, 0:256], ge, AF.Square, reads=[("sd", gi)], writes=[("F", 0)])
                self.S.op("dve", (lambda o_, i_: lambda e: e.tensor_reduce(o_, i_, AX.X, ALU.add))(st2[:, 0:4], sqf[:, 0:256].rearrange("p (g d) -> p g d", g=4)),
                          reads=[("F", 0)], writes=["st2"])
                self.ts("dve", mean[:, 0:4], st1[:, 0:4], 1.0 / 64, None, ALU.mult, None, reads=["st1"], writes=["mean"])
                self.tt("dve", var[:, 0:4], mean[:, 0:4], mean[:, 0:4], ALU.mult, reads=["mean"], writes=["var"])
                self.stt("dve", var[:, 0:4], st2[:, 0:4], 1.0 / 64, var[:, 0:4], ALU.mult, ALU.subtract, reads=["st2", "var"], writes=["var"])
                self.act(rstd8[:, 0:4], var[:, 0:4], AF.Sqrt, reads=["var"], writes=["rstd8"], bias=self.eps_t[:, 0:1], scale=1.0)
                self.recip(rstd8[:, 0:4], rstd8[:, 0:4], reads=["rstd8"], writes=["rstd8"])
                xc3 = xc[:, 0:256].rearrange("p (g d) -> p g d", g=4)
                self.tt("dve", xc3, ge3, mean[:, 0:4].unsqueeze(2).broadcast_to([128, 4, 64]), ALU.subtract, reads=[("sd", gi), "mean"], writes=[("F", 1)])
                self.tt("dve", xhat[:, tok, 256 * sl2:256 * sl2 + 256].rearrange("p (g d) -> p g d", g=4), xc3,
                        rstd8[:, 0:4].unsqueeze(2).broadcast_to([128, 4, 64]), ALU.mult, reads=[("F", 1), "rstd8"], writes=[("xhat", tok)])
        for sl2 in range(2):
            wv, wk = slab(1536 + 256 * sl2)
            for j in range(2):
                c = 2 * sl2 + j
                for t in range(4):
                    sl = slice(512 * t, 512 * t + 512)
                    b = self.bank()
                    for k in range(8):
                        self.mm(self.psf(b), wv[:, k, 128 * j:128 * j + 128], self.hT[:, k, sl], k == 0, k == 7,
                                reads=[wk, ("hT", k, t)], writes=[("ps", b)])
                    self.act(gT[:, c, sl], self.psf(b), AF.Gelu, reads=[("ps", b)], writes=[("gT", c, t)])
        for g in range(8):
            c, hf = g // 2, g % 2
            pr = slice(64 * hf, 64 * hf + 64)
            for t in range(4):
                sl = slice(512 * t, 512 * t + 512)
                b = self.bank()
                for tt in range(4):
                    tok = 4 * t + tt
                    self.mm(self.PS[pr, 512 * b + 128 * tt:512 * b + 128 * tt + 128], xhat[:, tok, 64 * g:64 * g + 64], wsT[:, g, :], True, True,
                            reads=[("xhat", tok), ("wsT", g)], writes=[("ps", b)])
                self.stt("dve", tmpg[pr, :].rearrange("p (t n) -> p t n", t=4), self.PS[pr, 512 * b:512 * b + 512].rearrange("p (t n) -> p t n", t=4),
                         lng[pr, c:c + 1], btab[pr, c, :].unsqueeze(1).broadcast_to([64, 4, 128]), ALU.mult, ALU.add,
                         reads=[("ps", b), ("btab", g)], writes=[("F", 0)])
                self.tt("dve", gT[pr, c, sl], tmpg[pr, :], gT[pr, c, sl], ALU.mult, reads=[("F", 0), ("gT", c, t)], writes=[("gT", c, t)])
        S.barrier()
        for which in range(2):
            dst = qf if which == 0 else kf
            gcol = qg if which == 0 else kg
            name = "qf" if which == 0 else "kf"
            for sl2 in range(2):
                wv, wk = slab(512 * which + 256 * sl2)
                for j in range(2):
                    c = 2 * sl2 + j
                    for t in range(4):
                        sl = slice(512 * t, 512 * t + 512)
                        b = self.bank()
                        for k in range(8):
                            self.mm(self.psf(b), wv[:, k, 128 * j:128 * j + 128], self.hT[:, k, sl], k == 0, k == 7,
                                    reads=[wk, ("hT", k, t)], writes=[("ps", b)])
                        si = t % 2
                        self.act(sqb[si], self.psf(b), AF.Square, reads=[("ps", b)], writes=[("sq", si)])
                        self.act(qgb[si], self.psf(b), AF.Copy, reads=[("ps", b)], writes=[("B", si)])
                        b2 = self.bank()
                        self.mm(self.psf(b2), self.bo_bf, sqb[si], True, True, reads=[("sq", si)], writes=[("ps", b2)])
                        self.act(sd[0], self.psf(b2), AF.Sqrt, reads=[("ps", b2)], writes=[("sd", 0)], bias=self.eps_t[:, 0:1], scale=1.0 / 64)
                        self.recip(sd[1], sd[0], reads=[("sd", 0)], writes=[("sd", 1)])
                        self.stt("dve", qn, qgb[si], gcol, sd[1], ALU.mult, ALU.mult, reads=[("B", si), ("sd", 1)], writes=[("B", 2)])
                        b3 = self.bank()
                        self.mm(self.psf(b3), self.pm_bf, qn, True, True, reads=[("B", 2)], writes=[("ps", b3)])
                        self.tt("pool", t1, qn, self.ropeC[:, sl], ALU.mult, reads=[("B", 2)], writes=[("F", 0)])
                        self.tt("dve", t2, self.psf(b3), self.ropeS[:, sl], ALU.mult, reads=[("ps", b3)], writes=[("F", 1)])
                        for hf in range(2):
                            pr = slice(64 * hf, 64 * hf + 64)
                            self.tt("dve", dst[pr, c, sl], t1[pr, :], t2[pr, :], ALU.add, reads=[("F", 0), ("F", 1)],
                                    writes=[(name, c, hf, 2 * t), (name, c, hf, 2 * t + 1)])
        wv0, wk0 = slab(1024)
        wv1, wk1 = slab(1280)
        for tok in range(16):
            b = self.bank()
            for sl2, (wv, wk) in enumerate(((wv0, wk0), (wv1, wk1))):
                for k in range(8):
                    self.mm(self.psf(b, 256 * sl2, 256 * sl2 + 256), self.hT[:, k, tok * 128:(tok + 1) * 128], wv[:, k, :], k == 0, k == 7,
                            reads=[wk, ("hT", k, tok // 4)], writes=[("ps", b)], skip_group_check=True)
            self.cp("act", self.hT[:, 4:8, tok * 128:(tok + 1) * 128], self.psf(b).rearrange("p (a n) -> p a n", a=4),
                    reads=[("ps", b)], writes=[("hT", 4 + a_, tok // 4) for a_ in range(4)])
        S.barrier()

        def vsl(kc, h):
            c0 = kc * 128 + 64 * (h % 2)
            return self.hT[:, 4 + h // 2, c0:c0 + 64]
        negT = [self.hT.rearrange("p k n -> p (k n)")[:, 2048 * s_:2048 * s_ + 2048] for s_ in range(3)]
        allk = lambda nm, c: [(nm, c, hf, ib) for hf in range(2) for ib in range(8)]
        for c in range(4):
            self.S.op("dve", (lambda o_, i_: lambda e: e.tensor_reduce(o_, i_, AX.X, ALU.add))(kmean[:, c, :], kf[:, c, :].rearrange("p (n k) -> p n k", n=8)),
                      reads=allk("kf", c), writes=[("kmean", c)])
            self.memset("pool", kmbd[:, c, :], 0.0, writes=[("kmbd", c)])
            for hf in range(2):
                pr = slice(64 * hf, 64 * hf + 64)
                self.ts("dve", kmbd[pr, c, 8 * hf:8 * hf + 8], kmean[pr, c, :], 1.0 / 256, None, ALU.mult, None, reads=[("kmean", c)], writes=[("kmbd", c)])
        for tok in range(8, 16):
            o = tok // 2
            bg = self.bank()
            for c in range(4):
                self.mm(self.psf(bg, 16 * c, 16 * c + 16), qf[:, c, tok * 128:(tok + 1) * 128], kmbd[:, c, :], True, True,
                        reads=allk("qf", c) + [("kmbd", c)], writes=[("ps", bg)], skip_group_check=True)
            self.cp("dve", gsb, self.psf(bg, 0, 64), reads=[("ps", bg)], writes=["gsb"])
            g3 = gsb.rearrange("p (h n) -> p h n", h=8)[:, :, 0:o]
            cm4 = cmp_[:, 0:8 * o * o].rearrange("p (h n m) -> p h n m", h=8, n=o)
            self.tt("dve", cm4, g3.unsqueeze(2).broadcast_to([128, 8, o, o]), g3.unsqueeze(3).broadcast_to([128, 8, o, o]), ALU.is_gt,
                    reads=["gsb"], writes=[("F", 0)])
            cn3 = cnt[:, 0:8 * o].rearrange("p (h n) -> p h n", h=8)
            self.S.op("dve", (lambda o_, i_: lambda e: e.tensor_reduce(o_, i_, AX.X, ALU.add))(cn3, cm4), reads=[("F", 0)], writes=["cnt"])
            nm = nm_tok[tok % 2]
            self.memset("pool", nm, 0.0, writes=[("nm", tok % 2)])
            for s_ in range(3):
                ng = 3 if s_ < 2 else 2
                self.ts("dve", nm[:, 128 * s_:128 * s_ + 96].rearrange("p (g n) -> p g n", n=32)[:, 0:ng, 0:o], cn3[:, 3 * s_:3 * s_ + ng, :],
                        3.0, NEG, ALU.is_ge, ALU.mult, reads=["cnt"], writes=[("nm", tok % 2)])
            bt = self.bank()
            pb = self.psb(bt)
            for s_ in range(3):
                self.tr(pb[:, 128 * s_:128 * s_ + 128], nm[:, 128 * s_:128 * s_ + 128], self.ident_bf, reads=[("nm", tok % 2)], writes=[("ps", bt)])
            for s_ in range(3):
                self.cp("act", negT[s_][:, tok * 128:(tok + 1) * 128], pb[:, 128 * s_:128 * s_ + 128], reads=[("ps", bt)], writes=[("negT", s_, tok)])
        scale = 0.125
        for h in range(8):
            c, hf = h // 2, h % 2
            pr = slice(64 * hf, 64 * hf + 64)
            s_, g_ = h // 3, h % 3
            gr = slice(32 * g_, 32 * g_ + 32)
            for ib in range(8):
                qsl = slice(256 * ib, 256 * ib + 256)
                ba, bd = self.reserve(2)
                acc = self.PS[pr, 512 * ba:512 * ba + 256]
                den = self.PS[pr, 512 * bd:512 * bd + 256]
                for jb in range(ib + 1):
                    b = self.bank()
                    pi = (h * 64 + ib * 8 + jb) % 3
                    kc0, kc1 = 2 * jb, 2 * jb + 1
                    if jb < ib:
                        self.mm(self.psf(b, 0, 256), kf[pr, c, kc0 * 128:kc0 * 128 + 128], qf[pr, c, qsl], True, False,
                                reads=[("kf", c, hf, jb), ("qf", c, hf, ib)], writes=[("ps", b)], skip_group_check=True)
                        self.mm(self.psf(b, 256, 512), kf[pr, c, kc1 * 128:kc1 * 128 + 128], qf[pr, c, qsl], False, ib <= 3,
                                reads=[("kf", c, hf, jb), ("qf", c, hf, ib)], writes=[("ps", b)], skip_group_check=True)
                        if ib > 3:
                            self.mm(self.psf(b), self.e8_bf[gr, jb, :],
                                    negT[s_][gr, qsl].unsqueeze(1).broadcast_to([32, 2, 256]), False, True,
                                    reads=[("negT", s_, 2 * ib), ("negT", s_, 2 * ib + 1)], writes=[("ps", b)], skip_group_check=True)
                        w = 512
                    else:
                        self.mm(self.psf(b, 0, 256), kf[pr, c, kc0 * 128:kc0 * 128 + 128], qf[pr, c, qsl], True, False,
                                reads=[("kf", c, hf, jb), ("qf", c, hf, ib)], writes=[("ps", b)], skip_group_check=True)
                        self.mm(self.psf(b, 256, 384), kf[pr, c, kc1 * 128:kc1 * 128 + 128], qf[pr, c, 256 * ib + 128:256 * ib + 256], False, False,
                                reads=[("kf", c, hf, jb), ("qf", c, hf, ib)], writes=[("ps", b)], skip_group_check=True)
                        self.mm(self.psf(b, 0, 128), self.ident_bf, self.cn_bf, False, False,
                                reads=[], writes=[("ps", b)], skip_group_check=True)
                        self.mm(self.psf(b, 256, 384), self.ident_bf, self.cn_bf, False, True,
                                reads=[], writes=[("ps", b)], skip_group_check=True)
                        w = 384
                    self.act(pT[pi][:, 0:w], self.psf(b, 0, w), AF.Exp, reads=[("ps", b)], writes=[("B", pi)], scale=scale)
                    last = (jb == ib)
                    self.mm(acc, vsl(kc0, h), pT[pi][:, 0:256], jb == 0, False,
                            reads=[("B", pi)], writes=[("ps", ba)], skip_group_check=True)
                    self.mm(den, self.ones_bf[:, 0:64], pT[pi][:, 0:256], jb == 0, False,
                            reads=[("B", pi)], writes=[("ps", bd)], skip_group_check=True)
                    if not last:
                        self.mm(acc, vsl(kc1, h), pT[pi][:, 256:512], False, False,
                                reads=[("B", pi)], writes=[("ps", ba)], skip_group_check=True)
                        self.mm(den, self.ones_bf[:, 0:64], pT[pi][:, 256:512], False, False,
                                reads=[("B", pi)], writes=[("ps", bd)], skip_group_check=True)
                    else:
                        self.mm(self.PS[pr, 512 * ba + 128:512 * ba + 256], vsl(kc1, h), pT[pi][:, 256:384], False, True,
                                reads=[("B", pi)], writes=[("ps", ba)], skip_group_check=True)
                        self.mm(self.PS[pr, 512 * bd + 128:512 * bd + 256], self.ones_bf[:, 0:64], pT[pi][:, 256:384], False, True,
                                reads=[("B", pi)], writes=[("ps", bd)], skip_group_check=True)
                self.recip(rec[pr, :], den, reads=[("ps", bd)], writes=[("rec", hf)])
                self.tt("dve", qf[pr, c, qsl], acc, rec[pr, :], ALU.mult, reads=[("ps", ba), ("rec", hf)], writes=[("qf", c, hf, ib)])
                self.release([ba, bd])
        for cg in range(4):
            wb, wk = self.wslab()
            wv = wb.rearrange("p (k n) -> p k n", k=8)
            self.load_cols(wout, [(cg * 256, 256)], wv, None, wk)
            for j in range(2):
                dk = cg * 2 + j
                for t in range(4):
                    sl = slice(512 * t, 512 * t + 512)
                    b = self.bank()
                    for k in range(8):
                        if k < 4:
                            rhs = qf[:, k, sl]
                            rk = [("qf", k, hf, ib) for hf in range(2) for ib in (2 * t, 2 * t + 1)]
                        else:
                            rhs = gT[:, k - 4, sl]
                            rk = [("gT", k - 4, t)]
                        self.mm(self.psf(b), wv[:, k, j * 128:(j + 1) * 128], rhs, k == 0, k == 7, reads=[wk] + rk, writes=[("ps", b)])
                    self.resid_add(b, dk, t)

    def build(self):
        nc = self.nc
        x_d = self.din("x", [S_LEN, D])
        mem_d = self.din("mem", [NMEM, D])
        pos_d = self.din("positions", [1, S_LEN], I32)
        cst_d = self.din("consts", [128, C_END])
        G_d = self.din("gains", [128, DEPTH * 32])
        xqg_d = self.din("xa_qg", [128, DEPTH * 2])
        xkg_d = self.din("xa_kg", [128, DEPTH * 2])
        evq_d = self.din("ev_qg", [128, 2])
        evk_d = self.din("ev_kg", [128, 2])
        evlng_d = self.din("ev_lng", [128, 8])
        evlnb_d = self.din("ev_lnb", [128, 8])
        og_d = self.din("od_og", [128, 16])
        self.din("ev_w_in", [2, D, 2560]); self.din("ev_w_s", [2, 8, 128, 128]); self.din("ev_b_s", [2, 8, 128])
        self.din("ev_w_out", [2, D, D]); self.din("od_w_in", [2, D, 3072]); self.din("od_w_g1", [2, D, 16])
        self.din("od_w_g2", [2, 16, 512]); self.din("od_b_g", [2, 512]); self.din("od_w_out", [2, D, D])
        self.din("xa_w_q", [DEPTH, D, D]); self.din("xa_w_kv", [DEPTH, D, 2 * D]); self.din("xa_w_out", [DEPTH, D, D])
        self.din("ffn_w_gu", [DEPTH, D, 2 * DFF]); self.din("ffn_w_down", [DEPTH, DFF, D])
        out_d = nc.dram_tensor("out", [S_LEN, D], F32, kind="ExternalOutput").ap()

        with ExitStack() as es:
            def sb(name, shape, dt=F32):
                return es.enter_context(nc.sbuf_tensor(name, shape, dt))[:]
            self.S = S = Sched(nc)
            self.PS = es.enter_context(nc.psum_tensor("ps", [128, 4096], F32))[:]
            self.xT = sb("xT", [128, 8 * 2048]).rearrange("p (k n) -> p k n", k=8)
            self.hT = sb("hT", [128, 8 * 2048], BF16).rearrange("p (k n) -> p k n", k=8)
            self.memT = sb("memT", [128, 8 * 256], BF16).rearrange("p (k n) -> p k n", k=8)
            cf = sb("cf", [128, 386])
            self.G = sb("G", [128, DEPTH * 32]).rearrange("p (l n) -> p l n", l=DEPTH)
            self.xqg = sb("xqg", [128, DEPTH * 2]).rearrange("p (l n) -> p l n", l=DEPTH)
            self.xkg = sb("xkg", [128, DEPTH * 2]).rearrange("p (l n) -> p l n", l=DEPTH)
            self.evq = sb("evq", [128, 2]).rearrange("p (l n) -> p l n", l=2)
            self.evk = sb("evk", [128, 2]).rearrange("p (l n) -> p l n", l=2)
            self.evlng = sb("evlng", [128, 8]).rearrange("p (l n) -> p l n", l=2)
            self.evlnb = sb("evlnb", [128, 8]).rearrange("p (l n) -> p l n", l=2)
            self.ogain = sb("ogain", [128, 16]).rearrange("p (l n) -> p l n", l=2)
            self.ident_f = cf[:, 0:128]
            self.C01 = cf[:, 128:256]
            self.U01 = cf[:, 256:384]
            invf_t = cf[:, 384:385]
            sgn_t = cf[:, 385:386]
            cbf = sb("cbf", [128, 7 * 128 + 1024], BF16)
            self.ident_bf = cbf[:, 0:128]
            self.bo_bf = cbf[:, 128:256]
            self.pm_bf = cbf[:, 256:384]
            self.cn_bf = cbf[:, 384:512]
            self.U_bf = cbf[:, 640:768]
            self.ones_bf = cbf[:, 768:896]
            self.e8_bf = cbf[:, 896:1920].rearrange("p (n m) -> p n m", n=8)
            self.eps_t = sb("eps_t", [128, 1])
            self.one_t = sb("one_t", [128, 1])
            self.ropeC = sb("ropeC", [128, 2048], BF16)
            self.ropeS = sb("ropeS", [128, 2048], BF16)
            self.STAGE = [sb("stage0", [128, 1024]), sb("stage1", [128, 1024])]
            self.WB = [sb("wb%d" % i, [128, 2048], BF16) for i in range(3)]
            self.SQ = [sb("sq%d" % i, [128, 512], BF16) for i in range(2)]
            self.SD = [sb("sd%d" % i, [128, 512]) for i in range(2)]
            self.A_WORDS = (nc.sbuf_bytes_remaining // 4) - 64
            self.ARENA = sb("arena", [128, self.A_WORDS])

            self.a_reset()
            cst = self.a_f32(C_END)
            self.dma(cst, cst_d, slot=("c", 0), writes=["cst"])
            self.dma(self.G.rearrange("p l n -> p (l n)"), G_d, slot=("c", 0), writes=["G"])
            self.dma(self.xqg.rearrange("p l n -> p (l n)"), xqg_d, slot=("c", 0), writes=["G"])
            self.dma(self.xkg.rearrange("p l n -> p (l n)"), xkg_d, slot=("c", 0), writes=["G"])
            self.dma(self.evq.rearrange("p l n -> p (l n)"), evq_d, slot=("c", 0), writes=["G"])
            self.dma(self.evk.rearrange("p l n -> p (l n)"), evk_d, slot=("c", 0), writes=["G"])
            self.dma(self.evlng.rearrange("p l n -> p (l n)"), evlng_d, slot=("c", 0), writes=["G"])
            self.dma(self.evlnb.rearrange("p l n -> p (l n)"), evlnb_d, slot=("c", 0), writes=["G"])
            self.dma(self.ogain.rearrange("p l n -> p (l n)"), og_d, slot=("c", 0), writes=["G"])
            self.cp("dve", cbf[:, 0:1920], cst[:, 0:1920], reads=["cst"], writes=["cbf"])
            self.cp("dve", cf[:, 0:128], cst[:, C_ID:C_ID + 128], reads=["cst"], writes=["cf"])
            self.cp("dve", cf[:, 128:384], cst[:, C_C01:C_C01 + 256], reads=["cst"], writes=["cf"])
            self.cp("dve", cf[:, 384:386], cst[:, C_INVF:C_INVF + 2], reads=["cst"], writes=["cf"])
            self.memset("dve", self.eps_t, EPS, writes=["eps"])
            self.memset("dve", self.one_t, 1.0, writes=["eps"])
            S.barrier()

            self.a_reset()
            posi = self.a_f32(2048).bitcast(I32)
            posf = self.a_f32(2048)
            tq = self.a_f32(2048)
            tn_i = self.a_f32(2048).bitcast(I32)
            tn = self.a_f32(2048)
            self.dma(posi, pos_d[0:1, :].partition_broadcast(128), slot=("c", 1), writes=["posi"])
            self.cp("dve", posf, posi, reads=["posi"], writes=["posf"])
            inv2pi = float(1.0 / (2.0 * np.pi))
            for which in range(2):
                self.ts("dve", tq, posf, invf_t, None, ALU.mult, None, reads=["posf"], writes=["tq"])
                self.ts("dve", tq, tq, inv2pi, 0.25 if which == 0 else 0.0, ALU.mult, ALU.add, reads=["tq"], writes=["tq"])
                self.cp("dve", tn_i, tq, reads=["tq"], writes=["tn_i"])
                self.cp("dve", tn, tn_i, reads=["tn_i"], writes=["tn"])
                self.tt("dve", tq, tq, tn, ALU.subtract, reads=["tq", "tn"], writes=["tq"])
                self.ts("dve", tq, tq, -0.4999, 0.4999, ALU.max, ALU.min, reads=["tq"], writes=["tq"])
                if which == 0:
                    self.act(self.ropeC, tq, AF.Sin, reads=["tq"], writes=["ropeC"], scale=float(2.0 * np.pi))
                else:
                    self.act(tn, tq, AF.Sin, reads=["tq"], writes=["tn"], scale=float(2.0 * np.pi))
                    self.ts("dve", self.ropeS, tn, sgn_t, None, ALU.mult, None, reads=["tn"], writes=["ropeS"])
            S.barrier()

            self.a_reset()
            mt_f = self.a_f32(1024)
            mt_j = self.a_f32(1024)
            mt_b = self.a_bf(1024)
            mss = self.a_f32(2)
            for mt in range(2):
                self.dma(mt_f, mem_d[mt * 128:(mt + 1) * 128, :], slot=("c", 2), writes=["mt_f"])
                self.act(mt_j, mt_f, AF.Square, reads=["mt_f"], writes=["mt_j", "mss"], accum_out=mss[:, 0:1])
                self.act(mss[:, 1:2], mss[:, 0:1], AF.Sqrt, reads=["mss"], writes=["mss2"], bias=self.eps_t[:, 0:1], scale=1.0 / D)
                self.recip(mss[:, 1:2], mss[:, 1:2], reads=["mss2"], writes=["mss2"])
                self.ts("dve", mt_b, mt_f, mss[:, 1:2], None, ALU.mult, None, reads=["mt_f", "mss2"], writes=["mt_b"])
                for kk in range(0, 8, 4):
                    b = self.bank()
                    pb = self.psb(b)
                    for k in range(kk, kk + 4):
                        self.tr(pb[:, 128 * (k - kk):128 * (k - kk) + 128], mt_b[:, 128 * k:128 * k + 128], self.ident_bf, reads=["mt_b"], writes=[("ps", b)])
                    self.cp("dve", self.memT[:, kk:kk + 4, mt * 128:(mt + 1) * 128], pb[:, 0:512].rearrange("p (k n) -> p k n", k=4),
                            reads=[("ps", b)], writes=["memT"])
            S.barrier()

            self.a_reset()
            xs = [self.a_f32(1024) for _ in range(4)]
            for t in range(4):
                for tt in range(4):
                    tok = 4 * t + tt
                    self.dma(xs[tt], x_d[tok * 128:(tok + 1) * 128, :], slot=("xs", tt), writes=[("xs", tt)])
                for k in range(8):
                    b = self.bank()
                    for tt in range(4):
                        self.tr(self.psf(b, 128 * tt, 128 * tt + 128), xs[tt][:, 128 * k:128 * k + 128], self.ident_f, reads=[("xs", tt)], writes=[("ps", b)])
                    self.cp("dve" if k % 2 == 0 else "act", self.xT[:, k, 512 * t:512 * t + 512], self.psf(b), reads=[("ps", b)], writes=[("xT", k, t)])

            for l in self.layers:
                if "mix" in self.subs:
                    if l % 2 == 0:
                        self.even(l)
                    else:
                        self.gla(l)
                if "xa" in self.subs:
                    self.xattn(l)
                if "ffn" in self.subs:
                    self.ffn(l)

            S.barrier()
            self.a_reset()
            os_ = [self.a_f32(1024) for _ in range(2)]
            stores = []
            for tok in range(16):
                ob = os_[tok % 2]
                t = tok // 4
                for kk in range(0, 8, 4):
                    b = self.bank()
                    for k in range(kk, kk + 4):
                        self.tr(self.psf(b, 128 * (k - kk), 128 * (k - kk) + 128), self.xT[:, k, tok * 128:(tok + 1) * 128], self.ident_f,
                                reads=[("xT", k, t)], writes=[("ps", b)])
                    self.cp("dve" if kk == 0 else "act", ob[:, 128 * kk:128 * kk + 512], self.psf(b), reads=[("ps", b)], writes=[("os", tok % 2, kk)])
                stores.append(self.dma(out_d[tok * 128:(tok + 1) * 128, :], ob, slot=("out", tok % 2), reads=[("os", tok % 2, 0), ("os", tok % 2, 4)]))
            S.final_wait("sp", stores)
            S.finalize(es)
            block = es.enter_context(nc.Block())
            S.emit(block)
        return nc


def _pk(v, n):
    v = np.asarray(v, np.float32)
    lead = v.shape[:-1]
    a = v.reshape(lead + (n, 128))
    a = np.moveaxis(a, -1, 0)
    return np.ascontiguousarray(a.reshape(128, -1))


def _host_inputs(inp):
    f = lambda k: np.ascontiguousarray(np.asarray(inp[k], np.float32))
    gains = np.concatenate([_pk(inp["norm_mix"], 8).reshape(128, DEPTH, 8), _pk(inp["norm_mem_q"], 8).reshape(128, DEPTH, 8),
                            _pk(inp["norm_mem_kv"], 8).reshape(128, DEPTH, 8), _pk(inp["norm_ffn"], 8).reshape(128, DEPTH, 8)], axis=2)
    shared = {
        "consts": _consts(),
        "gains": np.ascontiguousarray(gains.reshape(128, DEPTH * 32)),
        "xa_qg": _pk(inp["xa_q_gain"], 2),
        "xa_kg": _pk(inp["xa_k_gain"], 2),
        "ev_qg": np.ascontiguousarray(np.tile(np.asarray(inp["ev_q_gain"], np.float32), (1, 2)).T),
        "ev_kg": np.ascontiguousarray(np.tile(np.asarray(inp["ev_k_gain"], np.float32), (1, 2)).T),
        "ev_lng": _pk(np.asarray(inp["ev_ln_g"], np.float32).reshape(2, 512), 4),
        "ev_lnb": _pk(np.asarray(inp["ev_ln_b"], np.float32).reshape(2, 512), 4),
        "od_og": _pk(np.asarray(inp["od_o_gain"], np.float32).reshape(2, 1024), 8),
    }
    for k in ("ev_w_in", "ev_w_s", "ev_b_s", "ev_w_out", "od_w_in", "od_w_g1", "od_w_g2", "od_b_g", "od_w_out",
              "xa_w_q", "xa_w_kv", "xa_w_out", "ffn_w_gu", "ffn_w_down"):
        shared[k] = f(k)
    x = f("x")
    mem = f("mem")
    pos = np.ascontiguousarray(np.asarray(inp["positions"], np.int32))
    maps = []
    for b in range(8):
        m = dict(shared)
        m["x"] = x[b]
        m["mem"] = mem[b]
        m["positions"] = pos[b:b + 1]
        maps.append(m)
    return maps


_NC_CACHE = {}


def run_layers(inp, layers, subs=("mix", "xa", "ffn"), cores=8, stop_at=0):
    key = (tuple(layers), tuple(subs), stop_at)
    if key not in _NC_CACHE:
        bld = Builder(layers, subs)
        bld.stop_at = stop_at
        _NC_CACHE[key] = bld.build()
    nc = _NC_CACHE[key]
    maps = _host_inputs(inp)[:cores]
    res = run_bass_kernel_spmd(nc, maps, core_ids=list(range(cores)))
    return np.stack([np.asarray(r["out"], np.float32) for r in res.results], axis=0)


def kernel(**inputs):
    return run_layers(inputs, list(range(DEPTH)))
```

```python
import numpy as np
from contextlib import ExitStack
import concourse.bass as bass
import concourse.mybir as mybir
from concourse.bass_utils import run_bass_kernel_spmd

F32 = mybir.dt.float32
BF16 = mybir.dt.bfloat16
I32 = mybir.dt.int32
AF = mybir.ActivationFunctionType
ALU = mybir.AluOpType
AX = mybir.AxisListType

S_LEN = 2048
D = 1024
NMEM = 256
DEPTH = 4
DFF = 2816
EPS = 1e-6
NEG = -30000.0
ENGS = ("pe", "act", "dve", "pool", "sp")


class _Op:
    __slots__ = ("eng", "fn", "deps", "is_dma", "slot", "ticket", "needs_inc", "waits")

    def __init__(self, eng, fn, is_dma, slot):
        self.eng = eng
        self.fn = fn
        self.deps = ()
        self.is_dma = is_dma
        self.slot = slot
        self.ticket = None
        self.needs_inc = False
        self.waits = None


class Sched:
    def __init__(self, nc):
        self.nc = nc
        self.ops = {e: [] for e in ENGS}
        self.last_w = {}
        self.readers = {}
        self.all_ops = []
        self.slot_last = {}

    def op(self, eng, fn, reads=(), writes=(), dma_slot=None):
        o = _Op(eng, fn, dma_slot is not None, dma_slot)
        if dma_slot is not None:
            self.slot_last[dma_slot] = o
        deps = set()
        for r in reads:
            w = self.last_w.get(r)
            if w is not None:
                deps.add(w)
        for r in writes:
            w = self.last_w.get(r)
            if w is not None:
                deps.add(w)
            rl = self.readers.get(r)
            if rl:
                deps.update(rl)
        deps = set((self.slot_last[d.slot] if d.is_dma else d) for d in deps)
        deps.discard(o)
        o.deps = tuple(deps)
        for r in reads:
            self.readers.setdefault(r, []).append(o)
        for r in writes:
            self.last_w[r] = o
            self.readers[r] = []
        self.all_ops.append(o)
        self.ops[eng].append(o)
        return o

    def barrier(self):
        lasts = []
        for e in ENGS:
            for o in reversed(self.ops[e]):
                if not o.is_dma and o.fn is not None:
                    lasts.append(o)
                    break
        slots = {}
        for o in self.all_ops:
            if o.is_dma:
                slots[o.slot] = o
        lasts.extend(slots.values())
        for e in ENGS:
            b = _Op(e, None, False, None)
            b.deps = tuple(lasts)
            self.all_ops.append(b)
            self.ops[e].append(b)
        self.last_w = {}
        self.readers = {}

    def final_wait(self, eng, ops):
        b = _Op(eng, None, False, None)
        b.deps = tuple(ops)
        self.all_ops.append(b)
        self.ops[eng].append(b)

    @staticmethod
    def _skip(d, o):
        return (not d.is_dma) and d.eng == o.eng and d.eng == "pe"

    def finalize(self, es):
        nc = self.nc
        for o in self.all_ops:
            for d in o.deps:
                if d.is_dma or self._skip(d, o):
                    continue
                d.needs_inc = True
        self.sems = {e: es.enter_context(nc.semaphore("prog_" + e)) for e in ENGS}
        cnt = {e: 0 for e in ENGS}
        slot_cnt = {}
        self.slot_sems = {}
        for o in self.all_ops:
            if o.is_dma:
                if o.slot not in self.slot_sems:
                    self.slot_sems[o.slot] = es.enter_context(nc.semaphore("dma_%d" % len(self.slot_sems)))
                    slot_cnt[o.slot] = 0
                slot_cnt[o.slot] += 16
                o.ticket = slot_cnt[o.slot]
            elif o.needs_inc:
                cnt[o.eng] += 1
                o.ticket = cnt[o.eng]
        waited = {e: {} for e in ENGS}
        for o in self.all_ops:
            need = {}
            for d in o.deps:
                if d.is_dma:
                    key = ("slot", d.slot)
                else:
                    if self._skip(d, o):
                        continue
                    key = ("eng", d.eng)
                if need.get(key, 0) < d.ticket:
                    need[key] = d.ticket
            w = []
            wd = waited[o.eng]
            for key, v in need.items():
                if wd.get(key, 0) >= v:
                    continue
                wd[key] = v
                w.append((key, v))
            o.waits = w

    def _sem(self, key):
        return self.sems[key[1]] if key[0] == "eng" else self.slot_sems[key[1]]

    def emit(self, block):
        def body(engname):
            def f(e):
                for o in self.ops[engname]:
                    for key, v in o.waits:
                        e.wait_ge(self._sem(key), v)
                    if o.fn is None:
                        continue
                    ins = o.fn(e)
                    if o.is_dma:
                        ins.then_inc(self.slot_sems[o.slot], 16)
                    elif o.needs_inc:
                        ins.then_inc(self.sems[engname], 1)
            return f
        block.tensor(body("pe"))
        block.scalar(body("act"))
        block.vector(body("dve"))
        block.gpsimd(body("pool"))
        block.sync(body("sp"))


C_ID, C_BO, C_PM, C_CN, C_C01, C_U01, C_ONE, C_E8, C_INVF, C_SGN, C_END = (
    0, 128, 256, 384, 512, 640, 768, 896, 1920, 1921, 1922)


def _consts():
    c = np.zeros((128, C_END), np.float32)
    p = np.arange(128)
    c[:, C_ID:C_ID + 128] = np.eye(128, dtype=np.float32)
    c[:, C_BO:C_BO + 128] = (p[:, None] // 64 == p[None, :] // 64).astype(np.float32)
    pm = np.zeros((128, 128), np.float32)
    for m in range(128):
        r = m % 64
        if r < 8:
            pm[m + 8, m] = 1.0
        elif r < 16:
            pm[m - 8, m] = 1.0
    c[:, C_PM:C_PM + 128] = pm
    c[:, C_CN:C_CN + 128] = np.where(p[:, None] <= p[None, :], 0.0, NEG)
    c[:, C_C01:C_C01 + 128] = (p[:, None] <= p[None, :]).astype(np.float32)
    c[:, C_U01:C_U01 + 128] = ((p[:, None] <= p[None, :]) & (p[:, None] // 64 == p[None, :] // 64)).astype(np.float32)
    c[:, C_ONE:C_ONE + 128] = 1.0
    e8 = np.zeros((128, 8, 128), np.float32)
    for n in range(8):
        e8[(p % 32) == n, n, :] = 1.0
    c[:, C_E8:C_E8 + 1024] = e8.reshape(128, 1024)
    half = 8
    inv_freq = (np.float32(500000.0) ** (-np.arange(half, dtype=np.float32) * np.float32(2.0) / np.float32(16))).astype(np.float32)
    r = p % 64
    c[:, C_INVF] = np.where(r < 16, inv_freq[r % 8], 0.0)
    c[:, C_SGN] = np.where(r < 8, -1.0, np.where(r < 16, 1.0, 0.0))
    return c


class Builder:
    def __init__(self, layers, subs=("mix", "xa", "ffn")):
        self.layers = list(layers)
        self.subs = subs
        self.nc = bass.Bass("TRN2", target_bir_lowering=False)
        self.dram = {}
        self.bank_ctr = 0
        self.reserved = set()
        self.conv_dve = False
        self.stop_at = 0
        self.stage_ctr = 0
        self.wb_ctr = 0
        self.uid = 0

    def din(self, name, shape, dt=F32):
        t = self.nc.dram_tensor(name, list(shape), dt, kind="ExternalInput").ap()
        self.dram[name] = t
        return t

    def bank(self):
        while True:
            b = self.bank_ctr % 8
            self.bank_ctr += 1
            if b not in self.reserved:
                return b

    def reserve(self, n):
        out = []
        for _ in range(n):
            b = self.bank()
            self.reserved.add(b)
            out.append(b)
        return out

    def release(self, banks):
        for b in banks:
            self.reserved.discard(b)

    def psf(self, b, lo=0, hi=512):
        return self.PS[:, 512 * b + lo:512 * b + hi]

    def psb(self, b):
        return self.PS[:, 512 * b:512 * b + 512].bitcast(BF16)

    def key(self, name):
        self.uid += 1
        return (name, self.uid)

    def mm(self, out, lhsT, rhs, start, stop, reads, writes, **kw):
        return self.S.op("pe", lambda e: e.matmul(out, lhsT, rhs, start=start, stop=stop, **kw), reads=reads, writes=writes)

    def tr(self, out, in_, ident, reads, writes):
        return self.S.op("pe", lambda e: e.transpose(out, in_, ident), reads=reads, writes=writes)

    def act(self, out, in_, func, reads, writes, **kw):
        return self.S.op("act", lambda e: e.activation(out=out, in_=in_, func=func, **kw), reads=reads, writes=writes)

    def tt(self, eng, out, in0, in1, op, reads, writes):
        return self.S.op(eng, lambda e: e.tensor_tensor(out, in0, in1, op), reads=reads, writes=writes)

    def ts(self, eng, out, in0, s1, s2, op0, op1, reads, writes):
        if op1 is None:
            return self.S.op(eng, lambda e: e.tensor_scalar(out, in0, s1, None, op0), reads=reads, writes=writes)
        return self.S.op(eng, lambda e: e.tensor_scalar(out, in0, s1, s2, op0, op1), reads=reads, writes=writes)

    def stt(self, eng, out, in0, scalar, in1, op0, op1, reads, writes):
        return self.S.op(eng, lambda e: e.scalar_tensor_tensor(out, in0, scalar, in1, op0, op1), reads=reads, writes=writes)

    def cp(self, eng, out, in_, reads, writes):
        if eng == "act":
            return self.S.op("act", lambda e: e.copy(out, in_), reads=reads, writes=writes)
        return self.S.op(eng, lambda e: e.tensor_copy(out, in_), reads=reads, writes=writes)

    def recip(self, out, in_, reads, writes):
        return self.S.op("dve", lambda e: e.reciprocal(out, in_), reads=reads, writes=writes)

    def memset(self, eng, ap, val, writes):
        return self.S.op(eng, lambda e: e.memset(ap, val), writes=writes)

    def dma(self, out, in_, slot, reads=(), writes=()):
        return self.S.op("sp", lambda e: e.dma_start(out=out, in_=in_), reads=reads, writes=writes, dma_slot=slot)

    def a_reset(self):
        self.a_off = 0

    def a_f32(self, n):
        ap = self.ARENA[:, self.a_off:self.a_off + n]
        self.a_off += n
        assert self.a_off <= self.A_WORDS, ("arena overflow", self.a_off, self.A_WORDS)
        return ap

    def a_bf(self, n):
        w = (n + 1) // 2
        return self.a_f32(w).bitcast(BF16)

    def _conv(self, i, dst, src, g, skey, wkey):
        e = ("act", "dve")[i % 2]
        if g is None:
            self.cp(e, dst, src, reads=[skey], writes=[wkey])
        elif e == "act":
            self.S.op("act", lambda en: en.activation(out=dst, in_=src, func=AF.Copy, scale=g), reads=[skey], writes=[wkey])
        else:
            self.ts(e, dst, src, g, None, ALU.mult, None, reads=[skey], writes=[wkey])

    def load_cols(self, wsrc, ranges, dst, gain, wkey):
        for kh in range(2):
            s = self.stage_ctr % 2
            self.stage_ctr += 1
            st = self.STAGE[s].rearrange("p (k n) -> p k n", k=4)
            skey = ("stage", s)
            off = 0
            for (c0, w) in ranges:
                self.dma(st[:, :, off:off + w], wsrc[:, 4 * kh:4 * kh + 4, c0:c0 + w], slot=("stage", s), writes=[skey])
                off += w
            for kk in range(4):
                k = 4 * kh + kk
                g = None if gain is None else gain[:, k:k + 1]
                self._conv(k, dst[:, k, :], st[:, kk, :], g, skey, wkey)

    def load_rows(self, wsrc, dst, n, gain, wkey):
        for i in range(n):
            s = self.stage_ctr % 2
            self.stage_ctr += 1
            st = self.STAGE[s]
            skey = ("stage", s)
            self.dma(st, wsrc[:, i, :], slot=("stage", s), writes=[skey])
            g = None if gain is None else gain[:, i:i + 1]
            for q in range(2):
                self._conv(2 * i + q, dst[:, i, 512 * q:512 * q + 512], st[:, 512 * q:512 * q + 512], g, skey, wkey)

    def wslab(self):
        i = self.wb_ctr % 3
        self.wb_ctr += 1
        return self.WB[i], ("wb", i)

    def norm_T(self):
        S = self.S
        for t in range(4):
            sl = slice(512 * t, 512 * t + 512)
            b = self.bank()
            for k in range(8):
                sq = self.SQ[k % 2]
                self.act(sq, self.xT[:, k, sl], AF.Square, reads=[("xT", k, t)], writes=[("sq", k % 2)])
                self.mm(self.psf(b), self.ones_bf, sq, k == 0, k == 7, reads=[("sq", k % 2)], writes=[("ps", b)])
            self.act(self.SD[0], self.psf(b), AF.Sqrt, reads=[("ps", b)], writes=[("sd", 0)], bias=self.eps_t[:, 0:1], scale=1.0 / D)
            self.recip(self.SD[1], self.SD[0], reads=[("sd", 0)], writes=[("sd", 1)])
            for k in range(8):
                self.tt("dve", self.hT[:, k, sl], self.xT[:, k, sl], self.SD[1], ALU.mult,
                        reads=[("xT", k, t), ("sd", 1)], writes=[("hT", k, t)])

    def resid_add(self, b, k, t):
        sl = slice(512 * t, 512 * t + 512)
        self.tt("dve", self.xT[:, k, sl], self.psf(b), self.xT[:, k, sl], ALU.add,
                reads=[("ps", b), ("xT", k, t)], writes=[("xT", k, t)])

    def ffn(self, l):
        self.S.barrier()
        self.a_reset()
        self.norm_T()
        gain = self.G[:, l, 24:32]
        wgu = self.dram["ffn_w_gu"][l].rearrange("(k p) n -> p k n", p=128)
        wdn = self.dram["ffn_w_down"][l].rearrange("(c p) n -> p c n", p=128)
        parts = [(0, 6), (6, 12), (12, 17), (17, 22)]
        actT = self.a_bf(6 * 2048).rearrange("p (c n) -> p c n", c=6)
        wd = self.a_bf(6 * 1024).rearrange("p (c n) -> p c n", c=6)
        sg = [self.a_f32(512), self.a_f32(512)]
        for (c0, c1) in parts:
            self.load_rows(wdn[:, c0:c1, :], wd, c1 - c0, None, "wd")
            for c in range(c0, c1):
                wb, wk = self.wslab()
                wv = wb.rearrange("p (k n) -> p k n", k=8)
                self.load_cols(wgu, [(c * 128, 128), (DFF + c * 128, 128)], wv, gain, wk)
                for t in range(4):
                    sl = slice(512 * t, 512 * t + 512)
                    bg, bu = self.bank(), self.bank()
                    for k in range(8):
                        self.mm(self.psf(bg), wv[:, k, 0:128], self.hT[:, k, sl], k == 0, k == 7, reads=[wk, ("hT", k, t)], writes=[("ps", bg)])
                    for k in range(8):
                        self.mm(self.psf(bu), wv[:, k, 128:256], self.hT[:, k, sl], k == 0, k == 7, reads=[wk, ("hT", k, t)], writes=[("ps", bu)])
                    si = (c * 4 + t) % 2
                    self.act(sg[si], self.psf(bg), AF.Silu, reads=[("ps", bg)], writes=[("sg", si)])
                    self.tt("dve", actT[:, c - c0, sl], sg[si], self.psf(bu), ALU.mult, reads=[("sg", si), ("ps", bu)], writes=[("actT", c - c0, t)])
            nchunk = c1 - c0
            for dk in range(8):
                for t in range(4):
                    sl = slice(512 * t, 512 * t + 512)
                    b = self.bank()
                    for j in range(nchunk):
                        self.mm(self.psf(b), wd[:, j, dk * 128:(dk + 1) * 128], actT[:, j, sl], j == 0, j == nchunk - 1,
                                reads=["wd", ("actT", j, t)], writes=[("ps", b)])
                    self.resid_add(b, dk, t)

    def xattn(self, l):
        S = self.S
        S.barrier()
        self.a_reset()
        self.norm_T()
        gq = self.G[:, l, 8:16]
        gkv = self.G[:, l, 16:24]
        wq = self.dram["xa_w_q"][l].rearrange("(k p) n -> p k n", p=128)
        wkv = self.dram["xa_w_kv"][l].rearrange("(k p) n -> p k n", p=128)
        wo = self.dram["xa_w_out"][l].rearrange("(k p) n -> p k n", p=128)
        qT = self.a_bf(8 * 2048).rearrange("p (c n) -> p c n", c=8)
        kT = self.a_bf(8 * 256).rearrange("p (c n) -> p c n", c=8)
        vt = self.a_bf(2 * 1024).rearrange("p (c n) -> p c n", c=2)
        sqb = [self.a_bf(512), self.a_bf(512)]
        sd = [self.a_f32(512), self.a_f32(512)]
        pT = [self.a_bf(512), self.a_bf(512)]
        rden = self.a_f32(512)
        qg = self.xqg[:, l, :]
        kg = self.xkg[:, l, :]
        for cg in range(8):
            wb, wk = self.wslab()
            wv = wb.rearrange("p (k n) -> p k n", k=8)
            self.load_cols(wkv, [(cg * 256, 256)], wv, gkv, wk)
            if cg < 4:
                for j in range(2):
                    fc = cg * 2 + j
                    b = self.bank()
                    for k in range(8):
                        self.mm(self.psf(b, 0, 256), wv[:, k, j * 128:(j + 1) * 128], self.memT[:, k, :], k == 0, k == 7,
                                reads=[wk, "memT"], writes=[("ps", b)])
                    self.cp("act", kT[:, fc, :], self.psf(b, 0, 256), reads=[("ps", b)], writes=[("kT", fc)])
                    self.act(sqb[j][:, 0:256], self.psf(b, 0, 256), AF.Square, reads=[("ps", b)], writes=[("sqb", j)])
                h = cg
                b2 = self.bank()
                for j in range(2):
                    self.mm(self.psf(b2, 0, 256), self.ones_bf, sqb[j][:, 0:256], j == 0, j == 1, reads=[("sqb", j)], writes=[("ps", b2)])
                self.act(sd[0][:, 0:256], self.psf(b2, 0, 256), AF.Sqrt, reads=[("ps", b2)], writes=[("sd", 0)], bias=self.eps_t[:, 0:1], scale=1.0 / 256)
                self.recip(sd[1][:, 0:256], sd[0][:, 0:256], reads=[("sd", 0)], writes=[("sd", 1)])
                for j in range(2):
                    fc = 2 * h + j
                    self.stt("dve", kT[:, fc, :], kT[:, fc, :], kg[:, j:j + 1], sd[1][:, 0:256], ALU.mult, ALU.mult,
                             reads=[("kT", fc), ("sd", 1)], writes=[("kT", fc)])
            else:
                for mt in range(2):
                    b = self.bank()
                    for k in range(8):
                        self.mm(self.psf(b, 0, 256), self.memT[:, k, mt * 128:(mt + 1) * 128], wv[:, k, :], k == 0, k == 7,
                                reads=[wk, "memT"], writes=[("ps", b)])
                    c0 = (cg - 4) * 256
                    self.cp("act", vt[:, mt, c0:c0 + 256], self.psf(b, 0, 256), reads=[("ps", b)], writes=[("vt", mt, cg)])
        if self.stop_at == 1:
            return
        for cg in range(4):
            h = cg
            wb, wk = self.wslab()
            wv = wb.rearrange("p (k n) -> p k n", k=8)
            self.load_cols(wq, [(cg * 256, 256)], wv, gq, wk)
            for t in range(4):
                sl = slice(512 * t, 512 * t + 512)
                for j in range(2):
                    fc = 2 * h + j
                    b = self.bank()
                    for k in range(8):
                        self.mm(self.psf(b), wv[:, k, j * 128:(j + 1) * 128], self.hT[:, k, sl], k == 0, k == 7,
                                reads=[wk, ("hT", k, t)], writes=[("ps", b)])
                    self.cp("act", qT[:, fc, sl], self.psf(b), reads=[("ps", b)], writes=[("qT", fc, t)])
                    self.act(sqb[j], self.psf(b), AF.Square, reads=[("ps", b)], writes=[("sqb", j)])
                b2 = self.bank()
                for j in range(2):
                    self.mm(self.psf(b2), self.ones_bf, sqb[j], j == 0, j == 1, reads=[("sqb", j)], writes=[("ps", b2)])
                self.act(sd[0], self.psf(b2), AF.Sqrt, reads=[("ps", b2)], writes=[("sd", 0)], bias=self.eps_t[:, 0:1], scale=1.0 / 256)
                self.recip(sd[1], sd[0], reads=[("sd", 0)], writes=[("sd", 1)])
                for j in range(2):
                    fc = 2 * h + j
                    self.stt("dve", qT[:, fc, sl], qT[:, fc, sl], qg[:, j:j + 1], sd[1], ALU.mult, ALU.mult,
                             reads=[("qT", fc, t), ("sd", 1)], writes=[("qT", fc, t)])
        if self.stop_at == 2:
            return
        S.barrier()
        oT = self.hT
        scale = float(256 ** -0.5)
        xitems = [(h, t) for h in range(4) for t in range(4)]
        pT4 = [pT[0], pT[1], self.SQ[0], self.SQ[1]]
        pkeys = [("pT", 0), ("pT", 1), ("sq", 0), ("sq", 1)]

        def xscore(n):
            h, t = xitems[n]
            sl = slice(512 * t, 512 * t + 512)
            for kt in range(2):
                b = self.bank()
                pi = 2 * (n % 2) + kt
                for j in range(2):
                    fc = 2 * h + j
                    self.mm(self.psf(b), kT[:, fc, kt * 128:(kt + 1) * 128], qT[:, fc, sl], j == 0, j == 1,
                            reads=[("kT", fc), ("qT", fc, t)], writes=[("ps", b)])
                self.act(pT4[pi], self.psf(b), AF.Exp, reads=[("ps", b)], writes=[pkeys[pi]], scale=scale)

        def xpv(n):
            h, t = xitems[n]
            sl = slice(512 * t, 512 * t + 512)
            bd = self.bank()
            for kt in range(2):
                pi = 2 * (n % 2) + kt
                self.mm(self.psf(bd), self.ones_bf, pT4[pi], kt == 0, kt == 1, reads=[pkeys[pi]], writes=[("ps", bd)])
            self.recip(rden, self.psf(bd), reads=[("ps", bd)], writes=["rden"])
            for j in range(2):
                fc = 2 * h + j
                b = self.bank()
                for kt in range(2):
                    pi = 2 * (n % 2) + kt
                    self.mm(self.psf(b), vt[:, kt, fc * 128:(fc + 1) * 128], pT4[pi], kt == 0, kt == 1,
                            reads=[("vt", kt, 4 + fc // 2), pkeys[pi]], writes=[("ps", b)])
                self.tt("dve", oT[:, fc, sl], self.psf(b), rden, ALU.mult, reads=[("ps", b), "rden"], writes=[("oT", fc, t)])

        xscore(0)
        for n in range(len(xitems)):
            if n + 1 < len(xitems):
                xscore(n + 1)
            xpv(n)
        if self.stop_at == 3:
            return
        for cg in range(4):
            wb, wk = self.wslab()
            wv = wb.rearrange("p (k n) -> p k n", k=8)
            self.load_cols(wo, [(cg * 256, 256)], wv, None, wk)
            for j in range(2):
                dk = cg * 2 + j
                for t in range(4):
                    sl = slice(512 * t, 512 * t + 512)
                    b = self.bank()
                    for k in range(8):
                        self.mm(self.psf(b), wv[:, k, j * 128:(j + 1) * 128], oT[:, k, sl], k == 0, k == 7,
                                reads=[wk, ("oT", k, t)], writes=[("ps", b)])
                    self.resid_add(b, dk, t)

    def gla(self, l):
        S = self.S
        i = l // 2
        S.barrier()
        self.a_reset()
        self.norm_T()
        gain = self.G[:, l, 0:8]
        win = self.dram["od_w_in"][i].rearrange("(k p) n -> p k n", p=128)
        wout = self.dram["od_w_out"][i].rearrange("(c p) n -> p c n", p=128)
        wg1 = self.dram["od_w_g1"][i].rearrange("(k p) n -> p k n", p=128)
        og = self.ogain[:, i, :]
        g1aug = self.a_bf(2048)
        wg2 = self.a_bf(512)
        wg1b = self.a_bf(128).rearrange("p (k n) -> p k n", k=8)
        qin = self.a_bf(2048)
        kin = self.a_bf(2048)
        q2 = self.a_bf(2048)
        kin_tok = self.a_bf(2048).rearrange("p (t n) -> p t n", t=16)
        l_tok = self.a_bf(2048).rearrange("p (t n) -> p t n", t=16)
        v_tok = self.a_bf(4096).rearrange("p (t n) -> p t n", t=16)
        rs = self.a_bf(4096).rearrange("p (j n) -> p j n", j=2)
        attT = l_tok
        ogt = self.a_bf(4096).rearrange("p (j n) -> p j n", j=2)
        sqb = self.SQ
        dec = self.a_f32(32)
        R = self.a_f32(256)
        Rbf = [self.a_bf(256), self.a_bf(256)]
        e1 = [self.a_f32(512)]
        eq = self.a_f32(512)
        ek = self.a_f32(512)
        sd = self.SD
        tmp = self.a_f32(512)
        wo_b = self.a_bf(2 * 1024).rearrange("p (j n) -> p j n", j=2)
        self.memset("pool", g1aug[0:32, :], 1.0, writes=["g1aug"])
        st = self.STAGE[self.stage_ctr % 2]
        skey = ("stage", self.stage_ctr % 2)
        sslot = ("stage", self.stage_ctr % 2)
        self.stage_ctr += 1
        self.dma(st[:, 0:128].rearrange("p (k n) -> p k n", k=8), wg1, slot=sslot, writes=[skey])
        self.dma(st[0:16, 128:640], self.dram["od_w_g2"][i], slot=sslot, writes=[skey])
        self.dma(st[16:17, 128:640], self.dram["od_b_g"][i:i + 1, :], slot=sslot, writes=[skey])
        for k in range(8):
            self.ts("dve", wg1b[:, k, :], st[:, 16 * k:16 * k + 16], gain[:, k:k + 1], None, ALU.mult, None, reads=[skey], writes=["wg1b"])
        self.cp("dve", wg2[0:17, :], st[0:17, 128:640], reads=[skey], writes=["wg2"])
        for t in range(4):
            sl = slice(512 * t, 512 * t + 512)
            b = self.bank()
            for k in range(8):
                self.mm(self.PS[0:16, 512 * b:512 * b + 512], wg1b[:, k, :], self.hT[:, k, sl], k == 0, k == 7,
                        reads=["wg1b", ("hT", k, t)], writes=[("ps", b)])
            self.cp("act", g1aug[0:16, sl], self.PS[0:16, 512 * b:512 * b + 512], reads=[("ps", b)], writes=["g1aug"])
        for h in range(4):
            wbqk, wkqk = self.wslab()
            wqk = wbqk.rearrange("p (k n) -> p k n", k=8)
            self.load_cols(win, [(128 * h, 128), (512 + 128 * h, 128)], wqk, gain, wkqk)
            wbv, wkv_ = self.wslab()
            wvv = wbv.rearrange("p (k n) -> p k n", k=8)
            self.load_cols(win, [(1024 + 256 * h, 256)], wvv, gain, wkv_)
            wbr, wkr = self.wslab()
            wrv = wbr.rearrange("p (k n) -> p k n", k=8)
            self.load_cols(win, [(2048 + 256 * h, 256)], wrv, gain, wkr)
            for t in range(4):
                b = self.bank()
                for tt in range(4):
                    tok = 4 * t + tt
                    self.mm(self.psf(b, 128 * tt, 128 * tt + 128), g1aug[0:17, tok * 128:(tok + 1) * 128], wg2[0:17, 128 * h:128 * h + 128],
                            True, True, reads=["g1aug", "wg2"], writes=[("ps", b)])
                self.act(e1[0], self.psf(b), AF.Exp, reads=[("ps", b)], writes=["e1"], scale=-1.0)
                self.act(l_tok[:, 4 * t:4 * t + 4, :], e1[0].rearrange("p (t n) -> p t n", t=4), AF.Ln,
                         reads=["e1"], writes=[("LA", t)], bias=self.one_t[:, 0:1], scale=1.0)
            for tk in range(0, 16, 2):
                b = self.bank()
                for u in range(2):
                    tok = tk + u
                    for k in range(8):
                        self.mm(self.psf(b, 256 * u, 256 * u + 256), self.hT[:, k, tok * 128:(tok + 1) * 128], wvv[:, k, :], k == 0, k == 7,
                                reads=[wkv_, ("hT", k, tok // 4)], writes=[("ps", b)])
                self.cp("act", v_tok[:, tk:tk + 2, :], self.psf(b).rearrange("p (u n) -> p u n", u=2), reads=[("ps", b)], writes=[("v_tok", tk // 2)])
            for j in range(2):
                for t in range(4):
                    sl = slice(512 * t, 512 * t + 512)
                    b = self.bank()
                    for k in range(8):
                        self.mm(self.psf(b), wrv[:, k, 128 * j:128 * j + 128], self.hT[:, k, sl], k == 0, k == 7,
                                reads=[wkr, ("hT", k, t)], writes=[("ps", b)])
                    self.act(rs[:, j, sl], self.psf(b), AF.Silu, reads=[("ps", b)], writes=[("rs", j, t)])
            for t in range(4):
                sl = slice(512 * t, 512 * t + 512)
                bb = self.bank()
                for tt in range(4):
                    tok = 4 * t + tt
                    self.mm(self.psf(bb, 128 * tt, 128 * tt + 128), l_tok[:, tok, :], self.U_bf, True, True,
                            reads=[("LA", t)], writes=[("ps", bb)])
                self.act(eq, self.psf(bb), AF.Exp, reads=[("ps", bb)], writes=["eq"], scale=-1.0 / 16)
                self.act(ek, self.psf(bb), AF.Exp, reads=[("ps", bb)], writes=["ek"], scale=1.0 / 16)
                self.act(dec[:, 8 * t:8 * t + 8], self.psf(bb).rearrange("p (n c) -> p n c", c=64)[:, :, 63], AF.Exp,
                         reads=[("ps", bb)], writes=["dec"], scale=-1.0 / 16)
                bq = self.bank()
                for k in range(8):
                    self.mm(self.psf(bq), wqk[:, k, 0:128], self.hT[:, k, sl], k == 0, k == 7, reads=[wkqk, ("hT", k, t)], writes=[("ps", bq)])
                self.stt("dve", qin[:, sl], self.psf(bq), float(128 ** -0.5), eq, ALU.mult, ALU.mult, reads=[("ps", bq), "eq"], writes=[("qin", t)])
                bk = self.bank()
                for k in range(8):
                    self.mm(self.psf(bk), wqk[:, k, 128:256], self.hT[:, k, sl], k == 0, k == 7, reads=[wkqk, ("hT", k, t)], writes=[("ps", bk)])
                self.tt("dve", kin[:, sl], self.psf(bk), ek, ALU.mult, reads=[("ps", bk), "ek"], writes=[("kin", t)])
            self.tt("dve", q2[:, 64:2048].rearrange("p (n c) -> p n c", c=64), qin[:, 64:2048].rearrange("p (n c) -> p n c", c=64),
                    dec[:, 0:31].unsqueeze(2).broadcast_to([128, 31, 64]), ALU.mult,
                    reads=[("qin", t) for t in range(4)] + ["dec"], writes=["q2"])
            for t in range(4):
                b = self.bank()
                pb = self.psb(b)
                for tt in range(4):
                    tok = 4 * t + tt
                    self.tr(pb[:, 128 * tt:128 * tt + 128], kin[:, tok * 128:(tok + 1) * 128], self.ident_bf, reads=[("kin", t)], writes=[("ps", b)])
                self.cp("act", kin_tok[:, 4 * t:4 * t + 4, :], pb[:, 0:512].rearrange("p (t n) -> p t n", t=4), reads=[("ps", b)], writes=[("kin_tok", t)])
            for t in range(4):
                b = self.bank()
                for tt in range(4):
                    tok = 4 * t + tt
                    tsl = slice(tok * 128, tok * 128 + 128)
                    self.mm(self.psf(b, 128 * tt, 128 * tt + 128), kin[:, tsl], qin[:, tsl], True, True, reads=[("kin", t), ("qin", t)], writes=[("ps", b)])
                self.tt("dve", attT[:, 4 * t:4 * t + 4, :], self.psf(b).rearrange("p (t n) -> p t n", t=4),
                        self.U01.unsqueeze(1).broadcast_to([128, 4, 128]), ALU.mult, reads=[("ps", b)], writes=[("LA", t)])
            for t in range(4):
                sl = slice(512 * t, 512 * t + 512)
                bo = self.reserve(2)
                for tt in range(4):
                    p = 4 * t + tt
                    for s2 in range(2):
                        n = 2 * p + s2
                        bs = self.bank()
                        self.mm(self.psf(bs, 0, 256), kin_tok[64 * s2:64 * s2 + 64, p, :], v_tok[64 * s2:64 * s2 + 64, p, :], True, True,
                                reads=[("kin_tok", t), ("v_tok", p // 2)], writes=[("ps", bs)])
                        if s2 == 0:
                            for j in range(2):
                                self.mm(self.psf(bo[j], 128 * tt, 128 * tt + 128), v_tok[:, p, 128 * j:128 * j + 128], attT[:, p, :], True, False,
                                        reads=[("v_tok", p // 2), ("LA", t)], writes=[("ps", bo[j])], skip_group_check=True)
                        if n >= 1:
                            for j in range(2):
                                c0 = 128 * tt + 64 * s2
                                self.mm(self.psf(bo[j], c0, c0 + 64), Rbf[(n - 1) % 2][:, 128 * j:128 * j + 128], q2[:, 64 * n:64 * n + 64], False, True,
                                        reads=[("Rbf", (n - 1) % 2), "q2"], writes=[("ps", bo[j])], skip_group_check=True)
                        if n == 0:
                            self.cp("dve", Rbf[0], self.psf(bs, 0, 256), reads=[("ps", bs)], writes=[("Rbf", 0)])
                            self.cp("dve", R, self.psf(bs, 0, 256), reads=[("ps", bs)], writes=["R"])
                        else:
                            if n < 31:
                                self.stt("dve", Rbf[n % 2], R, dec[:, n - 1:n], self.psf(bs, 0, 256), ALU.mult, ALU.add,
                                         reads=["R", "dec", ("ps", bs)], writes=[("Rbf", n % 2)])
                                self.stt("dve", R, R, dec[:, n - 1:n], self.psf(bs, 0, 256), ALU.mult, ALU.add, reads=["R", "dec", ("ps", bs)], writes=["R"])
                for j in range(2):
                    self.act(sqb[j], self.psf(bo[j]), AF.Square, reads=[("ps", bo[j])], writes=[("sq", j)])
                b2 = self.bank()
                for j in range(2):
                    self.mm(self.psf(b2), self.ones_bf, sqb[j], j == 0, j == 1, reads=[("sq", j)], writes=[("ps", b2)])
                self.act(sd[0], self.psf(b2), AF.Sqrt, reads=[("ps", b2)], writes=[("sd", 0)], bias=self.eps_t[:, 0:1], scale=1.0 / 256)
                self.recip(sd[1], sd[0], reads=[("sd", 0)], writes=[("sd", 1)])
                for j in range(2):
                    self.tt("dve", tmp, self.psf(bo[j]), sd[1], ALU.mult, reads=[("ps", bo[j]), ("sd", 1)], writes=["tmp"])
                    self.tt("dve", ogt[:, j, sl], tmp, rs[:, j, sl], ALU.mult, reads=["tmp", ("rs", j, t)], writes=[("ogt", j, t)])
                self.release(bo)
            self.load_rows(wout[:, 2 * h:2 * h + 2, :], wo_b, 2, og[:, 2 * h:2 * h + 2], "wo_b")
            for dk in range(8):
                for t in range(4):
                    sl = slice(512 * t, 512 * t + 512)
                    b = self.bank()
                    for j in range(2):
                        self.mm(self.psf(b), wo_b[:, j, dk * 128:(dk + 1) * 128], ogt[:, j, sl], j == 0, j == 1,
                                reads=["wo_b", ("ogt", j, t)], writes=[("ps", b)])
                    self.resid_add(b, dk, t)

    def even(self, l):
        S = self.S
        i = l // 2
        S.barrier()
        self.a_reset()
        self.norm_T()
        gain = self.G[:, l, 0:8]
        win = self.dram["ev_w_in"][i].rearrange("(k p) n -> p k n", p=128)
        wout = self.dram["ev_w_out"][i].rearrange("(k p) n -> p k n", p=128)
        qg = self.evq[:, i, 0:1]
        kg = self.evk[:, i, 0:1]
        lng = self.evlng[:, i, :]
        lnb = self.evlnb[:, i, :]
        gT = self.a_bf(4 * 2048).rearrange("p (c n) -> p c n", c=4)
        xhat = self.a_bf(16 * 512).rearrange("p (t n) -> p t n", t=16)
        qf = xhat.rearrange("p t n -> p (t n)").rearrange("p (c n) -> p c n", c=4)
        kf = self.a_bf(4 * 2048).rearrange("p (c n) -> p c n", c=4)
        wsT = self.a_bf(8 * 128).rearrange("p (g n) -> p g n", g=8)
        btab = self.a_f32(4 * 128).rearrange("p (c n) -> p c n", c=4)
        bsT = self.a_f32(4 * 128).rearrange("p (c n) -> p c n", c=4)
        kmbd = self.a_bf(4 * 16).rearrange("p (c n) -> p c n", c=4)
        kmean = self.a_f32(4 * 8).rearrange("p (c n) -> p c n", c=4)
        st1 = self.a_f32(8)
        st2 = self.a_f32(8)
        mean = self.a_f32(8)
        var = self.a_f32(8)
        rstd8 = self.a_f32(8)
        F = [self.a_f32(512), self.a_f32(512)]
        Bt = [self.a_bf(512), self.a_bf(512), self.a_bf(512)]
        gel = self.SD
        sqf, xc = F[0], F[1]
        sqb = self.SQ
        qgb = [Bt[0], Bt[1]]
        qn = Bt[2]
        sd = self.SD
        t1, t2 = F[0], F[1]
        tmpg = F[0]
        cmp_ = F[0]
        pT = Bt
        gsb = self.a_f32(64)
        cnt = self.a_f32(64)
        nm_tok = [self.a_bf(384), self.a_bf(384)]
        rec = self.a_f32(256)
        s = self.stage_ctr % 2
        self.stage_ctr += 1
        st = self.STAGE[s]
        skey = ("stage", s)
        sslot = ("stage", s)
        ws_d = self.dram["ev_w_s"][i]
        self.dma(st[:, 0:1024].rearrange("p (g n) -> p g n", g=8), ws_d.rearrange("g i j -> i g j"), slot=sslot, writes=[skey])
        bs_d = self.dram["ev_b_s"][i]
        for g in range(8):
            c, hf = g // 2, g % 2
            self.dma(bsT[64 * hf:64 * hf + 64, c, :], bs_d[g:g + 1, :].partition_broadcast(64), slot=("misc", 0), writes=["bsT"])
        for g in range(8):
            c, hf = g // 2, g % 2
            b = self.bank()
            self.tr(self.psf(b, 0, 128), st[:, 128 * g:128 * g + 128], self.ident_f, reads=[skey], writes=[("ps", b)])
            self.tt("dve", wsT[:, g, :], self.psf(b, 0, 128), self.C01, ALU.mult, reads=[("ps", b)], writes=[("wsT", g)])
            b2 = self.bank()
            self.mm(self.PS[64 * hf:64 * hf + 64, 512 * b2:512 * b2 + 128], self.ones_bf[:, 0:64], wsT[:, g, :], True, True,
                    reads=[("wsT", g)], writes=[("ps", b2)])
            self.stt("dve", btab[64 * hf:64 * hf + 64, c, :], self.PS[64 * hf:64 * hf + 64, 512 * b2:512 * b2 + 128],
                     lnb[64 * hf:64 * hf + 64, c:c + 1], bsT[64 * hf:64 * hf + 64, c, :], ALU.mult, ALU.add,
                     reads=[("ps", b2), "bsT"], writes=[("btab", g)])

        def slab(c0):
            wb, wk = self.wslab()
            wv = wb.rearrange("p (k n) -> p k n", k=8)
            self.load_cols(win, [(c0, 256)], wv, gain, wk)
            return wv, wk

        wvA, wkA = slab(2048)
        wvB, wkB = slab(2304)
        red = lambda o_, i_: (lambda e: e.tensor_reduce(o_, i_, AX.X, ALU.add))
        for tok in range(16):
            b = self.bank()
            for sl2, (wv, wk) in enumerate(((wvA, wkA), (wvB, wkB))):
                for k in range(8):
                    self.mm(self.psf(b, 256 * sl2, 256 * sl2 + 256), self.hT[:, k, tok * 128:(tok + 1) * 128], wv[:, k, :], k == 0, k == 7,
                            reads=[wk, ("hT", k, tok // 4)], writes=[("ps", b)], skip_group_check=True)
            gi = tok % 2
            ge = gel[gi]
            self.act(ge, self.psf(b), AF.Gelu, reads=[("ps", b)], writes=[("sd", gi)])
            ge3 = ge.rearrange("p (g d) -> p g d", g=8)
            self.S.op("dve", red(st1, ge3), reads=[("sd", gi)], writes=["st1"])
            self.act(sqf, ge, AF.Square, reads=[("sd", gi)], writes=[("F", 0)])
            self.S.op("dve", red(st2, sqf.rearrange("p (g d) -> p g d", g=8)), reads=[("F", 0)], writes=["st2"])
            self.ts("dve", mean, st1, 1.0 / 64, None, ALU.mult, None, reads=["st1"], writes=["mean"])
            self.tt("dve", var, mean, mean, ALU.mult, reads=["mean"], writes=["var"])
            self.stt("dve", var, st2, 1.0 / 64, var, ALU.mult, ALU.subtract, reads=["st2", "var"], writes=["var"])
            self.act(rstd8, var, AF.Sqrt, reads=["var"], writes=["rstd8"], bias=self.eps_t[:, 0:1], scale=1.0)
            self.recip(rstd8, rstd8, reads=["rstd8"], writes=["rstd8"])
            xc3 = xc.rearrange("p (g d) -> p g d", g=8)
            self.tt("dve", xc3, ge3, mean.unsqueeze(2).broadcast_to([128, 8, 64]), ALU.subtract, reads=[("sd", gi), "mean"], writes=[("F", 1)])
            self.tt("dve", xhat[:, tok, :].rearrange("p (g d) -> p g d", g=8), xc3,
                    rstd8.unsqueeze(2).broadcast_to([128, 8, 64]), ALU.mult, reads=[("F", 1), "rstd8"], writes=[("xhat", tok)])
        for sl2 in range(2):
            wv, wk = slab(1536 + 256 * sl2)
            for j in range(2):
                c = 2 * sl2 + j
                for t in range(4):
                    sl = slice(512 * t, 512 * t + 512)
                    b = self.bank()
                    for k in range(8):
                        self.mm(self.psf(b), wv[:, k, 128 * j:128 * j + 128], self.hT[:, k, sl], k == 0, k == 7,
                                reads=[wk, ("hT", k, t)], writes=[("ps", b)])
                    self.act(gT[:, c, sl], self.psf(b), AF.Gelu, reads=[("ps", b)], writes=[("gT", c, t)])
        for g in range(8):
            c, hf = g // 2, g % 2
            pr = slice(64 * hf, 64 * hf + 64)
            for t in range(4):
                sl = slice(512 * t, 512 * t + 512)
                b = self.bank()
                for tt in range(4):
                    tok = 4 * t + tt
                    self.mm(self.PS[pr, 512 * b + 128 * tt:512 * b + 128 * tt + 128], xhat[:, tok, 64 * g:64 * g + 64], wsT[:, g, :], True, True,
                            reads=[("xhat", tok), ("wsT", g)], writes=[("ps", b)])
                self.stt("dve", tmpg[pr, :].rearrange("p (t n) -> p t n", t=4), self.PS[pr, 512 * b:512 * b + 512].rearrange("p (t n) -> p t n", t=4),
                         lng[pr, c:c + 1], btab[pr, c, :].unsqueeze(1).broadcast_to([64, 4, 128]), ALU.mult, ALU.add,
                         reads=[("ps", b), ("btab", g)], writes=[("F", 0)])
                self.tt("dve", gT[pr, c, sl], tmpg[pr, :], gT[pr, c, sl], ALU.mult, reads=[("F", 0), ("gT", c, t)], writes=[("gT", c, t)])
        S.barrier()
        for which in range(2):
            dst = qf if which == 0 else kf
            gcol = qg if which == 0 else kg
            name = "qf" if which == 0 else "kf"
            for sl2 in range(2):
                wv, wk = slab(512 * which + 256 * sl2)
                for j in range(2):
                    c = 2 * sl2 + j
                    for t in range(4):
                        sl = slice(512 * t, 512 * t + 512)
                        b = self.bank()
                        for k in range(8):
                            self.mm(self.psf(b), wv[:, k, 128 * j:128 * j + 128], self.hT[:, k, sl], k == 0, k == 7,
                                    reads=[wk, ("hT", k, t)], writes=[("ps", b)])
                        si = t % 2
                        self.act(sqb[si], self.psf(b), AF.Square, reads=[("ps", b)], writes=[("sq", si)])
                        self.act(qgb[si], self.psf(b), AF.Copy, reads=[("ps", b)], writes=[("B", si)])
                        b2 = self.bank()
                        self.mm(self.psf(b2), self.bo_bf, sqb[si], True, True, reads=[("sq", si)], writes=[("ps", b2)])
                        self.act(sd[0], self.psf(b2), AF.Sqrt, reads=[("ps", b2)], writes=[("sd", 0)], bias=self.eps_t[:, 0:1], scale=1.0 / 64)
                        self.recip(sd[1], sd[0], reads=[("sd", 0)], writes=[("sd", 1)])
                        self.stt("dve", qn, qgb[si], gcol, sd[1], ALU.mult, ALU.mult, reads=[("B", si), ("sd", 1)], writes=[("B", 2)])
                        b3 = self.bank()
                        self.mm(self.psf(b3), self.pm_bf, qn, True, True, reads=[("B", 2)], writes=[("ps", b3)])
                        self.tt("dve", t1, qn, self.ropeC[:, sl], ALU.mult, reads=[("B", 2)], writes=[("F", 0)])
                        self.tt("dve", t2, self.psf(b3), self.ropeS[:, sl], ALU.mult, reads=[("ps", b3)], writes=[("F", 1)])
                        self.tt("dve", dst[:, c, sl], t1, t2, ALU.add, reads=[("F", 0), ("F", 1)],
                                writes=[(name, c, hf, 2 * t + u) for hf in range(2) for u in range(2)])
        wv0, wk0 = slab(1024)
        wv1, wk1 = slab(1280)
        for tok in range(16):
            b = self.bank()
            for sl2, (wv, wk) in enumerate(((wv0, wk0), (wv1, wk1))):
                for k in range(8):
                    self.mm(self.psf(b, 256 * sl2, 256 * sl2 + 256), self.hT[:, k, tok * 128:(tok + 1) * 128], wv[:, k, :], k == 0, k == 7,
                            reads=[wk, ("hT", k, tok // 4)], writes=[("ps", b)], skip_group_check=True)
            self.cp("act", self.hT[:, 4:8, tok * 128:(tok + 1) * 128], self.psf(b).rearrange("p (a n) -> p a n", a=4),
                    reads=[("ps", b)], writes=[("hT", 4 + a_, tok // 4) for a_ in range(4)])
        S.barrier()

        def vsl(kc, h):
            c0 = kc * 128 + 64 * (h % 2)
            return self.hT[:, 4 + h // 2, c0:c0 + 64]
        negT = [self.hT.rearrange("p k n -> p (k n)")[:, 2048 * s_:2048 * s_ + 2048] for s_ in range(3)]
        allk = lambda nm, c: [(nm, c, hf, ib) for hf in range(2) for ib in range(8)]
        for c in range(4):
            self.S.op("dve", (lambda o_, i_: lambda e: e.tensor_reduce(o_, i_, AX.X, ALU.add))(kmean[:, c, :], kf[:, c, :].rearrange("p (n k) -> p n k", n=8)),
                      reads=allk("kf", c), writes=[("kmean", c)])
            self.memset("pool", kmbd[:, c, :], 0.0, writes=[("kmbd", c)])
            for hf in range(2):
                pr = slice(64 * hf, 64 * hf + 64)
                self.ts("dve", kmbd[pr, c, 8 * hf:8 * hf + 8], kmean[pr, c, :], 1.0 / 256, None, ALU.mult, None, reads=[("kmean", c)], writes=[("kmbd", c)])
        for tok in range(8, 16):
            o = tok // 2
            bg = self.bank()
            for c in range(4):
                self.mm(self.psf(bg, 16 * c, 16 * c + 16), qf[:, c, tok * 128:(tok + 1) * 128], kmbd[:, c, :], True, True,
                        reads=allk("qf", c) + [("kmbd", c)], writes=[("ps", bg)], skip_group_check=True)
            self.cp("dve", gsb, self.psf(bg, 0, 64), reads=[("ps", bg)], writes=["gsb"])
            g3 = gsb.rearrange("p (h n) -> p h n", h=8)[:, :, 0:o]
            cm4 = cmp_[:, 0:8 * o * o].rearrange("p (h n m) -> p h n m", h=8, n=o)
            self.tt("dve", cm4, g3.unsqueeze(2).broadcast_to([128, 8, o, o]), g3.unsqueeze(3).broadcast_to([128, 8, o, o]), ALU.is_gt,
                    reads=["gsb"], writes=[("F", 0)])
            cn3 = cnt[:, 0:8 * o].rearrange("p (h n) -> p h n", h=8)
            self.S.op("dve", (lambda o_, i_: lambda e: e.tensor_reduce(o_, i_, AX.X, ALU.add))(cn3, cm4), reads=[("F", 0)], writes=["cnt"])
            nm = nm_tok[tok % 2]
            self.memset("pool", nm, 0.0, writes=[("nm", tok % 2)])
            for s_ in range(3):
                ng = 3 if s_ < 2 else 2
                self.ts("dve", nm[:, 128 * s_:128 * s_ + 96].rearrange("p (g n) -> p g n", n=32)[:, 0:ng, 0:o], cn3[:, 3 * s_:3 * s_ + ng, :],
                        3.0, NEG, ALU.is_ge, ALU.mult, reads=["cnt"], writes=[("nm", tok % 2)])
            bt = self.bank()
            pb = self.psb(bt)
            for s_ in range(3):
                self.tr(pb[:, 128 * s_:128 * s_ + 128], nm[:, 128 * s_:128 * s_ + 128], self.ident_bf, reads=[("nm", tok % 2)], writes=[("ps", bt)])
            for s_ in range(3):
                self.cp("act", negT[s_][:, tok * 128:(tok + 1) * 128], pb[:, 128 * s_:128 * s_ + 128], reads=[("ps", bt)], writes=[("negT", s_, tok)])
        scale = 0.125
        items = [(h, ib, jb) for h in range(8) for ib in range(8) for jb in range(ib + 1)]
        st_ = {}

        def score(n):
            h, ib, jb = items[n]
            c, hf = h // 2, h % 2
            pr = slice(64 * hf, 64 * hf + 64)
            s_, g_ = h // 3, h % 3
            gr = slice(32 * g_, 32 * g_ + 32)
            qsl = slice(256 * ib, 256 * ib + 256)
            b = self.bank()
            pi = n % 3
            kc0, kc1 = 2 * jb, 2 * jb + 1
            rd = [("kf", c, hf, jb), ("qf", c, hf, ib)]
            self.mm(self.psf(b, 0, 256), kf[pr, c, kc0 * 128:kc0 * 128 + 128], qf[pr, c, qsl], True, False,
                    reads=rd, writes=[("ps", b)], skip_group_check=True)
            if jb < ib:
                self.mm(self.psf(b, 256, 512), kf[pr, c, kc1 * 128:kc1 * 128 + 128], qf[pr, c, qsl], False, ib <= 3,
                        reads=rd, writes=[("ps", b)], skip_group_check=True)
                if ib > 3:
                    self.mm(self.psf(b), self.e8_bf[gr, jb, :],
                            negT[s_][gr, qsl].unsqueeze(1).broadcast_to([32, 2, 256]), False, True,
                            reads=[("negT", s_, 2 * ib), ("negT", s_, 2 * ib + 1)], writes=[("ps", b)], skip_group_check=True)
                w = 512
            else:
                self.mm(self.psf(b, 256, 384), kf[pr, c, kc1 * 128:kc1 * 128 + 128], qf[pr, c, 256 * ib + 128:256 * ib + 256], False, False,
                        reads=rd, writes=[("ps", b)], skip_group_check=True)
                self.mm(self.psf(b, 0, 128), self.ident_bf, self.cn_bf, False, False,
                        reads=[], writes=[("ps", b)], skip_group_check=True)
                self.mm(self.psf(b, 256, 384), self.ident_bf, self.cn_bf, False, True,
                        reads=[], writes=[("ps", b)], skip_group_check=True)
                w = 384
            self.act(pT[pi][:, 0:w], self.psf(b, 0, w), AF.Exp, reads=[("ps", b)], writes=[("B", pi)], scale=scale)

        def pv(n):
            h, ib, jb = items[n]
            c, hf = h // 2, h % 2
            pr = slice(64 * hf, 64 * hf + 64)
            qsl = slice(256 * ib, 256 * ib + 256)
            pi = n % 3
            kc0, kc1 = 2 * jb, 2 * jb + 1
            if jb == 0:
                st_["banks"] = self.reserve(2)
            ba, bd = st_["banks"]
            acc = self.PS[pr, 512 * ba:512 * ba + 256]
            den = self.PS[pr, 512 * bd:512 * bd + 256]
            last = (jb == ib)
            self.mm(acc, vsl(kc0, h), pT[pi][:, 0:256], jb == 0, False, reads=[("B", pi)], writes=[("ps", ba)], skip_group_check=True)
            self.mm(den, self.ones_bf[:, 0:64], pT[pi][:, 0:256], jb == 0, False, reads=[("B", pi)], writes=[("ps", bd)], skip_group_check=True)
            if not last:
                self.mm(acc, vsl(kc1, h), pT[pi][:, 256:512], False, False, reads=[("B", pi)], writes=[("ps", ba)], skip_group_check=True)
                self.mm(den, self.ones_bf[:, 0:64], pT[pi][:, 256:512], False, False, reads=[("B", pi)], writes=[("ps", bd)], skip_group_check=True)
            else:
                self.mm(self.PS[pr, 512 * ba + 128:512 * ba + 256], vsl(kc1, h), pT[pi][:, 256:384], False, True,
                        reads=[("B", pi)], writes=[("ps", ba)], skip_group_check=True)
                self.mm(self.PS[pr, 512 * bd + 128:512 * bd + 256], self.ones_bf[:, 0:64], pT[pi][:, 256:384], False, True,
                        reads=[("B", pi)], writes=[("ps", bd)], skip_group_check=True)
                self.recip(rec[pr, :], den, reads=[("ps", bd)], writes=[("rec", hf)])
                self.tt("dve", qf[pr, c, qsl], acc, rec[pr, :], ALU.mult, reads=[("ps", ba), ("rec", hf)], writes=[("qf", c, hf, ib)])
                self.release([ba, bd])

        score(0)
        for n in range(len(items)):
            if n + 1 < len(items):
                score(n + 1)
            pv(n)
        for cg in range(4):
            wb, wk = self.wslab()
            wv = wb.rearrange("p (k n) -> p k n", k=8)
            self.load_cols(wout, [(cg * 256, 256)], wv, None, wk)
            for j in range(2):
                dk = cg * 2 + j
                for t in range(4):
                    sl = slice(512 * t, 512 * t + 512)
                    b = self.bank()
                    for k in range(8):
                        if k < 4:
                            rhs = qf[:, k, sl]
                            rk = [("qf", k, hf, ib) for hf in range(2) for ib in (2 * t, 2 * t + 1)]
                        else:
                            rhs = gT[:, k - 4, sl]
                            rk = [("gT", k - 4, t)]
                        self.mm(self.psf(b), wv[:, k, j * 128:(j + 1) * 128], rhs, k == 0, k == 7, reads=[wk] + rk, writes=[("ps", b)])
                    self.resid_add(b, dk, t)

    def build(self):
        nc = self.nc
        x_d = self.din("x", [S_LEN, D])
        mem_d = self.din("mem", [NMEM, D])
        pos_d = self.din("positions", [1, S_LEN], I32)
        cst_d = self.din("consts", [128, C_END])
        G_d = self.din("gains", [128, DEPTH * 32])
        xqg_d = self.din("xa_qg", [128, DEPTH * 2])
        xkg_d = self.din("xa_kg", [128, DEPTH * 2])
        evq_d = self.din("ev_qg", [128, 2])
        evk_d = self.din("ev_kg", [128, 2])
        evlng_d = self.din("ev_lng", [128, 8])
        evlnb_d = self.din("ev_lnb", [128, 8])
        og_d = self.din("od_og", [128, 16])
        self.din("ev_w_in", [2, D, 2560]); self.din("ev_w_s", [2, 8, 128, 128]); self.din("ev_b_s", [2, 8, 128])
        self.din("ev_w_out", [2, D, D]); self.din("od_w_in", [2, D, 3072]); self.din("od_w_g1", [2, D, 16])
        self.din("od_w_g2", [2, 16, 512]); self.din("od_b_g", [2, 512]); self.din("od_w_out", [2, D, D])
        self.din("xa_w_q", [DEPTH, D, D]); self.din("xa_w_kv", [DEPTH, D, 2 * D]); self.din("xa_w_out", [DEPTH, D, D])
        self.din("ffn_w_gu", [DEPTH, D, 2 * DFF]); self.din("ffn_w_down", [DEPTH, DFF, D])
        out_d = nc.dram_tensor("out", [S_LEN, D], F32, kind="ExternalOutput").ap()

        with ExitStack() as es:
            def sb(name, shape, dt=F32):
                return es.enter_context(nc.sbuf_tensor(name, shape, dt))[:]
            self.S = S = Sched(nc)
            self.PS = es.enter_context(nc.psum_tensor("ps", [128, 4096], F32))[:]
            self.xT = sb("xT", [128, 8 * 2048]).rearrange("p (k n) -> p k n", k=8)
            self.hT = sb("hT", [128, 8 * 2048], BF16).rearrange("p (k n) -> p k n", k=8)
            self.memT = sb("memT", [128, 8 * 256], BF16).rearrange("p (k n) -> p k n", k=8)
            cf = sb("cf", [128, 386])
            self.G = sb("G", [128, DEPTH * 32]).rearrange("p (l n) -> p l n", l=DEPTH)
            self.xqg = sb("xqg", [128, DEPTH * 2]).rearrange("p (l n) -> p l n", l=DEPTH)
            self.xkg = sb("xkg", [128, DEPTH * 2]).rearrange("p (l n) -> p l n", l=DEPTH)
            self.evq = sb("evq", [128, 2]).rearrange("p (l n) -> p l n", l=2)
            self.evk = sb("evk", [128, 2]).rearrange("p (l n) -> p l n", l=2)
            self.evlng = sb("evlng", [128, 8]).rearrange("p (l n) -> p l n", l=2)
            self.evlnb = sb("evlnb", [128, 8]).rearrange("p (l n) -> p l n", l=2)
            self.ogain = sb("ogain", [128, 16]).rearrange("p (l n) -> p l n", l=2)
            self.ident_f = cf[:, 0:128]
            self.C01 = cf[:, 128:256]
            self.U01 = cf[:, 256:384]
            invf_t = cf[:, 384:385]
            sgn_t = cf[:, 385:386]
            cbf = sb("cbf", [128, 7 * 128 + 1024], BF16)
            self.ident_bf = cbf[:, 0:128]
            self.bo_bf = cbf[:, 128:256]
            self.pm_bf = cbf[:, 256:384]
            self.cn_bf = cbf[:, 384:512]
            self.U_bf = cbf[:, 640:768]
            self.ones_bf = cbf[:, 768:896]
            self.e8_bf = cbf[:, 896:1920].rearrange("p (n m) -> p n m", n=8)
            self.eps_t = sb("eps_t", [128, 1])
            self.one_t = sb("one_t", [128, 1])
            self.ropeC = sb("ropeC", [128, 2048], BF16)
            self.ropeS = sb("ropeS", [128, 2048], BF16)
            self.STAGE = [sb("stage0", [128, 1024]), sb("stage1", [128, 1024])]
            self.WB = [sb("wb%d" % i, [128, 2048], BF16) for i in range(3)]
            self.SQ = [sb("sq%d" % i, [128, 512], BF16) for i in range(2)]
            self.SD = [sb("sd%d" % i, [128, 512]) for i in range(2)]
            self.A_WORDS = (nc.sbuf_bytes_remaining // 4) - 64
            self.ARENA = sb("arena", [128, self.A_WORDS])

            self.a_reset()
            cst = self.a_f32(C_END)
            self.dma(cst, cst_d, slot=("c", 0), writes=["cst"])
            self.dma(self.G.rearrange("p l n -> p (l n)"), G_d, slot=("c", 0), writes=["G"])
            self.dma(self.xqg.rearrange("p l n -> p (l n)"), xqg_d, slot=("c", 0), writes=["G"])
            self.dma(self.xkg.rearrange("p l n -> p (l n)"), xkg_d, slot=("c", 0), writes=["G"])
            self.dma(self.evq.rearrange("p l n -> p (l n)"), evq_d, slot=("c", 0), writes=["G"])
            self.dma(self.evk.rearrange("p l n -> p (l n)"), evk_d, slot=("c", 0), writes=["G"])
            self.dma(self.evlng.rearrange("p l n -> p (l n)"), evlng_d, slot=("c", 0), writes=["G"])
            self.dma(self.evlnb.rearrange("p l n -> p (l n)"), evlnb_d, slot=("c", 0), writes=["G"])
            self.dma(self.ogain.rearrange("p l n -> p (l n)"), og_d, slot=("c", 0), writes=["G"])
            self.cp("dve", cbf[:, 0:1920], cst[:, 0:1920], reads=["cst"], writes=["cbf"])
            self.cp("dve", cf[:, 0:128], cst[:, C_ID:C_ID + 128], reads=["cst"], writes=["cf"])
            self.cp("dve", cf[:, 128:384], cst[:, C_C01:C_C01 + 256], reads=["cst"], writes=["cf"])
            self.cp("dve", cf[:, 384:386], cst[:, C_INVF:C_INVF + 2], reads=["cst"], writes=["cf"])
            self.memset("dve", self.eps_t, EPS, writes=["eps"])
            self.memset("dve", self.one_t, 1.0, writes=["eps"])
            S.barrier()

            self.a_reset()
            posi = self.a_f32(2048).bitcast(I32)
            posf = self.a_f32(2048)
            tq = self.a_f32(2048)
            tn_i = self.a_f32(2048).bitcast(I32)
            tn = self.a_f32(2048)
            self.dma(posi, pos_d[0:1, :].partition_broadcast(128), slot=("c", 1), writes=["posi"])
            self.cp("dve", posf, posi, reads=["posi"], writes=["posf"])
            inv2pi = float(1.0 / (2.0 * np.pi))
            for which in range(2):
                self.ts("dve", tq, posf, invf_t, None, ALU.mult, None, reads=["posf"], writes=["tq"])
                self.ts("dve", tq, tq, inv2pi, 0.25 if which == 0 else 0.0, ALU.mult, ALU.add, reads=["tq"], writes=["tq"])
                self.cp("dve", tn_i, tq, reads=["tq"], writes=["tn_i"])
                self.cp("dve", tn, tn_i, reads=["tn_i"], writes=["tn"])
                self.tt("dve", tq, tq, tn, ALU.subtract, reads=["tq", "tn"], writes=["tq"])
                self.ts("dve", tq, tq, -0.4999, 0.4999, ALU.max, ALU.min, reads=["tq"], writes=["tq"])
                if which == 0:
                    self.act(self.ropeC, tq, AF.Sin, reads=["tq"], writes=["ropeC"], scale=float(2.0 * np.pi))
                else:
                    self.act(tn, tq, AF.Sin, reads=["tq"], writes=["tn"], scale=float(2.0 * np.pi))
                    self.ts("dve", self.ropeS, tn, sgn_t, None, ALU.mult, None, reads=["tn"], writes=["ropeS"])
            S.barrier()

            self.a_reset()
            mt_f = self.a_f32(1024)
            mt_j = self.a_f32(1024)
            mt_b = self.a_bf(1024)
            mss = self.a_f32(2)
            for mt in range(2):
                self.dma(mt_f, mem_d[mt * 128:(mt + 1) * 128, :], slot=("c", 2), writes=["mt_f"])
                self.act(mt_j, mt_f, AF.Square, reads=["mt_f"], writes=["mt_j", "mss"], accum_out=mss[:, 0:1])
                self.act(mss[:, 1:2], mss[:, 0:1], AF.Sqrt, reads=["mss"], writes=["mss2"], bias=self.eps_t[:, 0:1], scale=1.0 / D)
                self.recip(mss[:, 1:2], mss[:, 1:2], reads=["mss2"], writes=["mss2"])
                self.ts("dve", mt_b, mt_f, mss[:, 1:2], None, ALU.mult, None, reads=["mt_f", "mss2"], writes=["mt_b"])
                for kk in range(0, 8, 4):
                    b = self.bank()
                    pb = self.psb(b)
                    for k in range(kk, kk + 4):
                        self.tr(pb[:, 128 * (k - kk):128 * (k - kk) + 128], mt_b[:, 128 * k:128 * k + 128], self.ident_bf, reads=["mt_b"], writes=[("ps", b)])
                    self.cp("dve", self.memT[:, kk:kk + 4, mt * 128:(mt + 1) * 128], pb[:, 0:512].rearrange("p (k n) -> p k n", k=4),
                            reads=[("ps", b)], writes=["memT"])
            S.barrier()

            self.a_reset()
            xs = [self.a_f32(1024) for _ in range(4)]
            for t in range(4):
                for tt in range(4):
                    tok = 4 * t + tt
                    self.dma(xs[tt], x_d[tok * 128:(tok + 1) * 128, :], slot=("xs", tt), writes=[("xs", tt)])
                for k in range(8):
                    b = self.bank()
                    for tt in range(4):
                        self.tr(self.psf(b, 128 * tt, 128 * tt + 128), xs[tt][:, 128 * k:128 * k + 128], self.ident_f, reads=[("xs", tt)], writes=[("ps", b)])
                    self.cp("dve" if k % 2 == 0 else "act", self.xT[:, k, 512 * t:512 * t + 512], self.psf(b), reads=[("ps", b)], writes=[("xT", k, t)])

            for l in self.layers:
                if "mix" in self.subs:
                    if l % 2 == 0:
                        self.even(l)
                    else:
                        self.gla(l)
                if "xa" in self.subs:
                    self.xattn(l)
                if "ffn" in self.subs:
                    self.ffn(l)

            S.barrier()
            self.a_reset()
            os_ = [self.a_f32(1024) for _ in range(2)]
            stores = []
            for tok in range(16):
                ob = os_[tok % 2]
                t = tok // 4
                for kk in range(0, 8, 4):
                    b = self.bank()
                    for k in range(kk, kk + 4):
                        self.tr(self.psf(b, 128 * (k - kk), 128 * (k - kk) + 128), self.xT[:, k, tok * 128:(tok + 1) * 128], self.ident_f,
                                reads=[("xT", k, t)], writes=[("ps", b)])
                    self.cp("dve" if kk == 0 else "act", ob[:, 128 * kk:128 * kk + 512], self.psf(b), reads=[("ps", b)], writes=[("os", tok % 2, kk)])
                stores.append(self.dma(out_d[tok * 128:(tok + 1) * 128, :], ob, slot=("out", tok % 2), reads=[("os", tok % 2, 0), ("os", tok % 2, 4)]))
            S.final_wait("sp", stores)
            S.finalize(es)
            block = es.enter_context(nc.Block())
            S.emit(block)
        return nc


def _pk(v, n):
    v = np.asarray(v, np.float32)
    lead = v.shape[:-1]
    a = v.reshape(lead + (n, 128))
    a = np.moveaxis(a, -1, 0)
    return np.ascontiguousarray(a.reshape(128, -1))


def _host_inputs(inp):
    f = lambda k: np.ascontiguousarray(np.asarray(inp[k], np.float32))
    gains = np.concatenate([_pk(inp["norm_mix"], 8).reshape(128, DEPTH, 8), _pk(inp["norm_mem_q"], 8).reshape(128, DEPTH, 8),
                            _pk(inp["norm_mem_kv"], 8).reshape(128, DEPTH, 8), _pk(inp["norm_ffn"], 8).reshape(128, DEPTH, 8)], axis=2)
    shared = {
        "consts": _consts(),
        "gains": np.ascontiguousarray(gains.reshape(128, DEPTH * 32)),
        "xa_qg": _pk(inp["xa_q_gain"], 2),
        "xa_kg": _pk(inp["xa_k_gain"], 2),
        "ev_qg": np.ascontiguousarray(np.tile(np.asarray(inp["ev_q_gain"], np.float32), (1, 2)).T),
        "ev_kg": np.ascontiguousarray(np.tile(np.asarray(inp["ev_k_gain"], np.float32), (1, 2)).T),
        "ev_lng": _pk(np.asarray(inp["ev_ln_g"], np.float32).reshape(2, 512), 4),
        "ev_lnb": _pk(np.asarray(inp["ev_ln_b"], np.float32).reshape(2, 512), 4),
        "od_og": _pk(np.asarray(inp["od_o_gain"], np.float32).reshape(2, 1024), 8),
    }
    for k in ("ev_w_in", "ev_w_s", "ev_b_s", "ev_w_out", "od_w_in", "od_w_g1", "od_w_g2", "od_b_g", "od_w_out",
              "xa_w_q", "xa_w_kv", "xa_w_out", "ffn_w_gu", "ffn_w_down"):
        shared[k] = f(k)
    x = f("x")
    mem = f("mem")
    pos = np.ascontiguousarray(np.asarray(inp["positions"], np.int32))
    maps = []
    for b in range(8):
        m = dict(shared)
        m["x"] = x[b]
        m["mem"] = mem[b]
        m["positions"] = pos[b:b + 1]
        maps.append(m)
    return maps


_NC_CACHE = {}


def run_layers(inp, layers, subs=("mix", "xa", "ffn"), cores=8, stop_at=0):
    key = (tuple(layers), tuple(subs), stop_at)
    if key not in _NC_CACHE:
        bld = Builder(layers, subs)
        bld.stop_at = stop_at
        _NC_CACHE[key] = bld.build()
    nc = _NC_CACHE[key]
    maps = _host_inputs(inp)[:cores]
    res = run_bass_kernel_spmd(nc, maps, core_ids=list(range(cores)))
    return np.stack([np.asarray(r["out"], np.float32) for r in res.results], axis=0)


def kernel(**inputs):
    return run_layers(inputs, list(range(DEPTH)))
```

```python
import numpy as np
from contextlib import ExitStack
import concourse.bass as bass
import concourse.mybir as mybir
from concourse.bass_utils import run_bass_kernel_spmd

F32 = mybir.dt.float32
BF16 = mybir.dt.bfloat16
I32 = mybir.dt.int32
AF = mybir.ActivationFunctionType
ALU = mybir.AluOpType
AX = mybir.AxisListType

S_LEN = 2048
D = 1024
NMEM = 256
DEPTH = 4
DFF = 2816
EPS = 1e-6
NEG = -30000.0
ENGS = ("pe", "act", "dve", "pool", "sp")


class _Op:
    __slots__ = ("eng", "fn", "deps", "is_dma", "slot", "ticket", "needs_inc", "waits")

    def __init__(self, eng, fn, is_dma, slot):
        self.eng = eng
        self.fn = fn
        self.deps = ()
        self.is_dma = is_dma
        self.slot = slot
        self.ticket = None
        self.needs_inc = False
        self.waits = None


class Sched:
    def __init__(self, nc):
        self.nc = nc
        self.ops = {e: [] for e in ENGS}
        self.last_w = {}
        self.readers = {}
        self.all_ops = []
        self.slot_last = {}

    def op(self, eng, fn, reads=(), writes=(), dma_slot=None):
        o = _Op(eng, fn, dma_slot is not None, dma_slot)
        if dma_slot is not None:
            self.slot_last[dma_slot] = o
        deps = set()
        for r in reads:
            w = self.last_w.get(r)
            if w is not None:
                deps.add(w)
        for r in writes:
            w = self.last_w.get(r)
            if w is not None:
                deps.add(w)
            rl = self.readers.get(r)
            if rl:
                deps.update(rl)
        deps = set((self.slot_last[d.slot] if d.is_dma else d) for d in deps)
        deps.discard(o)
        o.deps = tuple(deps)
        for r in reads:
            self.readers.setdefault(r, []).append(o)
        for r in writes:
            self.last_w[r] = o
            self.readers[r] = []
        self.all_ops.append(o)
        self.ops[eng].append(o)
        return o

    def barrier(self):
        lasts = []
        for e in ENGS:
            for o in reversed(self.ops[e]):
                if not o.is_dma and o.fn is not None:
                    lasts.append(o)
                    break
        slots = {}
        for o in self.all_ops:
            if o.is_dma:
                slots[o.slot] = o
        lasts.extend(slots.values())
        for e in ENGS:
            b = _Op(e, None, False, None)
            b.deps = tuple(lasts)
            self.all_ops.append(b)
            self.ops[e].append(b)
        self.last_w = {}
        self.readers = {}

    def final_wait(self, eng, ops):
        b = _Op(eng, None, False, None)
        b.deps = tuple(ops)
        self.all_ops.append(b)
        self.ops[eng].append(b)

    @staticmethod
    def _skip(d, o):
        return (not d.is_dma) and d.eng == o.eng and d.eng == "pe"

    def finalize(self, es):
        nc = self.nc
        for o in self.all_ops:
            for d in o.deps:
                if d.is_dma or self._skip(d, o):
                    continue
                d.needs_inc = True
        self.sems = {e: es.enter_context(nc.semaphore("prog_" + e)) for e in ENGS}
        cnt = {e: 0 for e in ENGS}
        slot_cnt = {}
        self.slot_sems = {}
        for o in self.all_ops:
            if o.is_dma:
                if o.slot not in self.slot_sems:
                    self.slot_sems[o.slot] = es.enter_context(nc.semaphore("dma_%d" % len(self.slot_sems)))
                    slot_cnt[o.slot] = 0
                slot_cnt[o.slot] += 16
                o.ticket = slot_cnt[o.slot]
            elif o.needs_inc:
                cnt[o.eng] += 1
                o.ticket = cnt[o.eng]
        waited = {e: {} for e in ENGS}
        for o in self.all_ops:
            need = {}
            for d in o.deps:
                if d.is_dma:
                    key = ("slot", d.slot)
                else:
                    if self._skip(d, o):
                        continue
                    key = ("eng", d.eng)
                if need.get(key, 0) < d.ticket:
                    need[key] = d.ticket
            w = []
            wd = waited[o.eng]
            for key, v in need.items():
                if wd.get(key, 0) >= v:
                    continue
                wd[key] = v
                w.append((key, v))
            o.waits = w

    def _sem(self, key):
        return self.sems[key[1]] if key[0] == "eng" else self.slot_sems[key[1]]

    def emit(self, block):
        def body(engname):
            def f(e):
                for o in self.ops[engname]:
                    for key, v in o.waits:
                        e.wait_ge(self._sem(key), v)
                    if o.fn is None:
                        continue
                    ins = o.fn(e)
                    if o.is_dma:
                        ins.then_inc(self.slot_sems[o.slot], 16)
                    elif o.needs_inc:
                        ins.then_inc(self.sems[engname], 1)
            return f
        block.tensor(body("pe"))
        block.scalar(body("act"))
        block.vector(body("dve"))
        block.gpsimd(body("pool"))
        block.sync(body("sp"))


C_ID, C_BO, C_PM, C_CN, C_C01, C_U01, C_ONE, C_E8, C_INVF, C_SGN, C_END = (
    0, 128, 256, 384, 512, 640, 768, 896, 1920, 1921, 1922)


def _consts():
    c = np.zeros((128, C_END), np.float32)
    p = np.arange(128)
    c[:, C_ID:C_ID + 128] = np.eye(128, dtype=np.float32)
    c[:, C_BO:C_BO + 128] = (p[:, None] // 64 == p[None, :] // 64).astype(np.float32)
    pm = np.zeros((128, 128), np.float32)
    for m in range(128):
        r = m % 64
        if r < 8:
            pm[m + 8, m] = 1.0
        elif r < 16:
            pm[m - 8, m] = 1.0
    c[:, C_PM:C_PM + 128] = pm
    c[:, C_CN:C_CN + 128] = np.where(p[:, None] <= p[None, :], 0.0, NEG)
    c[:, C_C01:C_C01 + 128] = (p[:, None] <= p[None, :]).astype(np.float32)
    c[:, C_U01:C_U01 + 128] = ((p[:, None] <= p[None, :]) & (p[:, None] // 64 == p[None, :] // 64)).astype(np.float32)
    c[:, C_ONE:C_ONE + 128] = 1.0
    e8 = np.zeros((128, 8, 128), np.float32)
    for n in range(8):
        e8[(p % 32) == n, n, :] = 1.0
    c[:, C_E8:C_E8 + 1024] = e8.reshape(128, 1024)
    half = 8
    inv_freq = (np.float32(500000.0) ** (-np.arange(half, dtype=np.float32) * np.float32(2.0) / np.float32(16))).astype(np.float32)
    r = p % 64
    c[:, C_INVF] = np.where(r < 16, inv_freq[r % 8], 0.0)
    c[:, C_SGN] = np.where(r < 8, -1.0, np.where(r < 16, 1.0, 0.0))
    return c


class Builder:
    def __init__(self, layers, subs=("mix", "xa", "ffn")):
        self.layers = list(layers)
        self.subs = subs
        self.nc = bass.Bass("TRN2", target_bir_lowering=False)
        self.dram = {}
        self.bank_ctr = 0
        self.reserved = set()
        self.conv_dve = False
        self.stop_at = 0
        self.stage_ctr = 0
        self.wb_ctr = 0
        self.uid = 0

    def din(self, name, shape, dt=F32):
        t = self.nc.dram_tensor(name, list(shape), dt, kind="ExternalInput").ap()
        self.dram[name] = t
        return t

    def bank(self):
        while True:
            b = self.bank_ctr % 8
            self.bank_ctr += 1
            if b not in self.reserved:
                return b

    def reserve(self, n):
        out = []
        for _ in range(n):
            b = self.bank()
            self.reserved.add(b)
            out.append(b)
        return out

    def release(self, banks):
        for b in banks:
            self.reserved.discard(b)

    def psf(self, b, lo=0, hi=512):
        return self.PS[:, 512 * b + lo:512 * b + hi]

    def psb(self, b):
        return self.PS[:, 512 * b:512 * b + 512].bitcast(BF16)

    def key(self, name):
        self.uid += 1
        return (name, self.uid)

    def mm(self, out, lhsT, rhs, start, stop, reads, writes, **kw):
        return self.S.op("pe", lambda e: e.matmul(out, lhsT, rhs, start=start, stop=stop, **kw), reads=reads, writes=writes)

    def tr(self, out, in_, ident, reads, writes):
        return self.S.op("pe", lambda e: e.transpose(out, in_, ident), reads=reads, writes=writes)

    def act(self, out, in_, func, reads, writes, **kw):
        return self.S.op("act", lambda e: e.activation(out=out, in_=in_, func=func, **kw), reads=reads, writes=writes)

    def tt(self, eng, out, in0, in1, op, reads, writes):
        return self.S.op(eng, lambda e: e.tensor_tensor(out, in0, in1, op), reads=reads, writes=writes)

    def ts(self, eng, out, in0, s1, s2, op0, op1, reads, writes):
        if op1 is None:
            return self.S.op(eng, lambda e: e.tensor_scalar(out, in0, s1, None, op0), reads=reads, writes=writes)
        return self.S.op(eng, lambda e: e.tensor_scalar(out, in0, s1, s2, op0, op1), reads=reads, writes=writes)

    def stt(self, eng, out, in0, scalar, in1, op0, op1, reads, writes):
        return self.S.op(eng, lambda e: e.scalar_tensor_tensor(out, in0, scalar, in1, op0, op1), reads=reads, writes=writes)

    def cp(self, eng, out, in_, reads, writes):
        if eng == "act":
            return self.S.op("act", lambda e: e.copy(out, in_), reads=reads, writes=writes)
        return self.S.op(eng, lambda e: e.tensor_copy(out, in_), reads=reads, writes=writes)

    def recip(self, out, in_, reads, writes):
        return self.S.op("dve", lambda e: e.reciprocal(out, in_), reads=reads, writes=writes)

    def memset(self, eng, ap, val, writes):
        return self.S.op(eng, lambda e: e.memset(ap, val), writes=writes)

    def dma(self, out, in_, slot, reads=(), writes=()):
        return self.S.op("sp", lambda e: e.dma_start(out=out, in_=in_), reads=reads, writes=writes, dma_slot=slot)

    def a_reset(self):
        self.a_off = 0

    def a_f32(self, n):
        ap = self.ARENA[:, self.a_off:self.a_off + n]
        self.a_off += n
        assert self.a_off <= self.A_WORDS, ("arena overflow", self.a_off, self.A_WORDS)
        return ap

    def a_bf(self, n):
        w = (n + 1) // 2
        return self.a_f32(w).bitcast(BF16)

    def _conv(self, i, dst, src, g, skey, wkey):
        e = ("act", "dve")[i % 2]
        if g is None:
            self.cp(e, dst, src, reads=[skey], writes=[wkey])
        elif e == "act":
            self.S.op("act", lambda en: en.activation(out=dst, in_=src, func=AF.Copy, scale=g), reads=[skey], writes=[wkey])
        else:
            self.ts(e, dst, src, g, None, ALU.mult, None, reads=[skey], writes=[wkey])

    def load_cols(self, wsrc, ranges, dst, gain, wkey):
        for kh in range(2):
            s = self.stage_ctr % 2
            self.stage_ctr += 1
            st = self.STAGE[s].rearrange("p (k n) -> p k n", k=4)
            skey = ("stage", s)
            off = 0
            for (c0, w) in ranges:
                self.dma(st[:, :, off:off + w], wsrc[:, 4 * kh:4 * kh + 4, c0:c0 + w], slot=("stage", s), writes=[skey])
                off += w
            for kk in range(4):
                k = 4 * kh + kk
                g = None if gain is None else gain[:, k:k + 1]
                self._conv(k, dst[:, k, :], st[:, kk, :], g, skey, wkey)

    def load_rows(self, wsrc, dst, n, gain, wkey):
        for i in range(n):
            s = self.stage_ctr % 2
            self.stage_ctr += 1
            st = self.STAGE[s]
            skey = ("stage", s)
            self.dma(st, wsrc[:, i, :], slot=("stage", s), writes=[skey])
            g = None if gain is None else gain[:, i:i + 1]
            for q in range(2):
                self._conv(2 * i + q, dst[:, i, 512 * q:512 * q + 512], st[:, 512 * q:512 * q + 512], g, skey, wkey)

    def wslab(self):
        i = self.wb_ctr % 3
        self.wb_ctr += 1
        return self.WB[i], ("wb", i)

    def norm_T(self):
        S = self.S
        for t in range(4):
            sl = slice(512 * t, 512 * t + 512)
            b = self.bank()
            for k in range(8):
                sq = self.SQ[k % 2]
                self.act(sq, self.xT[:, k, sl], AF.Square, reads=[("xT", k, t)], writes=[("sq", k % 2)])
                self.mm(self.psf(b), self.ones_bf, sq, k == 0, k == 7, reads=[("sq", k % 2)], writes=[("ps", b)])
            self.act(self.SD[0], self.psf(b), AF.Sqrt, reads=[("ps", b)], writes=[("sd", 0)], bias=self.eps_t[:, 0:1], scale=1.0 / D)
            self.recip(self.SD[1], self.SD[0], reads=[("sd", 0)], writes=[("sd", 1)])
            for k in range(8):
                self.tt("dve", self.hT[:, k, sl], self.xT[:, k, sl], self.SD[1], ALU.mult,
                        reads=[("xT", k, t), ("sd", 1)], writes=[("hT", k, t)])

    def resid_add(self, b, k, t):
        sl = slice(512 * t, 512 * t + 512)
        self.tt("dve", self.xT[:, k, sl], self.psf(b), self.xT[:, k, sl], ALU.add,
                reads=[("ps", b), ("xT", k, t)], writes=[("xT", k, t)])

    def ffn(self, l):
        self.S.barrier()
        self.a_reset()
        self.norm_T()
        gain = self.G[:, l, 24:32]
        wgu = self.dram["ffn_w_gu"][l].rearrange("(k p) n -> p k n", p=128)
        wdn = self.dram["ffn_w_down"][l].rearrange("(c p) n -> p c n", p=128)
        parts = [(0, 6), (6, 12), (12, 17), (17, 22)]
        actT = self.a_bf(6 * 2048).rearrange("p (c n) -> p c n", c=6)
        wd = self.a_bf(6 * 1024).rearrange("p (c n) -> p c n", c=6)
        sg = [self.a_f32(512), self.a_f32(512)]
        for (c0, c1) in parts:
            self.load_rows(wdn[:, c0:c1, :], wd, c1 - c0, None, "wd")
            for c in range(c0, c1):
                wb, wk = self.wslab()
                wv = wb.rearrange("p (k n) -> p k n", k=8)
                self.load_cols(wgu, [(c * 128, 128), (DFF + c * 128, 128)], wv, gain, wk)
                for t in range(4):
                    sl = slice(512 * t, 512 * t + 512)
                    bg, bu = self.bank(), self.bank()
                    for k in range(8):
                        self.mm(self.psf(bg), wv[:, k, 0:128], self.hT[:, k, sl], k == 0, k == 7, reads=[wk, ("hT", k, t)], writes=[("ps", bg)])
                    for k in range(8):
                        self.mm(self.psf(bu), wv[:, k, 128:256], self.hT[:, k, sl], k == 0, k == 7, reads=[wk, ("hT", k, t)], writes=[("ps", bu)])
                    si = (c * 4 + t) % 2
                    self.act(sg[si], self.psf(bg), AF.Silu, reads=[("ps", bg)], writes=[("sg", si)])
                    self.tt("dve", actT[:, c - c0, sl], sg[si], self.psf(bu), ALU.mult, reads=[("sg", si), ("ps", bu)], writes=[("actT", c - c0, t)])
            nchunk = c1 - c0
            for dk in range(8):
                for t in range(4):
                    sl = slice(512 * t, 512 * t + 512)
                    b = self.bank()
                    for j in range(nchunk):
                        self.mm(self.psf(b), wd[:, j, dk * 128:(dk + 1) * 128], actT[:, j, sl], j == 0, j == nchunk - 1,
                                reads=["wd", ("actT", j, t)], writes=[("ps", b)])
                    self.resid_add(b, dk, t)

    def xattn(self, l):
        S = self.S
        S.barrier()
        self.a_reset()
        self.norm_T()
        gq = self.G[:, l, 8:16]
        gkv = self.G[:, l, 16:24]
        wq = self.dram["xa_w_q"][l].rearrange("(k p) n -> p k n", p=128)
        wkv = self.dram["xa_w_kv"][l].rearrange("(k p) n -> p k n", p=128)
        wo = self.dram["xa_w_out"][l].rearrange("(k p) n -> p k n", p=128)
        qT = self.a_bf(8 * 2048).rearrange("p (c n) -> p c n", c=8)
        kT = self.a_bf(8 * 256).rearrange("p (c n) -> p c n", c=8)
        vt = self.a_bf(2 * 1024).rearrange("p (c n) -> p c n", c=2)
        sqb = [self.a_bf(512), self.a_bf(512)]
        sd = [self.a_f32(512), self.a_f32(512)]
        pT = [self.a_bf(512), self.a_bf(512)]
        rden = self.a_f32(512)
        qg = self.xqg[:, l, :]
        kg = self.xkg[:, l, :]
        for cg in range(8):
            wb, wk = self.wslab()
            wv = wb.rearrange("p (k n) -> p k n", k=8)
            self.load_cols(wkv, [(cg * 256, 256)], wv, gkv, wk)
            if cg < 4:
                for j in range(2):
                    fc = cg * 2 + j
                    b = self.bank()
                    for k in range(8):
                        self.mm(self.psf(b, 0, 256), wv[:, k, j * 128:(j + 1) * 128], self.memT[:, k, :], k == 0, k == 7,
                                reads=[wk, "memT"], writes=[("ps", b)])
                    self.cp("act", kT[:, fc, :], self.psf(b, 0, 256), reads=[("ps", b)], writes=[("kT", fc)])
                    self.act(sqb[j][:, 0:256], self.psf(b, 0, 256), AF.Square, reads=[("ps", b)], writes=[("sqb", j)])
                h = cg
                b2 = self.bank()
                for j in range(2):
                    self.mm(self.psf(b2, 0, 256), self.ones_bf, sqb[j][:, 0:256], j == 0, j == 1, reads=[("sqb", j)], writes=[("ps", b2)])
                self.act(sd[0][:, 0:256], self.psf(b2, 0, 256), AF.Sqrt, reads=[("ps", b2)], writes=[("sd", 0)], bias=self.eps_t[:, 0:1], scale=1.0 / 256)
                self.recip(sd[1][:, 0:256], sd[0][:, 0:256], reads=[("sd", 0)], writes=[("sd", 1)])
                for j in range(2):
                    fc = 2 * h + j
                    self.stt("dve", kT[:, fc, :], kT[:, fc, :], kg[:, j:j + 1], sd[1][:, 0:256], ALU.mult, ALU.mult,
                             reads=[("kT", fc), ("sd", 1)], writes=[("kT", fc)])
            else:
                for mt in range(2):
                    b = self.bank()
                    for k in range(8):
                        self.mm(self.psf(b, 0, 256), self.memT[:, k, mt * 128:(mt + 1) * 128], wv[:, k, :], k == 0, k == 7,
                                reads=[wk, "memT"], writes=[("ps", b)])
                    c0 = (cg - 4) * 256
                    self.cp("act", vt[:, mt, c0:c0 + 256], self.psf(b, 0, 256), reads=[("ps", b)], writes=[("vt", mt, cg)])
        if self.stop_at == 1:
            return
        for cg in range(4):
            h = cg
            wb, wk = self.wslab()
            wv = wb.rearrange("p (k n) -> p k n", k=8)
            self.load_cols(wq, [(cg * 256, 256)], wv, gq, wk)
            for t in range(4):
                sl = slice(512 * t, 512 * t + 512)
                for j in range(2):
                    fc = 2 * h + j
                    b = self.bank()
                    for k in range(8):
                        self.mm(self.psf(b), wv[:, k, j * 128:(j + 1) * 128], self.hT[:, k, sl], k == 0, k == 7,
                                reads=[wk, ("hT", k, t)], writes=[("ps", b)])
                    self.cp("act", qT[:, fc, sl], self.psf(b), reads=[("ps", b)], writes=[("qT", fc, t)])
                    self.act(sqb[j], self.psf(b), AF.Square, reads=[("ps", b)], writes=[("sqb", j)])
                b2 = self.bank()
                for j in range(2):
                    self.mm(self.psf(b2), self.ones_bf, sqb[j], j == 0, j == 1, reads=[("sqb", j)], writes=[("ps", b2)])
                self.act(sd[0], self.psf(b2), AF.Sqrt, reads=[("ps", b2)], writes=[("sd", 0)], bias=self.eps_t[:, 0:1], scale=1.0 / 256)
                self.recip(sd[1], sd[0], reads=[("sd", 0)], writes=[("sd", 1)])
                for j in range(2):
                    fc = 2 * h + j
                    self.stt("dve", qT[:, fc, sl], qT[:, fc, sl], qg[:, j:j + 1], sd[1], ALU.mult, ALU.mult,
                             reads=[("qT", fc, t), ("sd", 1)], writes=[("qT", fc, t)])
        if self.stop_at == 2:
            return
        oT = self.hT
        scale = float(256 ** -0.5)
        xitems = [(h, t) for h in range(4) for t in range(4)]
        pT4 = [pT[0], pT[1], self.SQ[0], self.SQ[1]]
        pkeys = [("pT", 0), ("pT", 1), ("sq", 0), ("sq", 1)]

        def xscore(n):
            h, t = xitems[n]
            sl = slice(512 * t, 512 * t + 512)
            for kt in range(2):
                b = self.bank()
                pi = 2 * (n % 2) + kt
                for j in range(2):
                    fc = 2 * h + j
                    self.mm(self.psf(b), kT[:, fc, kt * 128:(kt + 1) * 128], qT[:, fc, sl], j == 0, j == 1,
                            reads=[("kT", fc), ("qT", fc, t)], writes=[("ps", b)])
                self.act(pT4[pi], self.psf(b), AF.Exp, reads=[("ps", b)], writes=[pkeys[pi]], scale=scale)

        def xpv(n):
            h, t = xitems[n]
            sl = slice(512 * t, 512 * t + 512)
            bd = self.bank()
            for kt in range(2):
                pi = 2 * (n % 2) + kt
                self.mm(self.psf(bd), self.ones_bf, pT4[pi], kt == 0, kt == 1, reads=[pkeys[pi]], writes=[("ps", bd)])
            self.recip(rden, self.psf(bd), reads=[("ps", bd)], writes=["rden"])
            for j in range(2):
                fc = 2 * h + j
                b = self.bank()
                for kt in range(2):
                    pi = 2 * (n % 2) + kt
                    self.mm(self.psf(b), vt[:, kt, fc * 128:(fc + 1) * 128], pT4[pi], kt == 0, kt == 1,
                            reads=[("vt", kt, 4 + fc // 2), pkeys[pi]], writes=[("ps", b)])
                self.tt("dve", oT[:, fc, sl], self.psf(b), rden, ALU.mult, reads=[("ps", b), "rden"], writes=[("hT", fc, t)])

        xscore(0)
        for n in range(len(xitems)):
            if n + 1 < len(xitems):
                xscore(n + 1)
            xpv(n)
        if self.stop_at == 3:
            return
        for cg in range(4):
            wb, wk = self.wslab()
            wv = wb.rearrange("p (k n) -> p k n", k=8)
            self.load_cols(wo, [(cg * 256, 256)], wv, None, wk)
            for j in range(2):
                dk = cg * 2 + j
                for t in range(4):
                    sl = slice(512 * t, 512 * t + 512)
                    b = self.bank()
                    for k in range(8):
                        self.mm(self.psf(b), wv[:, k, j * 128:(j + 1) * 128], oT[:, k, sl], k == 0, k == 7,
                                reads=[wk, ("hT", k, t)], writes=[("ps", b)])
                    self.resid_add(b, dk, t)

    def gla(self, l):
        S = self.S
        i = l // 2
        S.barrier()
        self.a_reset()
        self.norm_T()
        gain = self.G[:, l, 0:8]
        win = self.dram["od_w_in"][i].rearrange("(k p) n -> p k n", p=128)
        wout = self.dram["od_w_out"][i].rearrange("(c p) n -> p c n", p=128)
        wg1 = self.dram["od_w_g1"][i].rearrange("(k p) n -> p k n", p=128)
        og = self.ogain[:, i, :]
        g1aug = self.a_bf(2048)
        wg2 = self.a_bf(512)
        wg1b = self.a_bf(128).rearrange("p (k n) -> p k n", k=8)
        qin = self.a_bf(2048)
        kin = self.a_bf(2048)
        q2 = self.a_bf(2048)
        kin_tok = self.a_bf(2048).rearrange("p (t n) -> p t n", t=16)
        l_tok = self.a_bf(2048).rearrange("p (t n) -> p t n", t=16)
        v_tok = self.a_bf(4096).rearrange("p (t n) -> p t n", t=16)
        rs = self.a_bf(4096).rearrange("p (j n) -> p j n", j=2)
        attT = l_tok
        ogt = self.a_bf(4096).rearrange("p (j n) -> p j n", j=2)
        sqb = self.SQ
        dec = self.a_f32(32)
        R = self.a_f32(256)
        Rbf = [self.a_bf(256), self.a_bf(256)]
        e1 = [self.a_f32(512)]
        eq = self.a_f32(512)
        ek = self.a_f32(512)
        sd = self.SD
        tmp = self.a_f32(512)
        wo_b = self.a_bf(2 * 1024).rearrange("p (j n) -> p j n", j=2)
        self.memset("pool", g1aug[0:32, :], 1.0, writes=["g1aug"])
        st = self.STAGE[self.stage_ctr % 2]
        skey = ("stage", self.stage_ctr % 2)
        sslot = ("stage", self.stage_ctr % 2)
        self.stage_ctr += 1
        self.dma(st[:, 0:128].rearrange("p (k n) -> p k n", k=8), wg1, slot=sslot, writes=[skey])
        self.dma(st[0:16, 128:640], self.dram["od_w_g2"][i], slot=sslot, writes=[skey])
        self.dma(st[16:17, 128:640], self.dram["od_b_g"][i:i + 1, :], slot=sslot, writes=[skey])
        for k in range(8):
            self.ts("dve", wg1b[:, k, :], st[:, 16 * k:16 * k + 16], gain[:, k:k + 1], None, ALU.mult, None, reads=[skey], writes=["wg1b"])
        self.cp("dve", wg2[0:17, :], st[0:17, 128:640], reads=[skey], writes=["wg2"])
        for t in range(4):
            sl = slice(512 * t, 512 * t + 512)
            b = self.bank()
            for k in range(8):
                self.mm(self.PS[0:16, 512 * b:512 * b + 512], wg1b[:, k, :], self.hT[:, k, sl], k == 0, k == 7,
                        reads=["wg1b", ("hT", k, t)], writes=[("ps", b)])
            self.cp("act", g1aug[0:16, sl], self.PS[0:16, 512 * b:512 * b + 512], reads=[("ps", b)], writes=["g1aug"])
        for h in range(4):
            wbqk, wkqk = self.wslab()
            wqk = wbqk.rearrange("p (k n) -> p k n", k=8)
            self.load_cols(win, [(128 * h, 128), (512 + 128 * h, 128)], wqk, gain, wkqk)
            wbv, wkv_ = self.wslab()
            wvv = wbv.rearrange("p (k n) -> p k n", k=8)
            self.load_cols(win, [(1024 + 256 * h, 256)], wvv, gain, wkv_)
            wbr, wkr = self.wslab()
            wrv = wbr.rearrange("p (k n) -> p k n", k=8)
            self.load_cols(win, [(2048 + 256 * h, 256)], wrv, gain, wkr)
            for t in range(4):
                b = self.bank()
                for tt in range(4):
                    tok = 4 * t + tt
                    self.mm(self.psf(b, 128 * tt, 128 * tt + 128), g1aug[0:17, tok * 128:(tok + 1) * 128], wg2[0:17, 128 * h:128 * h + 128],
                            True, True, reads=["g1aug", "wg2"], writes=[("ps", b)])
                self.act(e1[0], self.psf(b), AF.Exp, reads=[("ps", b)], writes=["e1"], scale=-1.0)
                self.act(l_tok[:, 4 * t:4 * t + 4, :], e1[0].rearrange("p (t n) -> p t n", t=4), AF.Ln,
                         reads=["e1"], writes=[("LA", t)], bias=self.one_t[:, 0:1], scale=1.0)
            for tk in range(0, 16, 2):
                b = self.bank()
                for u in range(2):
                    tok = tk + u
                    for k in range(8):
                        self.mm(self.psf(b, 256 * u, 256 * u + 256), self.hT[:, k, tok * 128:(tok + 1) * 128], wvv[:, k, :], k == 0, k == 7,
                                reads=[wkv_, ("hT", k, tok // 4)], writes=[("ps", b)])
                self.cp("act", v_tok[:, tk:tk + 2, :], self.psf(b).rearrange("p (u n) -> p u n", u=2), reads=[("ps", b)], writes=[("v_tok", tk // 2)])
            for j in range(2):
                for t in range(4):
                    sl = slice(512 * t, 512 * t + 512)
                    b = self.bank()
                    for k in range(8):
                        self.mm(self.psf(b), wrv[:, k, 128 * j:128 * j + 128], self.hT[:, k, sl], k == 0, k == 7,
                                reads=[wkr, ("hT", k, t)], writes=[("ps", b)])
                    self.act(rs[:, j, sl], self.psf(b), AF.Silu, reads=[("ps", b)], writes=[("rs", j, t)])
            for t in range(4):
                sl = slice(512 * t, 512 * t + 512)
                bb = self.bank()
                for tt in range(4):
                    tok = 4 * t + tt
                    self.mm(self.psf(bb, 128 * tt, 128 * tt + 128), l_tok[:, tok, :], self.U_bf, True, True,
                            reads=[("LA", t)], writes=[("ps", bb)])
                self.act(eq, self.psf(bb), AF.Exp, reads=[("ps", bb)], writes=["eq"], scale=-1.0 / 16)
                self.act(ek, self.psf(bb), AF.Exp, reads=[("ps", bb)], writes=["ek"], scale=1.0 / 16)
                self.act(dec[:, 8 * t:8 * t + 8], self.psf(bb).rearrange("p (n c) -> p n c", c=64)[:, :, 63], AF.Exp,
                         reads=[("ps", bb)], writes=["dec"], scale=-1.0 / 16)
                bq = self.bank()
                for k in range(8):
                    self.mm(self.psf(bq), wqk[:, k, 0:128], self.hT[:, k, sl], k == 0, k == 7, reads=[wkqk, ("hT", k, t)], writes=[("ps", bq)])
                self.stt("dve", qin[:, sl], self.psf(bq), float(128 ** -0.5), eq, ALU.mult, ALU.mult, reads=[("ps", bq), "eq"], writes=[("qin", t)])
                bk = self.bank()
                for k in range(8):
                    self.mm(self.psf(bk), wqk[:, k, 128:256], self.hT[:, k, sl], k == 0, k == 7, reads=[wkqk, ("hT", k, t)], writes=[("ps", bk)])
                self.tt("dve", kin[:, sl], self.psf(bk), ek, ALU.mult, reads=[("ps", bk), "ek"], writes=[("kin", t)])
            self.tt("dve", q2[:, 64:2048].rearrange("p (n c) -> p n c", c=64), qin[:, 64:2048].rearrange("p (n c) -> p n c", c=64),
                    dec[:, 0:31].unsqueeze(2).broadcast_to([128, 31, 64]), ALU.mult,
                    reads=[("qin", t) for t in range(4)] + ["dec"], writes=["q2"])
            for t in range(4):
                b = self.bank()
                pb = self.psb(b)
                for tt in range(4):
                    tok = 4 * t + tt
                    self.tr(pb[:, 128 * tt:128 * tt + 128], kin[:, tok * 128:(tok + 1) * 128], self.ident_bf, reads=[("kin", t)], writes=[("ps", b)])
                self.cp("act", kin_tok[:, 4 * t:4 * t + 4, :], pb[:, 0:512].rearrange("p (t n) -> p t n", t=4), reads=[("ps", b)], writes=[("kin_tok", t)])
            for t in range(4):
                b = self.bank()
                for tt in range(4):
                    tok = 4 * t + tt
                    tsl = slice(tok * 128, tok * 128 + 128)
                    self.mm(self.psf(b, 128 * tt, 128 * tt + 128), kin[:, tsl], qin[:, tsl], True, True, reads=[("kin", t), ("qin", t)], writes=[("ps", b)])
                self.tt("dve", attT[:, 4 * t:4 * t + 4, :], self.psf(b).rearrange("p (t n) -> p t n", t=4),
                        self.U01.unsqueeze(1).broadcast_to([128, 4, 128]), ALU.mult, reads=[("ps", b)], writes=[("LA", t)])
            for t in range(4):
                sl = slice(512 * t, 512 * t + 512)
                bo = self.reserve(2)
                for tt in range(4):
                    p = 4 * t + tt
                    for s2 in range(2):
                        n = 2 * p + s2
                        bs = self.bank()
                        self.mm(self.psf(bs, 0, 256), kin_tok[64 * s2:64 * s2 + 64, p, :], v_tok[64 * s2:64 * s2 + 64, p, :], True, True,
                                reads=[("kin_tok", t), ("v_tok", p // 2)], writes=[("ps", bs)])
                        if s2 == 0:
                            for j in range(2):
                                self.mm(self.psf(bo[j], 128 * tt, 128 * tt + 128), v_tok[:, p, 128 * j:128 * j + 128], attT[:, p, :], True, False,
                                        reads=[("v_tok", p // 2), ("LA", t)], writes=[("ps", bo[j])], skip_group_check=True)
                        if n >= 1:
                            for j in range(2):
                                c0 = 128 * tt + 64 * s2
                                self.mm(self.psf(bo[j], c0, c0 + 64), Rbf[(n - 1) % 2][:, 128 * j:128 * j + 128], q2[:, 64 * n:64 * n + 64], False, True,
                                        reads=[("Rbf", (n - 1) % 2), "q2"], writes=[("ps", bo[j])], skip_group_check=True)
                        if n == 0:
                            self.cp("dve", Rbf[0], self.psf(bs, 0, 256), reads=[("ps", bs)], writes=[("Rbf", 0)])
                            self.cp("dve", R, self.psf(bs, 0, 256), reads=[("ps", bs)], writes=["R"])
                        else:
                            if n < 31:
                                self.stt("dve", Rbf[n % 2], R, dec[:, n - 1:n], self.psf(bs, 0, 256), ALU.mult, ALU.add,
                                         reads=["R", "dec", ("ps", bs)], writes=[("Rbf", n % 2)])
                                self.stt("dve", R, R, dec[:, n - 1:n], self.psf(bs, 0, 256), ALU.mult, ALU.add, reads=["R", "dec", ("ps", bs)], writes=["R"])
                for j in range(2):
                    self.act(sqb[j], self.psf(bo[j]), AF.Square, reads=[("ps", bo[j])], writes=[("sq", j)])
                b2 = self.bank()
                for j in range(2):
                    self.mm(self.psf(b2), self.ones_bf, sqb[j], j == 0, j == 1, reads=[("sq", j)], writes=[("ps", b2)])
                self.act(sd[0], self.psf(b2), AF.Sqrt, reads=[("ps", b2)], writes=[("sd", 0)], bias=self.eps_t[:, 0:1], scale=1.0 / 256)
                self.recip(sd[1], sd[0], reads=[("sd", 0)], writes=[("sd", 1)])
                for j in range(2):
                    self.tt("dve", tmp, self.psf(bo[j]), sd[1], ALU.mult, reads=[("ps", bo[j]), ("sd", 1)], writes=["tmp"])
                    self.tt("dve", ogt[:, j, sl], tmp, rs[:, j, sl], ALU.mult, reads=["tmp", ("rs", j, t)], writes=[("ogt", j, t)])
                self.release(bo)
            self.load_rows(wout[:, 2 * h:2 * h + 2, :], wo_b, 2, og[:, 2 * h:2 * h + 2], "wo_b")
            for dk in range(8):
                for t in range(4):
                    sl = slice(512 * t, 512 * t + 512)
                    b = self.bank()
                    for j in range(2):
                        self.mm(self.psf(b), wo_b[:, j, dk * 128:(dk + 1) * 128], ogt[:, j, sl], j == 0, j == 1,
                                reads=["wo_b", ("ogt", j, t)], writes=[("ps", b)])
                    self.resid_add(b, dk, t)

    def even(self, l):
        S = self.S
        i = l // 2
        S.barrier()
        self.a_reset()
        self.norm_T()
        gain = self.G[:, l, 0:8]
        win = self.dram["ev_w_in"][i].rearrange("(k p) n -> p k n", p=128)
        wout = self.dram["ev_w_out"][i].rearrange("(k p) n -> p k n", p=128)
        qg = self.evq[:, i, 0:1]
        kg = self.evk[:, i, 0:1]
        lng = self.evlng[:, i, :]
        lnb = self.evlnb[:, i, :]
        gT = self.a_bf(4 * 2048).rearrange("p (c n) -> p c n", c=4)
        xhat = self.a_bf(16 * 512).rearrange("p (t n) -> p t n", t=16)
        qf = xhat.rearrange("p t n -> p (t n)").rearrange("p (c n) -> p c n", c=4)
        kf = self.a_bf(4 * 2048).rearrange("p (c n) -> p c n", c=4)
        wsT = self.a_bf(8 * 128).rearrange("p (g n) -> p g n", g=8)
        btab = self.a_f32(4 * 128).rearrange("p (c n) -> p c n", c=4)
        bsT = self.a_f32(4 * 128).rearrange("p (c n) -> p c n", c=4)
        kmbd = self.a_bf(4 * 16).rearrange("p (c n) -> p c n", c=4)
        kmean = self.a_f32(4 * 8).rearrange("p (c n) -> p c n", c=4)
        st1 = self.a_f32(8)
        st2 = self.a_f32(8)
        mean = self.a_f32(8)
        var = self.a_f32(8)
        rstd8 = self.a_f32(8)
        F = [self.a_f32(512), self.a_f32(512)]
        Bt = [self.a_bf(512), self.a_bf(512), self.a_bf(512)]
        gel = self.SD
        sqf, xc = F[0], F[1]
        sqb = self.SQ
        qgb = [Bt[0], Bt[1]]
        qn = Bt[2]
        sd = self.SD
        t1, t2 = F[0], F[1]
        tmpg = F[0]
        cmp_ = F[0]
        pT = Bt
        gsb = self.a_f32(64)
        cnt = self.a_f32(64)
        nm_tok = [self.a_bf(384), self.a_bf(384)]
        rec = self.a_f32(256)
        s = self.stage_ctr % 2
        self.stage_ctr += 1
        st = self.STAGE[s]
        skey = ("stage", s)
        sslot = ("stage", s)
        ws_d = self.dram["ev_w_s"][i]
        self.dma(st[:, 0:1024].rearrange("p (g n) -> p g n", g=8), ws_d.rearrange("g i j -> i g j"), slot=sslot, writes=[skey])
        bs_d = self.dram["ev_b_s"][i]
        for g in range(8):
            c, hf = g // 2, g % 2
            self.dma(bsT[64 * hf:64 * hf + 64, c, :], bs_d[g:g + 1, :].partition_broadcast(64), slot=("misc", 0), writes=["bsT"])
        for g in range(8):
            c, hf = g // 2, g % 2
            b = self.bank()
            self.tr(self.psf(b, 0, 128), st[:, 128 * g:128 * g + 128], self.ident_f, reads=[skey], writes=[("ps", b)])
            self.tt("dve", wsT[:, g, :], self.psf(b, 0, 128), self.C01, ALU.mult, reads=[("ps", b)], writes=[("wsT", g)])
            b2 = self.bank()
            self.mm(self.PS[64 * hf:64 * hf + 64, 512 * b2:512 * b2 + 128], self.ones_bf[:, 0:64], wsT[:, g, :], True, True,
                    reads=[("wsT", g)], writes=[("ps", b2)])
            self.stt("dve", btab[64 * hf:64 * hf + 64, c, :], self.PS[64 * hf:64 * hf + 64, 512 * b2:512 * b2 + 128],
                     lnb[64 * hf:64 * hf + 64, c:c + 1], bsT[64 * hf:64 * hf + 64, c, :], ALU.mult, ALU.add,
                     reads=[("ps", b2), "bsT"], writes=[("btab", g)])

        def slab(c0):
            wb, wk = self.wslab()
            wv = wb.rearrange("p (k n) -> p k n", k=8)
            self.load_cols(win, [(c0, 256)], wv, gain, wk)
            return wv, wk

        wvA, wkA = slab(2048)
        wvB, wkB = slab(2304)
        red = lambda o_, i_: (lambda e: e.tensor_reduce(o_, i_, AX.X, ALU.add))
        for tok in range(16):
            b = self.bank()
            for sl2, (wv, wk) in enumerate(((wvA, wkA), (wvB, wkB))):
                for k in range(8):
                    self.mm(self.psf(b, 256 * sl2, 256 * sl2 + 256), self.hT[:, k, tok * 128:(tok + 1) * 128], wv[:, k, :], k == 0, k == 7,
                            reads=[wk, ("hT", k, tok // 4)], writes=[("ps", b)], skip_group_check=True)
            gi = tok % 2
            ge = gel[gi]
            self.act(ge, self.psf(b), AF.Gelu, reads=[("ps", b)], writes=[("sd", gi)])
            ge3 = ge.rearrange("p (g d) -> p g d", g=8)
            self.S.op("dve", red(st1, ge3), reads=[("sd", gi)], writes=["st1"])
            self.act(sqf, ge, AF.Square, reads=[("sd", gi)], writes=[("F", 0)])
            self.S.op("dve", red(st2, sqf.rearrange("p (g d) -> p g d", g=8)), reads=[("F", 0)], writes=["st2"])
            self.ts("dve", mean, st1, 1.0 / 64, None, ALU.mult, None, reads=["st1"], writes=["mean"])
            self.tt("dve", var, mean, mean, ALU.mult, reads=["mean"], writes=["var"])
            self.stt("dve", var, st2, 1.0 / 64, var, ALU.mult, ALU.subtract, reads=["st2", "var"], writes=["var"])
            self.act(rstd8, var, AF.Sqrt, reads=["var"], writes=["rstd8"], bias=self.eps_t[:, 0:1], scale=1.0)
            self.recip(rstd8, rstd8, reads=["rstd8"], writes=["rstd8"])
            xc3 = xc.rearrange("p (g d) -> p g d", g=8)
            self.tt("dve", xc3, ge3, mean.unsqueeze(2).broadcast_to([128, 8, 64]), ALU.subtract, reads=[("sd", gi), "mean"], writes=[("F", 1)])
            self.tt("dve", xhat[:, tok, :].rearrange("p (g d) -> p g d", g=8), xc3,
                    rstd8.unsqueeze(2).broadcast_to([128, 8, 64]), ALU.mult, reads=[("F", 1), "rstd8"], writes=[("xhat", tok)])
        for sl2 in range(2):
            wv, wk = slab(1536 + 256 * sl2)
            for j in range(2):
                c = 2 * sl2 + j
                for t in range(4):
                    sl = slice(512 * t, 512 * t + 512)
                    b = self.bank()
                    for k in range(8):
                        self.mm(self.psf(b), wv[:, k, 128 * j:128 * j + 128], self.hT[:, k, sl], k == 0, k == 7,
                                reads=[wk, ("hT", k, t)], writes=[("ps", b)])
                    self.act(gT[:, c, sl], self.psf(b), AF.Gelu, reads=[("ps", b)], writes=[("gT", c, t)])
        for g in range(8):
            c, hf = g // 2, g % 2
            pr = slice(64 * hf, 64 * hf + 64)
            for t in range(4):
                sl = slice(512 * t, 512 * t + 512)
                b = self.bank()
                for tt in range(4):
                    tok = 4 * t + tt
                    self.mm(self.PS[pr, 512 * b + 128 * tt:512 * b + 128 * tt + 128], xhat[:, tok, 64 * g:64 * g + 64], wsT[:, g, :], True, True,
                            reads=[("xhat", tok), ("wsT", g)], writes=[("ps", b)])
                self.stt("dve", tmpg[pr, :].rearrange("p (t n) -> p t n", t=4), self.PS[pr, 512 * b:512 * b + 512].rearrange("p (t n) -> p t n", t=4),
                         lng[pr, c:c + 1], btab[pr, c, :].unsqueeze(1).broadcast_to([64, 4, 128]), ALU.mult, ALU.add,
                         reads=[("ps", b), ("btab", g)], writes=[("F", 0)])
                self.tt("dve", gT[pr, c, sl], tmpg[pr, :], gT[pr, c, sl], ALU.mult, reads=[("F", 0), ("gT", c, t)], writes=[("gT", c, t)])
        for which in range(2):
            dst = qf if which == 0 else kf
            gcol = qg if which == 0 else kg
            name = "qf" if which == 0 else "kf"
            for sl2 in range(2):
                wv, wk = slab(512 * which + 256 * sl2)
                for j in range(2):
                    c = 2 * sl2 + j
                    for t in range(4):
                        sl = slice(512 * t, 512 * t + 512)
                        b = self.bank()
                        for k in range(8):
                            self.mm(self.psf(b), wv[:, k, 128 * j:128 * j + 128], self.hT[:, k, sl], k == 0, k == 7,
                                    reads=[wk, ("hT", k, t)], writes=[("ps", b)])
                        si = t % 2
                        self.act(sqb[si], self.psf(b), AF.Square, reads=[("ps", b)], writes=[("sq", si)])
                        self.act(qgb[si], self.psf(b), AF.Copy, reads=[("ps", b)], writes=[("B", si)])
                        b2 = self.bank()
                        self.mm(self.psf(b2), self.bo_bf, sqb[si], True, True, reads=[("sq", si)], writes=[("ps", b2)])
                        self.act(sd[0], self.psf(b2), AF.Sqrt, reads=[("ps", b2)], writes=[("sd", 0)], bias=self.eps_t[:, 0:1], scale=1.0 / 64)
                        self.recip(sd[1], sd[0], reads=[("sd", 0)], writes=[("sd", 1)])
                        self.stt("dve", qn, qgb[si], gcol, sd[1], ALU.mult, ALU.mult, reads=[("B", si), ("sd", 1)], writes=[("B", 2)])
                        b3 = self.bank()
                        self.mm(self.psf(b3), self.pm_bf, qn, True, True, reads=[("B", 2)], writes=[("ps", b3)])
                        self.tt("dve", t1, qn, self.ropeC[:, sl], ALU.mult, reads=[("B", 2)], writes=[("F", 0)])
                        self.tt("dve", t2, self.psf(b3), self.ropeS[:, sl], ALU.mult, reads=[("ps", b3)], writes=[("F", 1)])
                        alias = [("xhat", 4 * c + u) for u in range(4)] if which == 0 else []
                        self.tt("dve", dst[:, c, sl], t1, t2, ALU.add, reads=[("F", 0), ("F", 1)],
                                writes=[(name, c, hf, 2 * t + u) for hf in range(2) for u in range(2)] + alias)
        wv0, wk0 = slab(1024)
        wv1, wk1 = slab(1280)
        for tok in range(16):
            b = self.bank()
            for sl2, (wv, wk) in enumerate(((wv0, wk0), (wv1, wk1))):
                for k in range(8):
                    self.mm(self.psf(b, 256 * sl2, 256 * sl2 + 256), self.hT[:, k, tok * 128:(tok + 1) * 128], wv[:, k, :], k == 0, k == 7,
                            reads=[wk, ("hT", k, tok // 4)], writes=[("ps", b)], skip_group_check=True)
            self.cp("act", self.hT[:, 4:8, tok * 128:(tok + 1) * 128], self.psf(b).rearrange("p (a n) -> p a n", a=4),
                    reads=[("ps", b)], writes=[("hT", 4 + a_, tok // 4) for a_ in range(4)])

        def vsl(kc, h):
            c0 = kc * 128 + 64 * (h % 2)
            return self.hT[:, 4 + h // 2, c0:c0 + 64]
        negT = [self.hT.rearrange("p k n -> p (k n)")[:, 2048 * s_:2048 * s_ + 2048] for s_ in range(3)]
        allk = lambda nm, c: [(nm, c, hf, ib) for hf in range(2) for ib in range(8)]
        for c in range(4):
            self.S.op("dve", (lambda o_, i_: lambda e: e.tensor_reduce(o_, i_, AX.X, ALU.add))(kmean[:, c, :], kf[:, c, :].rearrange("p (n k) -> p n k", n=8)),
                      reads=allk("kf", c), writes=[("kmean", c)])
            self.memset("pool", kmbd[:, c, :], 0.0, writes=[("kmbd", c)])
            for hf in range(2):
                pr = slice(64 * hf, 64 * hf + 64)
                self.ts("dve", kmbd[pr, c, 8 * hf:8 * hf + 8], kmean[pr, c, :], 1.0 / 256, None, ALU.mult, None, reads=[("kmean", c)], writes=[("kmbd", c)])
        for tok in range(8, 16):
            o = tok // 2
            bg = self.bank()
            for c in range(4):
                self.mm(self.psf(bg, 16 * c, 16 * c + 16), qf[:, c, tok * 128:(tok + 1) * 128], kmbd[:, c, :], True, True,
                        reads=allk("qf", c) + [("kmbd", c)], writes=[("ps", bg)], skip_group_check=True)
            self.cp("dve", gsb, self.psf(bg, 0, 64), reads=[("ps", bg)], writes=["gsb"])
            g3 = gsb.rearrange("p (h n) -> p h n", h=8)[:, :, 0:o]
            cm4 = cmp_[:, 0:8 * o * o].rearrange("p (h n m) -> p h n m", h=8, n=o)
            self.tt("dve", cm4, g3.unsqueeze(2).broadcast_to([128, 8, o, o]), g3.unsqueeze(3).broadcast_to([128, 8, o, o]), ALU.is_gt,
                    reads=["gsb"], writes=[("F", 0)])
            cn3 = cnt[:, 0:8 * o].rearrange("p (h n) -> p h n", h=8)
            self.S.op("dve", (lambda o_, i_: lambda e: e.tensor_reduce(o_, i_, AX.X, ALU.add))(cn3, cm4), reads=[("F", 0)], writes=["cnt"])
            nm = nm_tok[tok % 2]
            self.memset("pool", nm, 0.0, writes=[("nm", tok % 2)])
            for s_ in range(3):
                ng = 3 if s_ < 2 else 2
                self.ts("dve", nm[:, 128 * s_:128 * s_ + 96].rearrange("p (g n) -> p g n", n=32)[:, 0:ng, 0:o], cn3[:, 3 * s_:3 * s_ + ng, :],
                        3.0, NEG, ALU.is_ge, ALU.mult, reads=["cnt"], writes=[("nm", tok % 2)])
            bt = self.bank()
            pb = self.psb(bt)
            for s_ in range(3):
                self.tr(pb[:, 128 * s_:128 * s_ + 128], nm[:, 128 * s_:128 * s_ + 128], self.ident_bf, reads=[("nm", tok % 2)], writes=[("ps", bt)])
            for s_ in range(3):
                self.cp("act", negT[s_][:, tok * 128:(tok + 1) * 128], pb[:, 128 * s_:128 * s_ + 128], reads=[("ps", bt)], writes=[("negT", s_, tok), ("hT", s_, tok // 4)])
        scale = 0.125
        items = [(h, ib, jb) for h in range(8) for ib in range(8) for jb in range(ib + 1)]
        st_ = {}

        def score(n):
            h, ib, jb = items[n]
            c, hf = h // 2, h % 2
            pr = slice(64 * hf, 64 * hf + 64)
            s_, g_ = h // 3, h % 3
            gr = slice(32 * g_, 32 * g_ + 32)
            qsl = slice(256 * ib, 256 * ib + 256)
            b = self.bank()
            pi = n % 3
            kc0, kc1 = 2 * jb, 2 * jb + 1
            rd = [("kf", c, hf, jb), ("qf", c, hf, ib)]
            self.mm(self.psf(b, 0, 256), kf[pr, c, kc0 * 128:kc0 * 128 + 128], qf[pr, c, qsl], True, False,
                    reads=rd, writes=[("ps", b)], skip_group_check=True)
            if jb < ib:
                self.mm(self.psf(b, 256, 512), kf[pr, c, kc1 * 128:kc1 * 128 + 128], qf[pr, c, qsl], False, ib <= 3,
                        reads=rd, writes=[("ps", b)], skip_group_check=True)
                if ib > 3:
                    self.mm(self.psf(b), self.e8_bf[gr, jb, :],
                            negT[s_][gr, qsl].unsqueeze(1).broadcast_to([32, 2, 256]), False, True,
                            reads=[("negT", s_, 2 * ib), ("negT", s_, 2 * ib + 1)], writes=[("ps", b)], skip_group_check=True)
                w = 512
            else:
                self.mm(self.psf(b, 256, 384), kf[pr, c, kc1 * 128:kc1 * 128 + 128], qf[pr, c, 256 * ib + 128:256 * ib + 256], False, False,
                        reads=rd, writes=[("ps", b)], skip_group_check=True)
                self.mm(self.psf(b, 0, 128), self.ident_bf, self.cn_bf, False, False,
                        reads=[], writes=[("ps", b)], skip_group_check=True)
                self.mm(self.psf(b, 256, 384), self.ident_bf, self.cn_bf, False, True,
                        reads=[], writes=[("ps", b)], skip_group_check=True)
                w = 384
            self.act(pT[pi][:, 0:w], self.psf(b, 0, w), AF.Exp, reads=[("ps", b)], writes=[("B", pi)], scale=scale)

        def pv(n):
            h, ib, jb = items[n]
            c, hf = h // 2, h % 2
            pr = slice(64 * hf, 64 * hf + 64)
            qsl = slice(256 * ib, 256 * ib + 256)
            pi = n % 3
            kc0, kc1 = 2 * jb, 2 * jb + 1
            if jb == 0:
                st_["banks"] = self.reserve(2)
            ba, bd = st_["banks"]
            acc = self.PS[pr, 512 * ba:512 * ba + 256]
            den = self.PS[pr, 512 * bd:512 * bd + 256]
            last = (jb == ib)
            vk0 = ("hT", 4 + h // 2, kc0 // 4)
            vk1 = ("hT", 4 + h // 2, kc1 // 4)
            self.mm(acc, vsl(kc0, h), pT[pi][:, 0:256], jb == 0, False, reads=[("B", pi), vk0], writes=[("ps", ba)], skip_group_check=True)
            self.mm(den, self.ones_bf[:, 0:64], pT[pi][:, 0:256], jb == 0, False, reads=[("B", pi)], writes=[("ps", bd)], skip_group_check=True)
            if not last:
                self.mm(acc, vsl(kc1, h), pT[pi][:, 256:512], False, False, reads=[("B", pi), vk1], writes=[("ps", ba)], skip_group_check=True)
                self.mm(den, self.ones_bf[:, 0:64], pT[pi][:, 256:512], False, False, reads=[("B", pi)], writes=[("ps", bd)], skip_group_check=True)
            else:
                self.mm(self.PS[pr, 512 * ba + 128:512 * ba + 256], vsl(kc1, h), pT[pi][:, 256:384], False, True,
                        reads=[("B", pi), vk1], writes=[("ps", ba)], skip_group_check=True)
                self.mm(self.PS[pr, 512 * bd + 128:512 * bd + 256], self.ones_bf[:, 0:64], pT[pi][:, 256:384], False, True,
                        reads=[("B", pi)], writes=[("ps", bd)], skip_group_check=True)
                self.recip(rec[pr, :], den, reads=[("ps", bd)], writes=[("rec", hf)])
                self.tt("dve", qf[pr, c, qsl], acc, rec[pr, :], ALU.mult, reads=[("ps", ba), ("rec", hf)], writes=[("qf", c, hf, ib)])
                self.release([ba, bd])

        score(0)
        for n in range(len(items)):
            if n + 1 < len(items):
                score(n + 1)
            pv(n)
        for cg in range(4):
            wb, wk = self.wslab()
            wv = wb.rearrange("p (k n) -> p k n", k=8)
            self.load_cols(wout, [(cg * 256, 256)], wv, None, wk)
            for j in range(2):
                dk = cg * 2 + j
                for t in range(4):
                    sl = slice(512 * t, 512 * t + 512)
                    b = self.bank()
                    for k in range(8):
                        if k < 4:
                            rhs = qf[:, k, sl]
                            rk = [("qf", k, hf, ib) for hf in range(2) for ib in (2 * t, 2 * t + 1)]
                        else:
                            rhs = gT[:, k - 4, sl]
                            rk = [("gT", k - 4, t)]
                        self.mm(self.psf(b), wv[:, k, j * 128:(j + 1) * 128], rhs, k == 0, k == 7, reads=[wk] + rk, writes=[("ps", b)])
                    self.resid_add(b, dk, t)

    def build(self):
        nc = self.nc
        x_d = self.din("x", [S_LEN, D])
        mem_d = self.din("mem", [NMEM, D])
        pos_d = self.din("positions", [1, S_LEN], I32)
        cst_d = self.din("consts", [128, C_END])
        G_d = self.din("gains", [128, DEPTH * 32])
        xqg_d = self.din("xa_qg", [128, DEPTH * 2])
        xkg_d = self.din("xa_kg", [128, DEPTH * 2])
        evq_d = self.din("ev_qg", [128, 2])
        evk_d = self.din("ev_kg", [128, 2])
        evlng_d = self.din("ev_lng", [128, 8])
        evlnb_d = self.din("ev_lnb", [128, 8])
        og_d = self.din("od_og", [128, 16])
        self.din("ev_w_in", [2, D, 2560]); self.din("ev_w_s", [2, 8, 128, 128]); self.din("ev_b_s", [2, 8, 128])
        self.din("ev_w_out", [2, D, D]); self.din("od_w_in", [2, D, 3072]); self.din("od_w_g1", [2, D, 16])
        self.din("od_w_g2", [2, 16, 512]); self.din("od_b_g", [2, 512]); self.din("od_w_out", [2, D, D])
        self.din("xa_w_q", [DEPTH, D, D]); self.din("xa_w_kv", [DEPTH, D, 2 * D]); self.din("xa_w_out", [DEPTH, D, D])
        self.din("ffn_w_gu", [DEPTH, D, 2 * DFF]); self.din("ffn_w_down", [DEPTH, DFF, D])
        out_d = nc.dram_tensor("out", [S_LEN, D], F32, kind="ExternalOutput").ap()

        with ExitStack() as es:
            def sb(name, shape, dt=F32):
                return es.enter_context(nc.sbuf_tensor(name, shape, dt))[:]
            self.S = S = Sched(nc)
            self.PS = es.enter_context(nc.psum_tensor("ps", [128, 4096], F32))[:]
            self.xT = sb("xT", [128, 8 * 2048]).rearrange("p (k n) -> p k n", k=8)
            self.hT = sb("hT", [128, 8 * 2048], BF16).rearrange("p (k n) -> p k n", k=8)
            self.memT = sb("memT", [128, 8 * 256], BF16).rearrange("p (k n) -> p k n", k=8)
            cf = sb("cf", [128, 386])
            self.G = sb("G", [128, DEPTH * 32]).rearrange("p (l n) -> p l n", l=DEPTH)
            self.xqg = sb("xqg", [128, DEPTH * 2]).rearrange("p (l n) -> p l n", l=DEPTH)
            self.xkg = sb("xkg", [128, DEPTH * 2]).rearrange("p (l n) -> p l n", l=DEPTH)
            self.evq = sb("evq", [128, 2]).rearrange("p (l n) -> p l n", l=2)
            self.evk = sb("evk", [128, 2]).rearrange("p (l n) -> p l n", l=2)
            self.evlng = sb("evlng", [128, 8]).rearrange("p (l n) -> p l n", l=2)
            self.evlnb = sb("evlnb", [128, 8]).rearrange("p (l n) -> p l n", l=2)
            self.ogain = sb("ogain", [128, 16]).rearrange("p (l n) -> p l n", l=2)
            self.ident_f = cf[:, 0:128]
            self.C01 = cf[:, 128:256]
            self.U01 = cf[:, 256:384]
            invf_t = cf[:, 384:385]
            sgn_t = cf[:, 385:386]
            cbf = sb("cbf", [128, 7 * 128 + 1024], BF16)
            self.ident_bf = cbf[:, 0:128]
            self.bo_bf = cbf[:, 128:256]
            self.pm_bf = cbf[:, 256:384]
            self.cn_bf = cbf[:, 384:512]
            self.U_bf = cbf[:, 640:768]
            self.ones_bf = cbf[:, 768:896]
            self.e8_bf = cbf[:, 896:1920].rearrange("p (n m) -> p n m", n=8)
            self.eps_t = sb("eps_t", [128, 1])
            self.one_t = sb("one_t", [128, 1])
            self.ropeC = sb("ropeC", [128, 2048], BF16)
            self.ropeS = sb("ropeS", [128, 2048], BF16)
            self.STAGE = [sb("stage0", [128, 1024]), sb("stage1", [128, 1024])]
            self.WB = [sb("wb%d" % i, [128, 2048], BF16) for i in range(3)]
            self.SQ = [sb("sq%d" % i, [128, 512], BF16) for i in range(2)]
            self.SD = [sb("sd%d" % i, [128, 512]) for i in range(2)]
            self.A_WORDS = (nc.sbuf_bytes_remaining // 4) - 64
            self.ARENA = sb("arena", [128, self.A_WORDS])

            self.a_reset()
            cst = self.a_f32(C_END)
            self.dma(cst, cst_d, slot=("c", 0), writes=["cst"])
            self.dma(self.G.rearrange("p l n -> p (l n)"), G_d, slot=("c", 0), writes=["G"])
            self.dma(self.xqg.rearrange("p l n -> p (l n)"), xqg_d, slot=("c", 0), writes=["G"])
            self.dma(self.xkg.rearrange("p l n -> p (l n)"), xkg_d, slot=("c", 0), writes=["G"])
            self.dma(self.evq.rearrange("p l n -> p (l n)"), evq_d, slot=("c", 0), writes=["G"])
            self.dma(self.evk.rearrange("p l n -> p (l n)"), evk_d, slot=("c", 0), writes=["G"])
            self.dma(self.evlng.rearrange("p l n -> p (l n)"), evlng_d, slot=("c", 0), writes=["G"])
            self.dma(self.evlnb.rearrange("p l n -> p (l n)"), evlnb_d, slot=("c", 0), writes=["G"])
            self.dma(self.ogain.rearrange("p l n -> p (l n)"), og_d, slot=("c", 0), writes=["G"])
            self.cp("dve", cbf[:, 0:1920], cst[:, 0:1920], reads=["cst"], writes=["cbf"])
            self.cp("dve", cf[:, 0:128], cst[:, C_ID:C_ID + 128], reads=["cst"], writes=["cf"])
            self.cp("dve", cf[:, 128:384], cst[:, C_C01:C_C01 + 256], reads=["cst"], writes=["cf"])
            self.cp("dve", cf[:, 384:386], cst[:, C_INVF:C_INVF + 2], reads=["cst"], writes=["cf"])
            self.memset("dve", self.eps_t, EPS, writes=["eps"])
            self.memset("dve", self.one_t, 1.0, writes=["eps"])
            S.barrier()

            self.a_reset()
            posi = self.a_f32(2048).bitcast(I32)
            posf = self.a_f32(2048)
            tq = self.a_f32(2048)
            tn_i = self.a_f32(2048).bitcast(I32)
            tn = self.a_f32(2048)
            self.dma(posi, pos_d[0:1, :].partition_broadcast(128), slot=("c", 1), writes=["posi"])
            self.cp("dve", posf, posi, reads=["posi"], writes=["posf"])
            inv2pi = float(1.0 / (2.0 * np.pi))
            for which in range(2):
                self.ts("dve", tq, posf, invf_t, None, ALU.mult, None, reads=["posf"], writes=["tq"])
                self.ts("dve", tq, tq, inv2pi, 0.25 if which == 0 else 0.0, ALU.mult, ALU.add, reads=["tq"], writes=["tq"])
                self.cp("dve", tn_i, tq, reads=["tq"], writes=["tn_i"])
                self.cp("dve", tn, tn_i, reads=["tn_i"], writes=["tn"])
                self.tt("dve", tq, tq, tn, ALU.subtract, reads=["tq", "tn"], writes=["tq"])
                self.ts("dve", tq, tq, -0.4999, 0.4999, ALU.max, ALU.min, reads=["tq"], writes=["tq"])
                if which == 0:
                    self.act(self.ropeC, tq, AF.Sin, reads=["tq"], writes=["ropeC"], scale=float(2.0 * np.pi))
                else:
                    self.act(tn, tq, AF.Sin, reads=["tq"], writes=["tn"], scale=float(2.0 * np.pi))
                    self.ts("dve", self.ropeS, tn, sgn_t, None, ALU.mult, None, reads=["tn"], writes=["ropeS"])
            S.barrier()

            self.a_reset()
            mt_f = self.a_f32(1024)
            mt_j = self.a_f32(1024)
            mt_b = self.a_bf(1024)
            mss = self.a_f32(2)
            for mt in range(2):
                self.dma(mt_f, mem_d[mt * 128:(mt + 1) * 128, :], slot=("c", 2), writes=["mt_f"])
                self.act(mt_j, mt_f, AF.Square, reads=["mt_f"], writes=["mt_j", "mss"], accum_out=mss[:, 0:1])
                self.act(mss[:, 1:2], mss[:, 0:1], AF.Sqrt, reads=["mss"], writes=["mss2"], bias=self.eps_t[:, 0:1], scale=1.0 / D)
                self.recip(mss[:, 1:2], mss[:, 1:2], reads=["mss2"], writes=["mss2"])
                self.ts("dve", mt_b, mt_f, mss[:, 1:2], None, ALU.mult, None, reads=["mt_f", "mss2"], writes=["mt_b"])
                for kk in range(0, 8, 4):
                    b = self.bank()
                    pb = self.psb(b)
                    for k in range(kk, kk + 4):
                        self.tr(pb[:, 128 * (k - kk):128 * (k - kk) + 128], mt_b[:, 128 * k:128 * k + 128], self.ident_bf, reads=["mt_b"], writes=[("ps", b)])
                    self.cp("dve", self.memT[:, kk:kk + 4, mt * 128:(mt + 1) * 128], pb[:, 0:512].rearrange("p (k n) -> p k n", k=4),
                            reads=[("ps", b)], writes=["memT"])
            S.barrier()

            self.a_reset()
            xs = [self.a_f32(1024) for _ in range(4)]
            for t in range(4):
                for tt in range(4):
                    tok = 4 * t + tt
                    self.dma(xs[tt], x_d[tok * 128:(tok + 1) * 128, :], slot=("xs", tt), writes=[("xs", tt)])
                for k in range(8):
                    b = self.bank()
                    for tt in range(4):
                        self.tr(self.psf(b, 128 * tt, 128 * tt + 128), xs[tt][:, 128 * k:128 * k + 128], self.ident_f, reads=[("xs", tt)], writes=[("ps", b)])
                    self.cp("dve" if k % 2 == 0 else "act", self.xT[:, k, 512 * t:512 * t + 512], self.psf(b), reads=[("ps", b)], writes=[("xT", k, t)])

            for l in self.layers:
                if "mix" in self.subs:
                    if l % 2 == 0:
                        self.even(l)
                    else:
                        self.gla(l)
                if "xa" in self.subs:
                    self.xattn(l)
                if "ffn" in self.subs:
                    self.ffn(l)

            S.barrier()
            self.a_reset()
            os_ = [self.a_f32(1024) for _ in range(2)]
            stores = []
            for tok in range(16):
                ob = os_[tok % 2]
                t = tok // 4
                for kk in range(0, 8, 4):
                    b = self.bank()
                    for k in range(kk, kk + 4):
                        self.tr(self.psf(b, 128 * (k - kk), 128 * (k - kk) + 128), self.xT[:, k, tok * 128:(tok + 1) * 128], self.ident_f,
                                reads=[("xT", k, t)], writes=[("ps", b)])
                    self.cp("dve" if kk == 0 else "act", ob[:, 128 * kk:128 * kk + 512], self.psf(b), reads=[("ps", b)], writes=[("os", tok % 2, kk)])
                stores.append(self.dma(out_d[tok * 128:(tok + 1) * 128, :], ob, slot=("out", tok % 2), reads=[("os", tok % 2, 0), ("os", tok % 2, 4)]))
            S.final_wait("sp", stores)
            S.finalize(es)
            block = es.enter_context(nc.Block())
            S.emit(block)
        return nc


def _pk(v, n):
    v = np.asarray(v, np.float32)
    lead = v.shape[:-1]
    a = v.reshape(lead + (n, 128))
    a = np.moveaxis(a, -1, 0)
    return np.ascontiguousarray(a.reshape(128, -1))


def _host_inputs(inp):
    f = lambda k: np.ascontiguousarray(np.asarray(inp[k], np.float32))
    gains = np.concatenate([_pk(inp["norm_mix"], 8).reshape(128, DEPTH, 8), _pk(inp["norm_mem_q"], 8).reshape(128, DEPTH, 8),
                            _pk(inp["norm_mem_kv"], 8).reshape(128, DEPTH, 8), _pk(inp["norm_ffn"], 8).reshape(128, DEPTH, 8)], axis=2)
    shared = {
        "consts": _consts(),
        "gains": np.ascontiguousarray(gains.reshape(128, DEPTH * 32)),
        "xa_qg": _pk(inp["xa_q_gain"], 2),
        "xa_kg": _pk(inp["xa_k_gain"], 2),
        "ev_qg": np.ascontiguousarray(np.tile(np.asarray(inp["ev_q_gain"], np.float32), (1, 2)).T),
        "ev_kg": np.ascontiguousarray(np.tile(np.asarray(inp["ev_k_gain"], np.float32), (1, 2)).T),
        "ev_lng": _pk(np.asarray(inp["ev_ln_g"], np.float32).reshape(2, 512), 4),
        "ev_lnb": _pk(np.asarray(inp["ev_ln_b"], np.float32).reshape(2, 512), 4),
        "od_og": _pk(np.asarray(inp["od_o_gain"], np.float32).reshape(2, 1024), 8),
    }
    for k in ("ev_w_in", "ev_w_s", "ev_b_s", "ev_w_out", "od_w_in", "od_w_g1", "od_w_g2", "od_b_g", "od_w_out",
              "xa_w_q", "xa_w_kv", "xa_w_out", "ffn_w_gu", "ffn_w_down"):
        shared[k] = f(k)
    x = f("x")
    mem = f("mem")
    pos = np.ascontiguousarray(np.asarray(inp["positions"], np.int32))
    maps = []
    for b in range(8):
        m = dict(shared)
        m["x"] = x[b]
        m["mem"] = mem[b]
        m["positions"] = pos[b:b + 1]
        maps.append(m)
    return maps


_NC_CACHE = {}


def run_layers(inp, layers, subs=("mix", "xa", "ffn"), cores=8, stop_at=0):
    key = (tuple(layers), tuple(subs), stop_at)
    if key not in _NC_CACHE:
        bld = Builder(layers, subs)
        bld.stop_at = stop_at
        _NC_CACHE[key] = bld.build()
    nc = _NC_CACHE[key]
    maps = _host_inputs(inp)[:cores]
    res = run_bass_kernel_spmd(nc, maps, core_ids=list(range(cores)))
    return np.stack([np.asarray(r["out"], np.float32) for r in res.results], axis=0)


def kernel(**inputs):
    return run_layers(inputs, list(range(DEPTH)))
```

```python
import numpy as np
from contextlib import ExitStack
import concourse.bass as bass
import concourse.mybir as mybir
from concourse.bass_utils import run_bass_kernel_spmd

F32 = mybir.dt.float32
BF16 = mybir.dt.bfloat16
I32 = mybir.dt.int32
AF = mybir.ActivationFunctionType
ALU = mybir.AluOpType
AX = mybir.AxisListType

S_LEN = 2048
D = 1024
NMEM = 256
DEPTH = 4
DFF = 2816
EPS = 1e-6
NEG = -30000.0
ENGS = ("pe", "act", "dve", "pool", "sp")


class _Op:
    __slots__ = ("eng", "fn", "deps", "is_dma", "slot", "ticket", "needs_inc", "waits")

    def __init__(self, eng, fn, is_dma, slot):
        self.eng = eng
        self.fn = fn
        self.deps = ()
        self.is_dma = is_dma
        self.slot = slot
        self.ticket = None
        self.needs_inc = False
        self.waits = None


class Sched:
    def __init__(self, nc):
        self.nc = nc
        self.ops = {e: [] for e in ENGS}
        self.last_w = {}
        self.readers = {}
        self.all_ops = []
        self.slot_last = {}

    def op(self, eng, fn, reads=(), writes=(), dma_slot=None):
        o = _Op(eng, fn, dma_slot is not None, dma_slot)
        if dma_slot is not None:
            self.slot_last[dma_slot] = o
        deps = set()
        for r in reads:
            w = self.last_w.get(r)
            if w is not None:
                deps.add(w)
        for r in writes:
            w = self.last_w.get(r)
            if w is not None:
                deps.add(w)
            rl = self.readers.get(r)
            if rl:
                deps.update(rl)
        deps = set((self.slot_last[d.slot] if d.is_dma else d) for d in deps)
        deps.discard(o)
        o.deps = tuple(deps)
        for r in reads:
            self.readers.setdefault(r, []).append(o)
        for r in writes:
            self.last_w[r] = o
            self.readers[r] = []
        self.all_ops.append(o)
        self.ops[eng].append(o)
        return o

    def barrier(self):
        lasts = []
        for e in ENGS:
            for o in reversed(self.ops[e]):
                if not o.is_dma and o.fn is not None:
                    lasts.append(o)
                    break
        slots = {}
        for o in self.all_ops:
            if o.is_dma:
                slots[o.slot] = o
        lasts.extend(slots.values())
        for e in ENGS:
            b = _Op(e, None, False, None)
            b.deps = tuple(lasts)
            self.all_ops.append(b)
            self.ops[e].append(b)
        self.last_w = {}
        self.readers = {}

    def final_wait(self, eng, ops):
        b = _Op(eng, None, False, None)
        b.deps = tuple(ops)
        self.all_ops.append(b)
        self.ops[eng].append(b)

    @staticmethod
    def _skip(d, o):
        return (not d.is_dma) and d.eng == o.eng and d.eng == "pe"

    def finalize(self, es):
        nc = self.nc
        for o in self.all_ops:
            for d in o.deps:
                if d.is_dma or self._skip(d, o):
                    continue
                d.needs_inc = True
        self.sems = {e: es.enter_context(nc.semaphore("prog_" + e)) for e in ENGS}
        cnt = {e: 0 for e in ENGS}
        slot_cnt = {}
        self.slot_sems = {}
        for o in self.all_ops:
            if o.is_dma:
                if o.slot not in self.slot_sems:
                    self.slot_sems[o.slot] = es.enter_context(nc.semaphore("dma_%d" % len(self.slot_sems)))
                    slot_cnt[o.slot] = 0
                slot_cnt[o.slot] += 16
                o.ticket = slot_cnt[o.slot]
            elif o.needs_inc:
                cnt[o.eng] += 1
                o.ticket = cnt[o.eng]
        waited = {e: {} for e in ENGS}
        for o in self.all_ops:
            need = {}
            for d in o.deps:
                if d.is_dma:
                    key = ("slot", d.slot)
                else:
                    if self._skip(d, o):
                        continue
                    key = ("eng", d.eng)
                if need.get(key, 0) < d.ticket:
                    need[key] = d.ticket
            w = []
            wd = waited[o.eng]
            for key, v in need.items():
                if wd.get(key, 0) >= v:
                    continue
                wd[key] = v
                w.append((key, v))
            o.waits = w

    def _sem(self, key):
        return self.sems[key[1]] if key[0] == "eng" else self.slot_sems[key[1]]

    def emit(self, block):
        def body(engname):
            def f(e):
                for o in self.ops[engname]:
                    for key, v in o.waits:
                        e.wait_ge(self._sem(key), v)
                    if o.fn is None:
                        continue
                    ins = o.fn(e)
                    if o.is_dma:
                        ins.then_inc(self.slot_sems[o.slot], 16)
                    elif o.needs_inc:
                        ins.then_inc(self.sems[engname], 1)
            return f
        block.tensor(body("pe"))
        block.scalar(body("act"))
        block.vector(body("dve"))
        block.gpsimd(body("pool"))
        block.sync(body("sp"))


C_ID, C_BO, C_PM, C_CN, C_C01, C_U01, C_ONE, C_E8, C_INVF, C_SGN, C_END = (
    0, 128, 256, 384, 512, 640, 768, 896, 1920, 1921, 1922)


def _consts():
    c = np.zeros((128, C_END), np.float32)
    p = np.arange(128)
    c[:, C_ID:C_ID + 128] = np.eye(128, dtype=np.float32)
    c[:, C_BO:C_BO + 128] = (p[:, None] // 64 == p[None, :] // 64).astype(np.float32)
    pm = np.zeros((128, 128), np.float32)
    for m in range(128):
        r = m % 64
        if r < 8:
            pm[m + 8, m] = 1.0
        elif r < 16:
            pm[m - 8, m] = 1.0
    c[:, C_PM:C_PM + 128] = pm
    c[:, C_CN:C_CN + 128] = np.where(p[:, None] <= p[None, :], 0.0, NEG)
    c[:, C_C01:C_C01 + 128] = (p[:, None] <= p[None, :]).astype(np.float32)
    c[:, C_U01:C_U01 + 128] = ((p[:, None] <= p[None, :]) & (p[:, None] // 64 == p[None, :] // 64)).astype(np.float32)
    c[:, C_ONE:C_ONE + 128] = 1.0
    e8 = np.zeros((128, 8, 128), np.float32)
    for n in range(8):
        e8[(p % 32) == n, n, :] = 1.0
    c[:, C_E8:C_E8 + 1024] = e8.reshape(128, 1024)
    half = 8
    inv_freq = (np.float32(500000.0) ** (-np.arange(half, dtype=np.float32) * np.float32(2.0) / np.float32(16))).astype(np.float32)
    r = p % 64
    c[:, C_INVF] = np.where(r < 16, inv_freq[r % 8], 0.0)
    c[:, C_SGN] = np.where(r < 8, -1.0, np.where(r < 16, 1.0, 0.0))
    return c


class Builder:
    def __init__(self, layers, subs=("mix", "xa", "ffn")):
        self.layers = list(layers)
        self.subs = subs
        self.nc = bass.Bass("TRN2", target_bir_lowering=False)
        self.dram = {}
        self.bank_ctr = 0
        self.reserved = set()
        self.conv_dve = False
        self.stop_at = 0
        self.stage_ctr = 0
        self.wb_ctr = 0
        self.uid = 0

    def din(self, name, shape, dt=F32):
        t = self.nc.dram_tensor(name, list(shape), dt, kind="ExternalInput").ap()
        self.dram[name] = t
        return t

    def bank(self):
        while True:
            b = self.bank_ctr % 8
            self.bank_ctr += 1
            if b not in self.reserved:
                return b

    def reserve(self, n):
        out = []
        for _ in range(n):
            b = self.bank()
            self.reserved.add(b)
            out.append(b)
        return out

    def release(self, banks):
        for b in banks:
            self.reserved.discard(b)

    def psf(self, b, lo=0, hi=512):
        return self.PS[:, 512 * b + lo:512 * b + hi]

    def psb(self, b):
        return self.PS[:, 512 * b:512 * b + 512].bitcast(BF16)

    def key(self, name):
        self.uid += 1
        return (name, self.uid)

    def mm(self, out, lhsT, rhs, start, stop, reads, writes, **kw):
        return self.S.op("pe", lambda e: e.matmul(out, lhsT, rhs, start=start, stop=stop, **kw), reads=reads, writes=writes)

    def tr(self, out, in_, ident, reads, writes):
        return self.S.op("pe", lambda e: e.transpose(out, in_, ident), reads=reads, writes=writes)

    def act(self, out, in_, func, reads, writes, **kw):
        return self.S.op("act", lambda e: e.activation(out=out, in_=in_, func=func, **kw), reads=reads, writes=writes)

    def tt(self, eng, out, in0, in1, op, reads, writes):
        return self.S.op(eng, lambda e: e.tensor_tensor(out, in0, in1, op), reads=reads, writes=writes)

    def ts(self, eng, out, in0, s1, s2, op0, op1, reads, writes):
        if op1 is None:
            return self.S.op(eng, lambda e: e.tensor_scalar(out, in0, s1, None, op0), reads=reads, writes=writes)
        return self.S.op(eng, lambda e: e.tensor_scalar(out, in0, s1, s2, op0, op1), reads=reads, writes=writes)

    def stt(self, eng, out, in0, scalar, in1, op0, op1, reads, writes):
        return self.S.op(eng, lambda e: e.scalar_tensor_tensor(out, in0, scalar, in1, op0, op1), reads=reads, writes=writes)

    def cp(self, eng, out, in_, reads, writes):
        if eng == "act":
            return self.S.op("act", lambda e: e.copy(out, in_), reads=reads, writes=writes)
        return self.S.op(eng, lambda e: e.tensor_copy(out, in_), reads=reads, writes=writes)

    def recip(self, out, in_, reads, writes):
        return self.S.op("dve", lambda e: e.reciprocal(out, in_), reads=reads, writes=writes)

    def memset(self, eng, ap, val, writes):
        return self.S.op(eng, lambda e: e.memset(ap, val), writes=writes)

    def dma(self, out, in_, slot, reads=(), writes=()):
        return self.S.op("sp", lambda e: e.dma_start(out=out, in_=in_), reads=reads, writes=writes, dma_slot=slot)

    def a_reset(self):
        self.a_off = 0

    def a_f32(self, n):
        ap = self.ARENA[:, self.a_off:self.a_off + n]
        self.a_off += n
        assert self.a_off <= self.A_WORDS, ("arena overflow", self.a_off, self.A_WORDS)
        return ap

    def a_bf(self, n):
        w = (n + 1) // 2
        return self.a_f32(w).bitcast(BF16)

    def _conv(self, i, dst, src, g, skey, wkey):
        e = ("act", "dve")[i % 2]
        if g is None:
            self.cp(e, dst, src, reads=[skey], writes=[wkey])
        elif e == "act":
            self.S.op("act", lambda en: en.activation(out=dst, in_=src, func=AF.Copy, scale=g), reads=[skey], writes=[wkey])
        else:
            self.ts(e, dst, src, g, None, ALU.mult, None, reads=[skey], writes=[wkey])

    def load_cols(self, wsrc, ranges, dst, gain, wkey):
        for kh in range(2):
            s = self.stage_ctr % 2
            self.stage_ctr += 1
            st = self.STAGE[s].rearrange("p (k n) -> p k n", k=4)
            skey = ("stage", s)
            off = 0
            for (c0, w) in ranges:
                self.dma(st[:, :, off:off + w], wsrc[:, 4 * kh:4 * kh + 4, c0:c0 + w], slot=("stage", s), writes=[skey])
                off += w
            for kk in range(4):
                k = 4 * kh + kk
                g = None if gain is None else gain[:, k:k + 1]
                self._conv(k, dst[:, k, :], st[:, kk, :], g, skey, wkey)

    def load_rows(self, wsrc, dst, n, gain, wkey):
        for i in range(n):
            s = self.stage_ctr % 2
            self.stage_ctr += 1
            st = self.STAGE[s]
            skey = ("stage", s)
            self.dma(st, wsrc[:, i, :], slot=("stage", s), writes=[skey])
            g = None if gain is None else gain[:, i:i + 1]
            for q in range(2):
                self._conv(2 * i + q, dst[:, i, 512 * q:512 * q + 512], st[:, 512 * q:512 * q + 512], g, skey, wkey)

    def slabs(self, n, loadfn):
        def issue(i):
            wb, wk = self.wslab()
            wv = wb.rearrange("p (k n) -> p k n", k=8)
            loadfn(i, wv, wk)
            return wv, wk
        nxt = issue(0)
        for i in range(n):
            cur = nxt
            if i + 1 < n:
                nxt = issue(i + 1)
            yield (i,) + cur

    def wslab(self):
        i = self.wb_ctr % 3
        self.wb_ctr += 1
        return self.WB[i], ("wb", i)

    def norm_T(self):
        S = self.S
        for t in range(4):
            sl = slice(512 * t, 512 * t + 512)
            b = self.bank()
            for k in range(8):
                sq = self.SQ[k % 2]
                self.act(sq, self.xT[:, k, sl], AF.Square, reads=[("xT", k, t)], writes=[("sq", k % 2)])
                self.mm(self.psf(b), self.ones_bf, sq, k == 0, k == 7, reads=[("sq", k % 2)], writes=[("ps", b)])
            self.act(self.SD[0], self.psf(b), AF.Sqrt, reads=[("ps", b)], writes=[("sd", 0)], bias=self.eps_t[:, 0:1], scale=1.0 / D)
            self.recip(self.SD[1], self.SD[0], reads=[("sd", 0)], writes=[("sd", 1)])
            for k in range(8):
                self.tt("dve", self.hT[:, k, sl], self.xT[:, k, sl], self.SD[1], ALU.mult,
                        reads=[("xT", k, t), ("sd", 1)], writes=[("hT", k, t)])

    def resid_add(self, b, k, t):
        sl = slice(512 * t, 512 * t + 512)
        self.tt("dve", self.xT[:, k, sl], self.psf(b), self.xT[:, k, sl], ALU.add,
                reads=[("ps", b), ("xT", k, t)], writes=[("xT", k, t)])

    def ffn(self, l):
        self.S.barrier()
        self.a_reset()
        self.norm_T()
        gain = self.G[:, l, 24:32]
        wgu = self.dram["ffn_w_gu"][l].rearrange("(k p) n -> p k n", p=128)
        wdn = self.dram["ffn_w_down"][l].rearrange("(c p) n -> p c n", p=128)
        parts = [(0, 6), (6, 12), (12, 17), (17, 22)]
        actT = self.a_bf(6 * 2048).rearrange("p (c n) -> p c n", c=6)
        wd = self.a_bf(6 * 1024).rearrange("p (c n) -> p c n", c=6)
        sg = [self.a_f32(512), self.a_f32(512)]
        def issue(c):
            wb, wk = self.wslab()
            wv = wb.rearrange("p (k n) -> p k n", k=8)
            self.load_cols(wgu, [(c * 128, 128), (DFF + c * 128, 128)], wv, gain, wk)
            return wv, wk

        nxt = issue(0)
        for (c0, c1) in parts:
            self.load_rows(wdn[:, c0:c1, :], wd, c1 - c0, None, "wd")
            for c in range(c0, c1):
                wv, wk = nxt
                if c + 1 < 22:
                    nxt = issue(c + 1)
                for t in range(4):
                    sl = slice(512 * t, 512 * t + 512)
                    bg, bu = self.bank(), self.bank()
                    for k in range(8):
                        self.mm(self.psf(bg), wv[:, k, 0:128], self.hT[:, k, sl], k == 0, k == 7, reads=[wk, ("hT", k, t)], writes=[("ps", bg)])
                    for k in range(8):
                        self.mm(self.psf(bu), wv[:, k, 128:256], self.hT[:, k, sl], k == 0, k == 7, reads=[wk, ("hT", k, t)], writes=[("ps", bu)])
                    si = (c * 4 + t) % 2
                    self.act(sg[si], self.psf(bg), AF.Silu, reads=[("ps", bg)], writes=[("sg", si)])
                    self.tt("dve", actT[:, c - c0, sl], sg[si], self.psf(bu), ALU.mult, reads=[("sg", si), ("ps", bu)], writes=[("actT", c - c0, t)])
            nchunk = c1 - c0
            for dk in range(8):
                for t in range(4):
                    sl = slice(512 * t, 512 * t + 512)
                    b = self.bank()
                    for j in range(nchunk):
                        self.mm(self.psf(b), wd[:, j, dk * 128:(dk + 1) * 128], actT[:, j, sl], j == 0, j == nchunk - 1,
                                reads=["wd", ("actT", j, t)], writes=[("ps", b)])
                    self.resid_add(b, dk, t)

    def xattn(self, l):
        S = self.S
        S.barrier()
        self.a_reset()
        self.norm_T()
        gq = self.G[:, l, 8:16]
        gkv = self.G[:, l, 16:24]
        wq = self.dram["xa_w_q"][l].rearrange("(k p) n -> p k n", p=128)
        wkv = self.dram["xa_w_kv"][l].rearrange("(k p) n -> p k n", p=128)
        wo = self.dram["xa_w_out"][l].rearrange("(k p) n -> p k n", p=128)
        qT = self.a_bf(8 * 2048).rearrange("p (c n) -> p c n", c=8)
        kT = self.a_bf(8 * 256).rearrange("p (c n) -> p c n", c=8)
        vt = self.a_bf(2 * 1024).rearrange("p (c n) -> p c n", c=2)
        sqb = [self.a_bf(512), self.a_bf(512)]
        sd = [self.a_f32(512), self.a_f32(512)]
        pT = [self.a_bf(512), self.a_bf(512)]
        rden = self.a_f32(512)
        qg = self.xqg[:, l, :]
        kg = self.xkg[:, l, :]
        for cg, wv, wk in self.slabs(8, lambda i, wv_, wk_: self.load_cols(wkv, [(i * 256, 256)], wv_, gkv, wk_)):
            if cg < 4:
                for j in range(2):
                    fc = cg * 2 + j
                    b = self.bank()
                    for k in range(8):
                        self.mm(self.psf(b, 0, 256), wv[:, k, j * 128:(j + 1) * 128], self.memT[:, k, :], k == 0, k == 7,
                                reads=[wk, "memT"], writes=[("ps", b)])
                    self.cp("act", kT[:, fc, :], self.psf(b, 0, 256), reads=[("ps", b)], writes=[("kT", fc)])
                    self.act(sqb[j][:, 0:256], self.psf(b, 0, 256), AF.Square, reads=[("ps", b)], writes=[("sqb", j)])
                h = cg
                b2 = self.bank()
                for j in range(2):
                    self.mm(self.psf(b2, 0, 256), self.ones_bf, sqb[j][:, 0:256], j == 0, j == 1, reads=[("sqb", j)], writes=[("ps", b2)])
                self.act(sd[0][:, 0:256], self.psf(b2, 0, 256), AF.Sqrt, reads=[("ps", b2)], writes=[("sd", 0)], bias=self.eps_t[:, 0:1], scale=1.0 / 256)
                self.recip(sd[1][:, 0:256], sd[0][:, 0:256], reads=[("sd", 0)], writes=[("sd", 1)])
                for j in range(2):
                    fc = 2 * h + j
                    self.stt("dve", kT[:, fc, :], kT[:, fc, :], kg[:, j:j + 1], sd[1][:, 0:256], ALU.mult, ALU.mult,
                             reads=[("kT", fc), ("sd", 1)], writes=[("kT", fc)])
            else:
                for mt in range(2):
                    b = self.bank()
                    for k in range(8):
                        self.mm(self.psf(b, 0, 256), self.memT[:, k, mt * 128:(mt + 1) * 128], wv[:, k, :], k == 0, k == 7,
                                reads=[wk, "memT"], writes=[("ps", b)])
                    c0 = (cg - 4) * 256
                    self.cp("act", vt[:, mt, c0:c0 + 256], self.psf(b, 0, 256), reads=[("ps", b)], writes=[("vt", mt, cg)])
        if self.stop_at == 1:
            return
        for cg, wv, wk in self.slabs(4, lambda i, wv_, wk_: self.load_cols(wq, [(i * 256, 256)], wv_, gq, wk_)):
            h = cg
            for t in range(4):
                sl = slice(512 * t, 512 * t + 512)
                for j in range(2):
                    fc = 2 * h + j
                    b = self.bank()
                    for k in range(8):
                        self.mm(self.psf(b), wv[:, k, j * 128:(j + 1) * 128], self.hT[:, k, sl], k == 0, k == 7,
                                reads=[wk, ("hT", k, t)], writes=[("ps", b)])
                    self.cp("act", qT[:, fc, sl], self.psf(b), reads=[("ps", b)], writes=[("qT", fc, t)])
                    self.act(sqb[j], self.psf(b), AF.Square, reads=[("ps", b)], writes=[("sqb", j)])
                b2 = self.bank()
                for j in range(2):
                    self.mm(self.psf(b2), self.ones_bf, sqb[j], j == 0, j == 1, reads=[("sqb", j)], writes=[("ps", b2)])
                self.act(sd[0], self.psf(b2), AF.Sqrt, reads=[("ps", b2)], writes=[("sd", 0)], bias=self.eps_t[:, 0:1], scale=1.0 / 256)
                self.recip(sd[1], sd[0], reads=[("sd", 0)], writes=[("sd", 1)])
                for j in range(2):
                    fc = 2 * h + j
                    self.stt("dve", qT[:, fc, sl], qT[:, fc, sl], qg[:, j:j + 1], sd[1], ALU.mult, ALU.mult,
                             reads=[("qT", fc, t), ("sd", 1)], writes=[("qT", fc, t)])
        if self.stop_at == 2:
            return
        S.barrier()
        oT = self.hT
        scale = float(256 ** -0.5)
        xitems = [(h, t) for h in range(4) for t in range(4)]
        pT4 = [pT[0], pT[1], self.SQ[0], self.SQ[1]]
        pkeys = [("pT", 0), ("pT", 1), ("sq", 0), ("sq", 1)]

        def xscore(n):
            h, t = xitems[n]
            sl = slice(512 * t, 512 * t + 512)
            for kt in range(2):
                b = self.bank()
                pi = 2 * (n % 2) + kt
                for j in range(2):
                    fc = 2 * h + j
                    self.mm(self.psf(b), kT[:, fc, kt * 128:(kt + 1) * 128], qT[:, fc, sl], j == 0, j == 1,
                            reads=[("kT", fc), ("qT", fc, t)], writes=[("ps", b)])
                self.act(pT4[pi], self.psf(b), AF.Exp, reads=[("ps", b)], writes=[pkeys[pi]], scale=scale)

        def xpv(n):
            h, t = xitems[n]
            sl = slice(512 * t, 512 * t + 512)
            bd = self.bank()
            for kt in range(2):
                pi = 2 * (n % 2) + kt
                self.mm(self.psf(bd), self.ones_bf, pT4[pi], kt == 0, kt == 1, reads=[pkeys[pi]], writes=[("ps", bd)])
            self.recip(rden, self.psf(bd), reads=[("ps", bd)], writes=["rden"])
            for j in range(2):
                fc = 2 * h + j
                b = self.bank()
                for kt in range(2):
                    pi = 2 * (n % 2) + kt
                    self.mm(self.psf(b), vt[:, kt, fc * 128:(fc + 1) * 128], pT4[pi], kt == 0, kt == 1,
                            reads=[("vt", kt, 4 + fc // 2), pkeys[pi]], writes=[("ps", b)])
                self.tt("dve", oT[:, fc, sl], self.psf(b), rden, ALU.mult, reads=[("ps", b), "rden"], writes=[("oT", fc, t)])

        xscore(0)
        for n in range(len(xitems)):
            if n + 1 < len(xitems):
                xscore(n + 1)
            xpv(n)
        if self.stop_at == 3:
            return
        for cg, wv, wk in self.slabs(4, lambda i, wv_, wk_: self.load_cols(wo, [(i * 256, 256)], wv_, None, wk_)):
            for j in range(2):
                dk = cg * 2 + j
                for t in range(4):
                    sl = slice(512 * t, 512 * t + 512)
                    b = self.bank()
                    for k in range(8):
                        self.mm(self.psf(b), wv[:, k, j * 128:(j + 1) * 128], oT[:, k, sl], k == 0, k == 7,
                                reads=[wk, ("oT", k, t)], writes=[("ps", b)])
                    self.resid_add(b, dk, t)

    def gla(self, l):
        S = self.S
        i = l // 2
        S.barrier()
        self.a_reset()
        self.norm_T()
        gain = self.G[:, l, 0:8]
        win = self.dram["od_w_in"][i].rearrange("(k p) n -> p k n", p=128)
        wout = self.dram["od_w_out"][i].rearrange("(c p) n -> p c n", p=128)
        wg1 = self.dram["od_w_g1"][i].rearrange("(k p) n -> p k n", p=128)
        og = self.ogain[:, i, :]
        g1aug = self.a_bf(2048)
        wg2 = self.a_bf(512)
        wg1b = self.a_bf(128).rearrange("p (k n) -> p k n", k=8)
        qin = self.a_bf(2048)
        kin = self.a_bf(2048)
        q2 = self.a_bf(2048)
        kin_tok = self.a_bf(2048).rearrange("p (t n) -> p t n", t=16)
        l_tok = self.a_bf(2048).rearrange("p (t n) -> p t n", t=16)
        v_tok = self.a_bf(4096).rearrange("p (t n) -> p t n", t=16)
        rs = self.a_bf(4096).rearrange("p (j n) -> p j n", j=2)
        attT = l_tok
        ogt = self.a_bf(4096).rearrange("p (j n) -> p j n", j=2)
        sqb = self.SQ
        dec = self.a_f32(32)
        R = self.a_f32(256)
        Rbf = [self.a_bf(256), self.a_bf(256)]
        e1 = [self.a_f32(512)]
        eq = self.a_f32(512)
        ek = self.a_f32(512)
        sd = self.SD
        tmp = self.a_f32(512)
        wo_b = self.a_bf(2 * 1024).rearrange("p (j n) -> p j n", j=2)
        self.memset("pool", g1aug[0:32, :], 1.0, writes=["g1aug"])
        st = self.STAGE[self.stage_ctr % 2]
        skey = ("stage", self.stage_ctr % 2)
        sslot = ("stage", self.stage_ctr % 2)
        self.stage_ctr += 1
        self.dma(st[:, 0:128].rearrange("p (k n) -> p k n", k=8), wg1, slot=sslot, writes=[skey])
        self.dma(st[0:16, 128:640], self.dram["od_w_g2"][i], slot=sslot, writes=[skey])
        self.dma(st[16:17, 128:640], self.dram["od_b_g"][i:i + 1, :], slot=sslot, writes=[skey])
        for k in range(8):
            self.ts("dve", wg1b[:, k, :], st[:, 16 * k:16 * k + 16], gain[:, k:k + 1], None, ALU.mult, None, reads=[skey], writes=["wg1b"])
        self.cp("dve", wg2[0:17, :], st[0:17, 128:640], reads=[skey], writes=["wg2"])
        for t in range(4):
            sl = slice(512 * t, 512 * t + 512)
            b = self.bank()
            for k in range(8):
                self.mm(self.PS[0:16, 512 * b:512 * b + 512], wg1b[:, k, :], self.hT[:, k, sl], k == 0, k == 7,
                        reads=["wg1b", ("hT", k, t)], writes=[("ps", b)])
            self.cp("act", g1aug[0:16, sl], self.PS[0:16, 512 * b:512 * b + 512], reads=[("ps", b)], writes=["g1aug"])
        for h in range(4):
            wbqk, wkqk = self.wslab()
            wqk = wbqk.rearrange("p (k n) -> p k n", k=8)
            self.load_cols(win, [(128 * h, 128), (512 + 128 * h, 128)], wqk, gain, wkqk)
            wbv, wkv_ = self.wslab()
            wvv = wbv.rearrange("p (k n) -> p k n", k=8)
            self.load_cols(win, [(1024 + 256 * h, 256)], wvv, gain, wkv_)
            wbr, wkr = self.wslab()
            wrv = wbr.rearrange("p (k n) -> p k n", k=8)
            self.load_cols(win, [(2048 + 256 * h, 256)], wrv, gain, wkr)
            for t in range(4):
                b = self.bank()
                for tt in range(4):
                    tok = 4 * t + tt
                    self.mm(self.psf(b, 128 * tt, 128 * tt + 128), g1aug[0:17, tok * 128:(tok + 1) * 128], wg2[0:17, 128 * h:128 * h + 128],
                            True, True, reads=["g1aug", "wg2"], writes=[("ps", b)])
                self.act(e1[0], self.psf(b), AF.Exp, reads=[("ps", b)], writes=["e1"], scale=-1.0)
                self.act(l_tok[:, 4 * t:4 * t + 4, :], e1[0].rearrange("p (t n) -> p t n", t=4), AF.Ln,
                         reads=["e1"], writes=[("LA", t)], bias=self.one_t[:, 0:1], scale=1.0)
            for tk in range(0, 16, 2):
                b = self.bank()
                for u in range(2):
                    tok = tk + u
                    for k in range(8):
                        self.mm(self.psf(b, 256 * u, 256 * u + 256), self.hT[:, k, tok * 128:(tok + 1) * 128], wvv[:, k, :], k == 0, k == 7,
                                reads=[wkv_, ("hT", k, tok // 4)], writes=[("ps", b)])
                self.cp("act", v_tok[:, tk:tk + 2, :], self.psf(b).rearrange("p (u n) -> p u n", u=2), reads=[("ps", b)], writes=[("v_tok", tk // 2)])
            for j in range(2):
                for t in range(4):
                    sl = slice(512 * t, 512 * t + 512)
                    b = self.bank()
                    for k in range(8):
                        self.mm(self.psf(b), wrv[:, k, 128 * j:128 * j + 128], self.hT[:, k, sl], k == 0, k == 7,
                                reads=[wkr, ("hT", k, t)], writes=[("ps", b)])
                    self.act(rs[:, j, sl], self.psf(b), AF.Silu, reads=[("ps", b)], writes=[("rs", j, t)])
            for t in range(4):
                sl = slice(512 * t, 512 * t + 512)
                bb = self.bank()
                for tt in range(4):
                    tok = 4 * t + tt
                    self.mm(self.psf(bb, 128 * tt, 128 * tt + 128), l_tok[:, tok, :], self.U_bf, True, True,
                            reads=[("LA", t)], writes=[("ps", bb)])
                self.act(eq, self.psf(bb), AF.Exp, reads=[("ps", bb)], writes=["eq"], scale=-1.0 / 16)
                self.act(ek, self.psf(bb), AF.Exp, reads=[("ps", bb)], writes=["ek"], scale=1.0 / 16)
                self.act(dec[:, 8 * t:8 * t + 8], self.psf(bb).rearrange("p (n c) -> p n c", c=64)[:, :, 63], AF.Exp,
                         reads=[("ps", bb)], writes=["dec"], scale=-1.0 / 16)
                bq = self.bank()
                for k in range(8):
                    self.mm(self.psf(bq), wqk[:, k, 0:128], self.hT[:, k, sl], k == 0, k == 7, reads=[wkqk, ("hT", k, t)], writes=[("ps", bq)])
                self.stt("dve", qin[:, sl], self.psf(bq), float(128 ** -0.5), eq, ALU.mult, ALU.mult, reads=[("ps", bq), "eq"], writes=[("qin", t)])
                bk = self.bank()
                for k in range(8):
                    self.mm(self.psf(bk), wqk[:, k, 128:256], self.hT[:, k, sl], k == 0, k == 7, reads=[wkqk, ("hT", k, t)], writes=[("ps", bk)])
                self.tt("dve", kin[:, sl], self.psf(bk), ek, ALU.mult, reads=[("ps", bk), "ek"], writes=[("kin", t)])
            self.tt("dve", q2[:, 64:2048].rearrange("p (n c) -> p n c", c=64), qin[:, 64:2048].rearrange("p (n c) -> p n c", c=64),
                    dec[:, 0:31].unsqueeze(2).broadcast_to([128, 31, 64]), ALU.mult,
                    reads=[("qin", t) for t in range(4)] + ["dec"], writes=["q2"])
            for t in range(4):
                b = self.bank()
                pb = self.psb(b)
                for tt in range(4):
                    tok = 4 * t + tt
                    self.tr(pb[:, 128 * tt:128 * tt + 128], kin[:, tok * 128:(tok + 1) * 128], self.ident_bf, reads=[("kin", t)], writes=[("ps", b)])
                self.cp("act", kin_tok[:, 4 * t:4 * t + 4, :], pb[:, 0:512].rearrange("p (t n) -> p t n", t=4), reads=[("ps", b)], writes=[("kin_tok", t)])
            for t in range(4):
                b = self.bank()
                for tt in range(4):
                    tok = 4 * t + tt
                    tsl = slice(tok * 128, tok * 128 + 128)
                    self.mm(self.psf(b, 128 * tt, 128 * tt + 128), kin[:, tsl], qin[:, tsl], True, True, reads=[("kin", t), ("qin", t)], writes=[("ps", b)])
                self.tt("dve", attT[:, 4 * t:4 * t + 4, :], self.psf(b).rearrange("p (t n) -> p t n", t=4),
                        self.U01.unsqueeze(1).broadcast_to([128, 4, 128]), ALU.mult, reads=[("ps", b)], writes=[("LA", t)])
            for t in range(4):
                sl = slice(512 * t, 512 * t + 512)
                bo = self.reserve(2)
                for tt in range(4):
                    p = 4 * t + tt
                    for s2 in range(2):
                        n = 2 * p + s2
                        bs = self.bank()
                        self.mm(self.psf(bs, 0, 256), kin_tok[64 * s2:64 * s2 + 64, p, :], v_tok[64 * s2:64 * s2 + 64, p, :], True, True,
                                reads=[("kin_tok", t), ("v_tok", p // 2)], writes=[("ps", bs)])
                        if s2 == 0:
                            for j in range(2):
                                self.mm(self.psf(bo[j], 128 * tt, 128 * tt + 128), v_tok[:, p, 128 * j:128 * j + 128], attT[:, p, :], True, False,
                                        reads=[("v_tok", p // 2), ("LA", t)], writes=[("ps", bo[j])], skip_group_check=True)
                        if n >= 1:
                            for j in range(2):
                                c0 = 128 * tt + 64 * s2
                                self.mm(self.psf(bo[j], c0, c0 + 64), Rbf[(n - 1) % 2][:, 128 * j:128 * j + 128], q2[:, 64 * n:64 * n + 64], False, True,
                                        reads=[("Rbf", (n - 1) % 2), "q2"], writes=[("ps", bo[j])], skip_group_check=True)
                        if n == 0:
                            self.cp("dve", Rbf[0], self.psf(bs, 0, 256), reads=[("ps", bs)], writes=[("Rbf", 0)])
                            self.cp("dve", R, self.psf(bs, 0, 256), reads=[("ps", bs)], writes=["R"])
                        else:
                            if n < 31:
                                self.stt("dve", Rbf[n % 2], R, dec[:, n - 1:n], self.psf(bs, 0, 256), ALU.mult, ALU.add,
                                         reads=["R", "dec", ("ps", bs)], writes=[("Rbf", n % 2)])
                                self.stt("dve", R, R, dec[:, n - 1:n], self.psf(bs, 0, 256), ALU.mult, ALU.add, reads=["R", "dec", ("ps", bs)], writes=["R"])
                for j in range(2):
                    self.act(sqb[j], self.psf(bo[j]), AF.Square, reads=[("ps", bo[j])], writes=[("sq", j)])
                b2 = self.bank()
                for j in range(2):
                    self.mm(self.psf(b2), self.ones_bf, sqb[j], j == 0, j == 1, reads=[("sq", j)], writes=[("ps", b2)])
                self.act(sd[0], self.psf(b2), AF.Sqrt, reads=[("ps", b2)], writes=[("sd", 0)], bias=self.eps_t[:, 0:1], scale=1.0 / 256)
                self.recip(sd[1], sd[0], reads=[("sd", 0)], writes=[("sd", 1)])
                for j in range(2):
                    self.tt("dve", tmp, self.psf(bo[j]), sd[1], ALU.mult, reads=[("ps", bo[j]), ("sd", 1)], writes=["tmp"])
                    self.tt("dve", ogt[:, j, sl], tmp, rs[:, j, sl], ALU.mult, reads=["tmp", ("rs", j, t)], writes=[("ogt", j, t)])
                self.release(bo)
            self.load_rows(wout[:, 2 * h:2 * h + 2, :], wo_b, 2, og[:, 2 * h:2 * h + 2], "wo_b")
            for dk in range(8):
                for t in range(4):
                    sl = slice(512 * t, 512 * t + 512)
                    b = self.bank()
                    for j in range(2):
                        self.mm(self.psf(b), wo_b[:, j, dk * 128:(dk + 1) * 128], ogt[:, j, sl], j == 0, j == 1,
                                reads=["wo_b", ("ogt", j, t)], writes=[("ps", b)])
                    self.resid_add(b, dk, t)

    def even(self, l):
        S = self.S
        i = l // 2
        S.barrier()
        self.a_reset()
        self.norm_T()
        gain = self.G[:, l, 0:8]
        win = self.dram["ev_w_in"][i].rearrange("(k p) n -> p k n", p=128)
        wout = self.dram["ev_w_out"][i].rearrange("(k p) n -> p k n", p=128)
        qg = self.evq[:, i, 0:1]
        kg = self.evk[:, i, 0:1]
        lng = self.evlng[:, i, :]
        lnb = self.evlnb[:, i, :]
        gT = self.a_bf(4 * 2048).rearrange("p (c n) -> p c n", c=4)
        xhat = self.a_bf(16 * 512).rearrange("p (t n) -> p t n", t=16)
        qf = xhat.rearrange("p t n -> p (t n)").rearrange("p (c n) -> p c n", c=4)
        kf = self.a_bf(4 * 2048).rearrange("p (c n) -> p c n", c=4)
        wsT = self.a_bf(8 * 128).rearrange("p (g n) -> p g n", g=8)
        btab = self.a_f32(4 * 128).rearrange("p (c n) -> p c n", c=4)
        bsT = self.a_f32(4 * 128).rearrange("p (c n) -> p c n", c=4)
        kmbd = self.a_bf(4 * 16).rearrange("p (c n) -> p c n", c=4)
        kmean = self.a_f32(4 * 8).rearrange("p (c n) -> p c n", c=4)
        st1 = self.a_f32(8)
        st2 = self.a_f32(8)
        mean = self.a_f32(8)
        var = self.a_f32(8)
        rstd8 = self.a_f32(8)
        F = [self.a_f32(512), self.a_f32(512)]
        Bt = [self.a_bf(512), self.a_bf(512), self.a_bf(512)]
        gel = self.SD
        sqf, xc = F[0], F[1]
        sqb = self.SQ
        qgb = [Bt[0], Bt[1]]
        qn = Bt[2]
        sd = self.SD
        t1, t2 = F[0], F[1]
        tmpg = F[0]
        cmp_ = F[0]
        pT = Bt
        gsb = self.a_f32(64)
        cnt = self.a_f32(64)
        nm_tok = [self.a_bf(384), self.a_bf(384)]
        rec = self.a_f32(256)
        s = self.stage_ctr % 2
        self.stage_ctr += 1
        st = self.STAGE[s]
        skey = ("stage", s)
        sslot = ("stage", s)
        ws_d = self.dram["ev_w_s"][i]
        self.dma(st[:, 0:1024].rearrange("p (g n) -> p g n", g=8), ws_d.rearrange("g i j -> i g j"), slot=sslot, writes=[skey])
        bs_d = self.dram["ev_b_s"][i]
        for g in range(8):
            c, hf = g // 2, g % 2
            self.dma(bsT[64 * hf:64 * hf + 64, c, :], bs_d[g:g + 1, :].partition_broadcast(64), slot=("misc", 0), writes=["bsT"])
        for g in range(8):
            c, hf = g // 2, g % 2
            b = self.bank()
            self.tr(self.psf(b, 0, 128), st[:, 128 * g:128 * g + 128], self.ident_f, reads=[skey], writes=[("ps", b)])
            self.tt("dve", wsT[:, g, :], self.psf(b, 0, 128), self.C01, ALU.mult, reads=[("ps", b)], writes=[("wsT", g)])
            b2 = self.bank()
            self.mm(self.PS[64 * hf:64 * hf + 64, 512 * b2:512 * b2 + 128], self.ones_bf[:, 0:64], wsT[:, g, :], True, True,
                    reads=[("wsT", g)], writes=[("ps", b2)])
            self.stt("dve", btab[64 * hf:64 * hf + 64, c, :], self.PS[64 * hf:64 * hf + 64, 512 * b2:512 * b2 + 128],
                     lnb[64 * hf:64 * hf + 64, c:c + 1], bsT[64 * hf:64 * hf + 64, c, :], ALU.mult, ALU.add,
                     reads=[("ps", b2), "bsT"], writes=[("btab", g)])

        def slab(c0):
            wb, wk = self.wslab()
            wv = wb.rearrange("p (k n) -> p k n", k=8)
            self.load_cols(win, [(c0, 256)], wv, gain, wk)
            return wv, wk

        wvA, wkA = slab(2048)
        wvB, wkB = slab(2304)
        red = lambda o_, i_: (lambda e: e.tensor_reduce(o_, i_, AX.X, ALU.add))
        for tok in range(16):
            b = self.bank()
            for sl2, (wv, wk) in enumerate(((wvA, wkA), (wvB, wkB))):
                for k in range(8):
                    self.mm(self.psf(b, 256 * sl2, 256 * sl2 + 256), self.hT[:, k, tok * 128:(tok + 1) * 128], wv[:, k, :], k == 0, k == 7,
                            reads=[wk, ("hT", k, tok // 4)], writes=[("ps", b)], skip_group_check=True)
            gi = tok % 2
            ge = gel[gi]
            self.act(ge, self.psf(b), AF.Gelu, reads=[("ps", b)], writes=[("sd", gi)])
            ge3 = ge.rearrange("p (g d) -> p g d", g=8)
            self.S.op("dve", red(st1, ge3), reads=[("sd", gi)], writes=["st1"])
            self.act(sqf, ge, AF.Square, reads=[("sd", gi)], writes=[("F", 0)])
            self.S.op("dve", red(st2, sqf.rearrange("p (g d) -> p g d", g=8)), reads=[("F", 0)], writes=["st2"])
            self.ts("dve", mean, st1, 1.0 / 64, None, ALU.mult, None, reads=["st1"], writes=["mean"])
            self.tt("dve", var, mean, mean, ALU.mult, reads=["mean"], writes=["var"])
            self.stt("dve", var, st2, 1.0 / 64, var, ALU.mult, ALU.subtract, reads=["st2", "var"], writes=["var"])
            self.act(rstd8, var, AF.Sqrt, reads=["var"], writes=["rstd8"], bias=self.eps_t[:, 0:1], scale=1.0)
            self.recip(rstd8, rstd8, reads=["rstd8"], writes=["rstd8"])
            xc3 = xc.rearrange("p (g d) -> p g d", g=8)
            self.tt("dve", xc3, ge3, mean.unsqueeze(2).broadcast_to([128, 8, 64]), ALU.subtract, reads=[("sd", gi), "mean"], writes=[("F", 1)])
            self.tt("dve", xhat[:, tok, :].rearrange("p (g d) -> p g d", g=8), xc3,
                    rstd8.unsqueeze(2).broadcast_to([128, 8, 64]), ALU.mult, reads=[("F", 1), "rstd8"], writes=[("xhat", tok)])
        for sl2 in range(2):
            wv, wk = slab(1536 + 256 * sl2)
            for j in range(2):
                c = 2 * sl2 + j
                for t in range(4):
                    sl = slice(512 * t, 512 * t + 512)
                    b = self.bank()
                    for k in range(8):
                        self.mm(self.psf(b), wv[:, k, 128 * j:128 * j + 128], self.hT[:, k, sl], k == 0, k == 7,
                                reads=[wk, ("hT", k, t)], writes=[("ps", b)])
                    self.act(gT[:, c, sl], self.psf(b), AF.Gelu, reads=[("ps", b)], writes=[("gT", c, t)])
        for g in range(8):
            c, hf = g // 2, g % 2
            pr = slice(64 * hf, 64 * hf + 64)
            for t in range(4):
                sl = slice(512 * t, 512 * t + 512)
                b = self.bank()
                for tt in range(4):
                    tok = 4 * t + tt
                    self.mm(self.PS[pr, 512 * b + 128 * tt:512 * b + 128 * tt + 128], xhat[:, tok, 64 * g:64 * g + 64], wsT[:, g, :], True, True,
                            reads=[("xhat", tok), ("wsT", g)], writes=[("ps", b)])
                self.stt("dve", tmpg[pr, :].rearrange("p (t n) -> p t n", t=4), self.PS[pr, 512 * b:512 * b + 512].rearrange("p (t n) -> p t n", t=4),
                         lng[pr, c:c + 1], btab[pr, c, :].unsqueeze(1).broadcast_to([64, 4, 128]), ALU.mult, ALU.add,
                         reads=[("ps", b), ("btab", g)], writes=[("F", 0)])
                self.tt("dve", gT[pr, c, sl], tmpg[pr, :], gT[pr, c, sl], ALU.mult, reads=[("F", 0), ("gT", c, t)], writes=[("gT", c, t)])
        S.barrier()
        for which in range(2):
            dst = qf if which == 0 else kf
            gcol = qg if which == 0 else kg
            name = "qf" if which == 0 else "kf"
            for sl2 in range(2):
                wv, wk = slab(512 * which + 256 * sl2)
                for j in range(2):
                    c = 2 * sl2 + j
                    for t in range(4):
                        sl = slice(512 * t, 512 * t + 512)
                        b = self.bank()
                        for k in range(8):
                            self.mm(self.psf(b), wv[:, k, 128 * j:128 * j + 128], self.hT[:, k, sl], k == 0, k == 7,
                                    reads=[wk, ("hT", k, t)], writes=[("ps", b)])
                        si = t % 2
                        self.act(sqb[si], self.psf(b), AF.Square, reads=[("ps", b)], writes=[("sq", si)])
                        self.act(qgb[si], self.psf(b), AF.Copy, reads=[("ps", b)], writes=[("B", si)])
                        b2 = self.bank()
                        self.mm(self.psf(b2), self.bo_bf, sqb[si], True, True, reads=[("sq", si)], writes=[("ps", b2)])
                        self.act(sd[0], self.psf(b2), AF.Sqrt, reads=[("ps", b2)], writes=[("sd", 0)], bias=self.eps_t[:, 0:1], scale=1.0 / 64)
                        self.recip(sd[1], sd[0], reads=[("sd", 0)], writes=[("sd", 1)])
                        self.stt("dve", qn, qgb[si], gcol, sd[1], ALU.mult, ALU.mult, reads=[("B", si), ("sd", 1)], writes=[("B", 2)])
                        b3 = self.bank()
                        self.mm(self.psf(b3), self.pm_bf, qn, True, True, reads=[("B", 2)], writes=[("ps", b3)])
                        self.tt("dve", t1, qn, self.ropeC[:, sl], ALU.mult, reads=[("B", 2)], writes=[("F", 0)])
                        self.tt("dve", t2, self.psf(b3), self.ropeS[:, sl], ALU.mult, reads=[("ps", b3)], writes=[("F", 1)])
                        self.tt("dve", dst[:, c, sl], t1, t2, ALU.add, reads=[("F", 0), ("F", 1)],
                                writes=[(name, c, hf, 2 * t + u) for hf in range(2) for u in range(2)])
        wv0, wk0 = slab(1024)
        wv1, wk1 = slab(1280)
        for tok in range(16):
            b = self.bank()
            for sl2, (wv, wk) in enumerate(((wv0, wk0), (wv1, wk1))):
                for k in range(8):
                    self.mm(self.psf(b, 256 * sl2, 256 * sl2 + 256), self.hT[:, k, tok * 128:(tok + 1) * 128], wv[:, k, :], k == 0, k == 7,
                            reads=[wk, ("hT", k, tok // 4)], writes=[("ps", b)], skip_group_check=True)
            self.cp("act", self.hT[:, 4:8, tok * 128:(tok + 1) * 128], self.psf(b).rearrange("p (a n) -> p a n", a=4),
                    reads=[("ps", b)], writes=[("hT", 4 + a_, tok // 4) for a_ in range(4)])
        S.barrier()

        def vsl(kc, h):
            c0 = kc * 128 + 64 * (h % 2)
            return self.hT[:, 4 + h // 2, c0:c0 + 64]
        negT = [self.hT.rearrange("p k n -> p (k n)")[:, 2048 * s_:2048 * s_ + 2048] for s_ in range(3)]
        allk = lambda nm, c: [(nm, c, hf, ib) for hf in range(2) for ib in range(8)]
        for c in range(4):
            self.S.op("dve", (lambda o_, i_: lambda e: e.tensor_reduce(o_, i_, AX.X, ALU.add))(kmean[:, c, :], kf[:, c, :].rearrange("p (n k) -> p n k", n=8)),
                      reads=allk("kf", c), writes=[("kmean", c)])
            self.memset("pool", kmbd[:, c, :], 0.0, writes=[("kmbd", c)])
            for hf in range(2):
                pr = slice(64 * hf, 64 * hf + 64)
                self.ts("dve", kmbd[pr, c, 8 * hf:8 * hf + 8], kmean[pr, c, :], 1.0 / 256, None, ALU.mult, None, reads=[("kmean", c)], writes=[("kmbd", c)])
        for tok in range(8, 16):
            o = tok // 2
            bg = self.bank()
            for c in range(4):
                self.mm(self.psf(bg, 16 * c, 16 * c + 16), qf[:, c, tok * 128:(tok + 1) * 128], kmbd[:, c, :], True, True,
                        reads=allk("qf", c) + [("kmbd", c)], writes=[("ps", bg)], skip_group_check=True)
            self.cp("dve", gsb, self.psf(bg, 0, 64), reads=[("ps", bg)], writes=["gsb"])
            g3 = gsb.rearrange("p (h n) -> p h n", h=8)[:, :, 0:o]
            cm4 = cmp_[:, 0:8 * o * o].rearrange("p (h n m) -> p h n m", h=8, n=o)
            self.tt("dve", cm4, g3.unsqueeze(2).broadcast_to([128, 8, o, o]), g3.unsqueeze(3).broadcast_to([128, 8, o, o]), ALU.is_gt,
                    reads=["gsb"], writes=[("F", 0)])
            cn3 = cnt[:, 0:8 * o].rearrange("p (h n) -> p h n", h=8)
            self.S.op("dve", (lambda o_, i_: lambda e: e.tensor_reduce(o_, i_, AX.X, ALU.add))(cn3, cm4), reads=[("F", 0)], writes=["cnt"])
            nm = nm_tok[tok % 2]
            self.memset("pool", nm, 0.0, writes=[("nm", tok % 2)])
            for s_ in range(3):
                ng = 3 if s_ < 2 else 2
                self.ts("dve", nm[:, 128 * s_:128 * s_ + 96].rearrange("p (g n) -> p g n", n=32)[:, 0:ng, 0:o], cn3[:, 3 * s_:3 * s_ + ng, :],
                        3.0, NEG, ALU.is_ge, ALU.mult, reads=["cnt"], writes=[("nm", tok % 2)])
            bt = self.bank()
            pb = self.psb(bt)
            for s_ in range(3):
                self.tr(pb[:, 128 * s_:128 * s_ + 128], nm[:, 128 * s_:128 * s_ + 128], self.ident_bf, reads=[("nm", tok % 2)], writes=[("ps", bt)])
            for s_ in range(3):
                self.cp("act", negT[s_][:, tok * 128:(tok + 1) * 128], pb[:, 128 * s_:128 * s_ + 128], reads=[("ps", bt)], writes=[("negT", s_, tok)])
        scale = 0.125
        items = [(h, ib, jb) for h in range(8) for ib in range(8) for jb in range(ib + 1)]
        st_ = {}

        def score(n):
            h, ib, jb = items[n]
            c, hf = h // 2, h % 2
            pr = slice(64 * hf, 64 * hf + 64)
            s_, g_ = h // 3, h % 3
            gr = slice(32 * g_, 32 * g_ + 32)
            qsl = slice(256 * ib, 256 * ib + 256)
            b = self.bank()
            pi = n % 3
            kc0, kc1 = 2 * jb, 2 * jb + 1
            rd = [("kf", c, hf, jb), ("qf", c, hf, ib)]
            self.mm(self.psf(b, 0, 256), kf[pr, c, kc0 * 128:kc0 * 128 + 128], qf[pr, c, qsl], True, False,
                    reads=rd, writes=[("ps", b)], skip_group_check=True)
            if jb < ib:
                self.mm(self.psf(b, 256, 512), kf[pr, c, kc1 * 128:kc1 * 128 + 128], qf[pr, c, qsl], False, ib <= 3,
                        reads=rd, writes=[("ps", b)], skip_group_check=True)
                if ib > 3:
                    self.mm(self.psf(b), self.e8_bf[gr, jb, :],
                            negT[s_][gr, qsl].unsqueeze(1).broadcast_to([32, 2, 256]), False, True,
                            reads=[("negT", s_, 2 * ib), ("negT", s_, 2 * ib + 1)], writes=[("ps", b)], skip_group_check=True)
                w = 512
            else:
                self.mm(self.psf(b, 256, 384), kf[pr, c, kc1 * 128:kc1 * 128 + 128], qf[pr, c, 256 * ib + 128:256 * ib + 256], False, False,
                        reads=rd, writes=[("ps", b)], skip_group_check=True)
                self.mm(self.psf(b, 0, 128), self.ident_bf, self.cn_bf, False, False,
                        reads=[], writes=[("ps", b)], skip_group_check=True)
                self.mm(self.psf(b, 256, 384), self.ident_bf, self.cn_bf, False, True,
                        reads=[], writes=[("ps", b)], skip_group_check=True)
                w = 384
            self.act(pT[pi][:, 0:w], self.psf(b, 0, w), AF.Exp, reads=[("ps", b)], writes=[("B", pi)], scale=scale)

        def pv(n):
            h, ib, jb = items[n]
            c, hf = h // 2, h % 2
            pr = slice(64 * hf, 64 * hf + 64)
            qsl = slice(256 * ib, 256 * ib + 256)
            pi = n % 3
            kc0, kc1 = 2 * jb, 2 * jb + 1
            if jb == 0:
                st_["banks"] = self.reserve(2)
            ba, bd = st_["banks"]
            acc = self.PS[pr, 512 * ba:512 * ba + 256]
            den = self.PS[pr, 512 * bd:512 * bd + 256]
            last = (jb == ib)
            self.mm(acc, vsl(kc0, h), pT[pi][:, 0:256], jb == 0, False, reads=[("B", pi)], writes=[("ps", ba)], skip_group_check=True)
            self.mm(den, self.ones_bf[:, 0:64], pT[pi][:, 0:256], jb == 0, False, reads=[("B", pi)], writes=[("ps", bd)], skip_group_check=True)
            if not last:
                self.mm(acc, vsl(kc1, h), pT[pi][:, 256:512], False, False, reads=[("B", pi)], writes=[("ps", ba)], skip_group_check=True)
                self.mm(den, self.ones_bf[:, 0:64], pT[pi][:, 256:512], False, False, reads=[("B", pi)], writes=[("ps", bd)], skip_group_check=True)
            else:
                self.mm(self.PS[pr, 512 * ba + 128:512 * ba + 256], vsl(kc1, h), pT[pi][:, 256:384], False, True,
                        reads=[("B", pi)], writes=[("ps", ba)], skip_group_check=True)
                self.mm(self.PS[pr, 512 * bd + 128:512 * bd + 256], self.ones_bf[:, 0:64], pT[pi][:, 256:384], False, True,
                        reads=[("B", pi)], writes=[("ps", bd)], skip_group_check=True)
                self.recip(rec[pr, :], den, reads=[("ps", bd)], writes=[("rec", hf)])
                self.tt("dve", qf[pr, c, qsl], acc, rec[pr, :], ALU.mult, reads=[("ps", ba), ("rec", hf)], writes=[("qf", c, hf, ib)])
                self.release([ba, bd])

        score(0)
        for n in range(len(items)):
            if n + 1 < len(items):
                score(n + 1)
            pv(n)
        for cg, wv, wk in self.slabs(4, lambda i, wv_, wk_: self.load_cols(wout, [(i * 256, 256)], wv_, None, wk_)):
            for j in range(2):
                dk = cg * 2 + j
                for t in range(4):
                    sl = slice(512 * t, 512 * t + 512)
                    b = self.bank()
                    for k in range(8):
                        if k < 4:
                            rhs = qf[:, k, sl]
                            rk = [("qf", k, hf, ib) for hf in range(2) for ib in (2 * t, 2 * t + 1)]
                        else:
                            rhs = gT[:, k - 4, sl]
                            rk = [("gT", k - 4, t)]
                        self.mm(self.psf(b), wv[:, k, j * 128:(j + 1) * 128], rhs, k == 0, k == 7, reads=[wk] + rk, writes=[("ps", b)])
                    self.resid_add(b, dk, t)

    def build(self):
        nc = self.nc
        x_d = self.din("x", [S_LEN, D])
        mem_d = self.din("mem", [NMEM, D])
        pos_d = self.din("positions", [1, S_LEN], I32)
        cst_d = self.din("consts", [128, C_END])
        G_d = self.din("gains", [128, DEPTH * 32])
        xqg_d = self.din("xa_qg", [128, DEPTH * 2])
        xkg_d = self.din("xa_kg", [128, DEPTH * 2])
        evq_d = self.din("ev_qg", [128, 2])
        evk_d = self.din("ev_kg", [128, 2])
        evlng_d = self.din("ev_lng", [128, 8])
        evlnb_d = self.din("ev_lnb", [128, 8])
        og_d = self.din("od_og", [128, 16])
        self.din("ev_w_in", [2, D, 2560]); self.din("ev_w_s", [2, 8, 128, 128]); self.din("ev_b_s", [2, 8, 128])
        self.din("ev_w_out", [2, D, D]); self.din("od_w_in", [2, D, 3072]); self.din("od_w_g1", [2, D, 16])
        self.din("od_w_g2", [2, 16, 512]); self.din("od_b_g", [2, 512]); self.din("od_w_out", [2, D, D])
        self.din("xa_w_q", [DEPTH, D, D]); self.din("xa_w_kv", [DEPTH, D, 2 * D]); self.din("xa_w_out", [DEPTH, D, D])
        self.din("ffn_w_gu", [DEPTH, D, 2 * DFF]); self.din("ffn_w_down", [DEPTH, DFF, D])
        out_d = nc.dram_tensor("out", [S_LEN, D], F32, kind="ExternalOutput").ap()

        with ExitStack() as es:
            def sb(name, shape, dt=F32):
                return es.enter_context(nc.sbuf_tensor(name, shape, dt))[:]
            self.S = S = Sched(nc)
            self.PS = es.enter_context(nc.psum_tensor("ps", [128, 4096], F32))[:]
            self.xT = sb("xT", [128, 8 * 2048]).rearrange("p (k n) -> p k n", k=8)
            self.hT = sb("hT", [128, 8 * 2048], BF16).rearrange("p (k n) -> p k n", k=8)
            self.memT = sb("memT", [128, 8 * 256], BF16).rearrange("p (k n) -> p k n", k=8)
            cf = sb("cf", [128, 386])
            self.G = sb("G", [128, DEPTH * 32]).rearrange("p (l n) -> p l n", l=DEPTH)
            self.xqg = sb("xqg", [128, DEPTH * 2]).rearrange("p (l n) -> p l n", l=DEPTH)
            self.xkg = sb("xkg", [128, DEPTH * 2]).rearrange("p (l n) -> p l n", l=DEPTH)
            self.evq = sb("evq", [128, 2]).rearrange("p (l n) -> p l n", l=2)
            self.evk = sb("evk", [128, 2]).rearrange("p (l n) -> p l n", l=2)
            self.evlng = sb("evlng", [128, 8]).rearrange("p (l n) -> p l n", l=2)
            self.evlnb = sb("evlnb", [128, 8]).rearrange("p (l n) -> p l n", l=2)
            self.ogain = sb("ogain", [128, 16]).rearrange("p (l n) -> p l n", l=2)
            self.ident_f = cf[:, 0:128]
            self.C01 = cf[:, 128:256]
            self.U01 = cf[:, 256:384]
            invf_t = cf[:, 384:385]
            sgn_t = cf[:, 385:386]
            cbf = sb("cbf", [128, 7 * 128 + 1024], BF16)
            self.ident_bf = cbf[:, 0:128]
            self.bo_bf = cbf[:, 128:256]
            self.pm_bf = cbf[:, 256:384]
            self.cn_bf = cbf[:, 384:512]
            self.U_bf = cbf[:, 640:768]
            self.ones_bf = cbf[:, 768:896]
            self.e8_bf = cbf[:, 896:1920].rearrange("p (n m) -> p n m", n=8)
            self.eps_t = sb("eps_t", [128, 1])
            self.one_t = sb("one_t", [128, 1])
            self.ropeC = sb("ropeC", [128, 2048], BF16)
            self.ropeS = sb("ropeS", [128, 2048], BF16)
            self.STAGE = [sb("stage0", [128, 1024]), sb("stage1", [128, 1024])]
            self.WB = [sb("wb%d" % i, [128, 2048], BF16) for i in range(3)]
            self.SQ = [sb("sq%d" % i, [128, 512], BF16) for i in range(2)]
            self.SD = [sb("sd%d" % i, [128, 512]) for i in range(2)]
            self.A_WORDS = (nc.sbuf_bytes_remaining // 4) - 64
            self.ARENA = sb("arena", [128, self.A_WORDS])

            self.a_reset()
            cst = self.a_f32(C_END)
            self.dma(cst, cst_d, slot=("c", 0), writes=["cst"])
            self.dma(self.G.rearrange("p l n -> p (l n)"), G_d, slot=("c", 0), writes=["G"])
            self.dma(self.xqg.rearrange("p l n -> p (l n)"), xqg_d, slot=("c", 0), writes=["G"])
            self.dma(self.xkg.rearrange("p l n -> p (l n)"), xkg_d, slot=("c", 0), writes=["G"])
            self.dma(self.evq.rearrange("p l n -> p (l n)"), evq_d, slot=("c", 0), writes=["G"])
            self.dma(self.evk.rearrange("p l n -> p (l n)"), evk_d, slot=("c", 0), writes=["G"])
            self.dma(self.evlng.rearrange("p l n -> p (l n)"), evlng_d, slot=("c", 0), writes=["G"])
            self.dma(self.evlnb.rearrange("p l n -> p (l n)"), evlnb_d, slot=("c", 0), writes=["G"])
            self.dma(self.ogain.rearrange("p l n -> p (l n)"), og_d, slot=("c", 0), writes=["G"])
            self.cp("dve", cbf[:, 0:1920], cst[:, 0:1920], reads=["cst"], writes=["cbf"])
            self.cp("dve", cf[:, 0:128], cst[:, C_ID:C_ID + 128], reads=["cst"], writes=["cf"])
            self.cp("dve", cf[:, 128:384], cst[:, C_C01:C_C01 + 256], reads=["cst"], writes=["cf"])
            self.cp("dve", cf[:, 384:386], cst[:, C_INVF:C_INVF + 2], reads=["cst"], writes=["cf"])
            self.memset("dve", self.eps_t, EPS, writes=["eps"])
            self.memset("dve", self.one_t, 1.0, writes=["eps"])
            S.barrier()

            self.a_reset()
            posi = self.a_f32(2048).bitcast(I32)
            posf = self.a_f32(2048)
            tq = self.a_f32(2048)
            tn_i = self.a_f32(2048).bitcast(I32)
            tn = self.a_f32(2048)
            self.dma(posi, pos_d[0:1, :].partition_broadcast(128), slot=("c", 1), writes=["posi"])
            self.cp("dve", posf, posi, reads=["posi"], writes=["posf"])
            inv2pi = float(1.0 / (2.0 * np.pi))
            for which in range(2):
                self.ts("dve", tq, posf, invf_t, None, ALU.mult, None, reads=["posf"], writes=["tq"])
                self.ts("dve", tq, tq, inv2pi, 0.25 if which == 0 else 0.0, ALU.mult, ALU.add, reads=["tq"], writes=["tq"])
                self.cp("dve", tn_i, tq, reads=["tq"], writes=["tn_i"])
                self.cp("dve", tn, tn_i, reads=["tn_i"], writes=["tn"])
                self.tt("dve", tq, tq, tn, ALU.subtract, reads=["tq", "tn"], writes=["tq"])
                self.ts("dve", tq, tq, -0.4999, 0.4999, ALU.max, ALU.min, reads=["tq"], writes=["tq"])
                if which == 0:
                    self.act(self.ropeC, tq, AF.Sin, reads=["tq"], writes=["ropeC"], scale=float(2.0 * np.pi))
                else:
                    self.act(tn, tq, AF.Sin, reads=["tq"], writes=["tn"], scale=float(2.0 * np.pi))
                    self.ts("dve", self.ropeS, tn, sgn_t, None, ALU.mult, None, reads=["tn"], writes=["ropeS"])
            S.barrier()

            self.a_reset()
            mt_f = self.a_f32(1024)
            mt_j = self.a_f32(1024)
            mt_b = self.a_bf(1024)
            mss = self.a_f32(2)
            for mt in range(2):
                self.dma(mt_f, mem_d[mt * 128:(mt + 1) * 128, :], slot=("c", 2), writes=["mt_f"])
                self.act(mt_j, mt_f, AF.Square, reads=["mt_f"], writes=["mt_j", "mss"], accum_out=mss[:, 0:1])
                self.act(mss[:, 1:2], mss[:, 0:1], AF.Sqrt, reads=["mss"], writes=["mss2"], bias=self.eps_t[:, 0:1], scale=1.0 / D)
                self.recip(mss[:, 1:2], mss[:, 1:2], reads=["mss2"], writes=["mss2"])
                self.ts("dve", mt_b, mt_f, mss[:, 1:2], None, ALU.mult, None, reads=["mt_f", "mss2"], writes=["mt_b"])
                for kk in range(0, 8, 4):
                    b = self.bank()
                    pb = self.psb(b)
                    for k in range(kk, kk + 4):
                        self.tr(pb[:, 128 * (k - kk):128 * (k - kk) + 128], mt_b[:, 128 * k:128 * k + 128], self.ident_bf, reads=["mt_b"], writes=[("ps", b)])
                    self.cp("dve", self.memT[:, kk:kk + 4, mt * 128:(mt + 1) * 128], pb[:, 0:512].rearrange("p (k n) -> p k n", k=4),
                            reads=[("ps", b)], writes=["memT"])
            S.barrier()

            self.a_reset()
            xs = [self.a_f32(1024) for _ in range(4)]
            for t in range(4):
                for tt in range(4):
                    tok = 4 * t + tt
                    self.dma(xs[tt], x_d[tok * 128:(tok + 1) * 128, :], slot=("xs", tt), writes=[("xs", tt)])
                for k in range(8):
                    b = self.bank()
                    for tt in range(4):
                        self.tr(self.psf(b, 128 * tt, 128 * tt + 128), xs[tt][:, 128 * k:128 * k + 128], self.ident_f, reads=[("xs", tt)], writes=[("ps", b)])
                    self.cp("dve" if k % 2 == 0 else "act", self.xT[:, k, 512 * t:512 * t + 512], self.psf(b), reads=[("ps", b)], writes=[("xT", k, t)])

            for l in self.layers:
                if "mix" in self.subs:
                    if l % 2 == 0:
                        self.even(l)
                    else:
                        self.gla(l)
                if "xa" in self.subs:
                    self.xattn(l)
                if "ffn" in self.subs:
                    self.ffn(l)

            S.barrier()
            self.a_reset()
            os_ = [self.a_f32(1024) for _ in range(2)]
            stores = []
            for tok in range(16):
                ob = os_[tok % 2]
                t = tok // 4
                for kk in range(0, 8, 4):
                    b = self.bank()
                    for k in range(kk, kk + 4):
                        self.tr(self.psf(b, 128 * (k - kk), 128 * (k - kk) + 128), self.xT[:, k, tok * 128:(tok + 1) * 128], self.ident_f,
                                reads=[("xT", k, t)], writes=[("ps", b)])
                    self.cp("dve" if kk == 0 else "act", ob[:, 128 * kk:128 * kk + 512], self.psf(b), reads=[("ps", b)], writes=[("os", tok % 2, kk)])
                stores.append(self.dma(out_d[tok * 128:(tok + 1) * 128, :], ob, slot=("out", tok % 2), reads=[("os", tok % 2, 0), ("os", tok % 2, 4)]))
            S.final_wait("sp", stores)
            S.finalize(es)
            block = es.enter_context(nc.Block())
            S.emit(block)
        return nc


def _pk(v, n):
    v = np.asarray(v, np.float32)
    lead = v.shape[:-1]
    a = v.reshape(lead + (n, 128))
    a = np.moveaxis(a, -1, 0)
    return np.ascontiguousarray(a.reshape(128, -1))


def _host_inputs(inp):
    f = lambda k: np.ascontiguousarray(np.asarray(inp[k], np.float32))
    gains = np.concatenate([_pk(inp["norm_mix"], 8).reshape(128, DEPTH, 8), _pk(inp["norm_mem_q"], 8).reshape(128, DEPTH, 8),
                            _pk(inp["norm_mem_kv"], 8).reshape(128, DEPTH, 8), _pk(inp["norm_ffn"], 8).reshape(128, DEPTH, 8)], axis=2)
    shared = {
        "consts": _consts(),
        "gains": np.ascontiguousarray(gains.reshape(128, DEPTH * 32)),
        "xa_qg": _pk(inp["xa_q_gain"], 2),
        "xa_kg": _pk(inp["xa_k_gain"], 2),
        "ev_qg": np.ascontiguousarray(np.tile(np.asarray(inp["ev_q_gain"], np.float32), (1, 2)).T),
        "ev_kg": np.ascontiguousarray(np.tile(np.asarray(inp["ev_k_gain"], np.float32), (1, 2)).T),
        "ev_lng": _pk(np.asarray(inp["ev_ln_g"], np.float32).reshape(2, 512), 4),
        "ev_lnb": _pk(np.asarray(inp["ev_ln_b"], np.float32).reshape(2, 512), 4),
        "od_og": _pk(np.asarray(inp["od_o_gain"], np.float32).reshape(2, 1024), 8),
    }
    for k in ("ev_w_in", "ev_w_s", "ev_b_s", "ev_w_out", "od_w_in", "od_w_g1", "od_w_g2", "od_b_g", "od_w_out",
              "xa_w_q", "xa_w_kv", "xa_w_out", "ffn_w_gu", "ffn_w_down"):
        shared[k] = f(k)
    x = f("x")
    mem = f("mem")
    pos = np.ascontiguousarray(np.asarray(inp["positions"], np.int32))
    maps = []
    for b in range(8):
        m = dict(shared)
        m["x"] = x[b]
        m["mem"] = mem[b]
        m["positions"] = pos[b:b + 1]
        maps.append(m)
    return maps


_NC_CACHE = {}


def run_layers(inp, layers, subs=("mix", "xa", "ffn"), cores=8, stop_at=0):
    key = (tuple(layers), tuple(subs), stop_at)
    if key not in _NC_CACHE:
        bld = Builder(layers, subs)
        bld.stop_at = stop_at
        _NC_CACHE[key] = bld.build()
    nc = _NC_CACHE[key]
    maps = _host_inputs(inp)[:cores]
    res = run_bass_kernel_spmd(nc, maps, core_ids=list(range(cores)))
    return np.stack([np.asarray(r["out"], np.float32) for r in res.results], axis=0)


def kernel(**inputs):
    return run_layers(inputs, list(range(DEPTH)))
```
